# Optimizing a Trainium2 kernel written in Bass

```python
import math
import jax, jax.numpy as jnp
from jax import lax
import numpy as np

D_MODEL = 1024
BATCH = 8
SEQ = 2048
DEPTH = 2

CHUNK = 64
QBLOCK = 64
N_EVEN = (DEPTH + 1) // 2
N_ODD = DEPTH // 2

A_HEADS = 8
A_HEAD_DIM = 64
A_WIDTH = A_HEADS * A_HEAD_DIM
IDX_HEADS = 16
IDX_DIM = 64
TOPK_MAX = 256
REL_BUCKETS = 32
REL_MAX_DIST = 128
B_GROUPS = 8
B_GROUP_DIM = 64
B_WIDTH = B_GROUPS * B_GROUP_DIM
B_CHUNK = 128
C_HEADS = 8
C_KEY_DIM = 128
C_VAL_DIM = 128
C_WIDTH = C_HEADS * C_VAL_DIM
D_FF = 2816
CONV_WIDTH = 3
EPS = 1e-6

AB_SPLITS = (A_WIDTH, A_WIDTH, A_WIDTH, IDX_HEADS * IDX_DIM, IDX_DIM, IDX_HEADS, B_WIDTH, B_WIDTH)
AB_IN = sum(AB_SPLITS)
C_SPLITS = (C_HEADS * C_KEY_DIM, C_HEADS * C_KEY_DIM, C_WIDTH, C_WIDTH)
C_IN = sum(C_SPLITS)

kernel_name = 'hybrid_dsa_gmlp_hgrn2_convffn'

F32 = jnp.float32


def rms_norm(x, g):
    xf = x.astype(F32)
    y = xf * lax.rsqrt(jnp.mean(xf * xf, axis=-1, keepdims=True) + EPS)
    return (y * g.astype(F32)).astype(x.dtype)


def split_cols(h, sizes):
    offs = np.cumsum(sizes)[:-1].tolist()
    return jnp.split(h, offs, axis=-1)


def t5_bucket(rel):
    half = REL_BUCKETS // 2
    max_exact = half // 2
    ret = jnp.where(rel > 0, half, 0)
    n = jnp.abs(rel)
    nf = jnp.maximum(n, max_exact).astype(F32)
    large = max_exact + (jnp.log(nf / max_exact) / math.log(REL_MAX_DIST / max_exact)
                         * (half - max_exact)).astype(jnp.int32)
    large = jnp.minimum(large, half - 1)
    return ret + jnp.where(n < max_exact, n, large)


def dsa_attention(q, k, v, iq, ik, iw, rel_bias):
    bsz, seq = q.shape[0], q.shape[1]
    top_k = min(TOPK_MAX, seq // 4)
    n_blocks = seq // QBLOCK
    key_chunk = jnp.arange(seq) // CHUNK
    kv = jnp.concatenate([k, v], axis=-1)
    ik32 = ik.astype(F32)
    idx_scale = IDX_DIM ** -0.5
    w_scale = IDX_HEADS ** -0.5
    att_scale = A_HEAD_DIM ** -0.5

    def block(i):
        start = i * QBLOCK
        t = start + jnp.arange(QBLOCK)
        q_b = lax.dynamic_slice_in_dim(q, start, QBLOCK, axis=1)
        iq_b = lax.dynamic_slice_in_dim(iq, start, QBLOCK, axis=1)
        iw_b = lax.dynamic_slice_in_dim(iw, start, QBLOCK, axis=1)
        s = jnp.einsum('bqhd,bsd->bqhs', iq_b.astype(F32), ik32) * idx_scale
        score = jnp.einsum('bqhs,bqh->bqs', jax.nn.relu(s), iw_b.astype(F32) * w_scale)
        allowed = key_chunk[None, :] <= (t // CHUNK)[:, None]
        score = jnp.where(allowed[None], score, -jnp.inf)
        _, sel = lax.top_k(score, top_k)
        kv_sel = jax.vmap(lambda kvb, ib: kvb[ib])(kv, sel)
        k_sel, v_sel = jnp.split(kv_sel, 2, axis=-1)
        valid = (sel // CHUNK) <= (t // CHUNK)[None, :, None]
        bias = rel_bias[t5_bucket(sel - t[None, :, None])]
        logits = (jnp.einsum('bqhd,bqkhd->bhqk', q_b, k_sel).astype(F32) * att_scale
                  + jnp.transpose(bias, (0, 3, 1, 2)).astype(F32))
        logits = jnp.where(valid[:, None], logits, -jnp.inf)
        p = jax.nn.softmax(logits, axis=-1).astype(v.dtype)
        return jnp.einsum('bhqk,bqkhd->bqhd', p, v_sel)

    out = lax.map(block, jnp.arange(n_blocks))
    return jnp.transpose(out, (1, 0, 2, 3, 4)).reshape(bsz, seq, A_WIDTH)


def gmlp_spatial_gate(u, v, norm_g, w_s, b_s):
    bsz, seq = u.shape[0], u.shape[1]
    v = rms_norm(v, norm_g)
    pos_chunk = jnp.arange(B_CHUNK) // CHUNK
    mask = (pos_chunk[:, None] >= pos_chunk[None, :]).astype(w_s.dtype)
    vb = v.reshape(bsz, seq // B_CHUNK, B_CHUNK, B_GROUPS, B_GROUP_DIM)
    s = (jnp.einsum('gij,bnjgc->bnigc', w_s * mask, vb)
         + jnp.transpose(b_s)[None, None, :, :, None])
    return u * s.reshape(bsz, seq, B_WIDTH)


def hgrn2(q, f, i, lb):
    bsz, seq = q.shape[0], q.shape[1]
    lb = lb.astype(F32)
    q = jax.nn.silu(q.astype(F32))
    g = lb + (1.0 - lb) * jax.nn.sigmoid(f.astype(F32))
    k = 1.0 - g
    log_g = jnp.log(g)
    nc = seq // CHUNK

    def to_chunks(a, d):
        return a.reshape(bsz, nc, CHUNK, C_HEADS, d).transpose(1, 0, 3, 2, 4)

    qc = to_chunks(q, C_KEY_DIM)
    kc = to_chunks(k, C_KEY_DIM)
    lgc = to_chunks(log_g, C_KEY_DIM)
    ic = to_chunks(i.astype(F32), C_VAL_DIM)
    causal = (jnp.arange(CHUNK)[:, None] >= jnp.arange(CHUNK)[None, :])[:, :, None]

    def step(S, xs):
        qb, kb, ib, lgb = xs
        b = jnp.cumsum(lgb, axis=-2)
        inter = jnp.einsum('bhtd,bhde->bhte', qb * jnp.exp(b), S)
        diff = b[:, :, :, None, :] - b[:, :, None, :, :]
        decay = jnp.where(causal, jnp.exp(jnp.where(causal, diff, 0.0)), 0.0)
        a = jnp.einsum('bhtd,bhtsd,bhsd->bhts', qb, decay, kb)
        intra = jnp.einsum('bhts,bhse->bhte', a, ib)
        b_last = b[:, :, -1:, :]
        S_new = (jnp.exp(b_last[:, :, 0, :])[..., None] * S
                 + jnp.einsum('bhsd,bhse->bhde', kb * jnp.exp(b_last - b), ib))
        return S_new, inter + intra

    S0 = jnp.zeros((bsz, C_HEADS, C_KEY_DIM, C_VAL_DIM), F32)
    _, o = lax.scan(step, S0, (qc, kc, ic, lgc))
    return o.transpose(1, 0, 3, 2, 4).reshape(bsz, seq, C_HEADS, C_VAL_DIM)


def conv_ffn(x, w_up, conv_w, conv_b, w_down):
    seq = x.shape[1]
    h = x @ w_up
    hp = jnp.pad(h, ((0, 0), (CONV_WIDTH - 1, 0), (0, 0)))
    y = conv_b + conv_w[CONV_WIDTH - 1] * hp[:, CONV_WIDTH - 1:CONV_WIDTH - 1 + seq]
    for j in range(CONV_WIDTH - 1):
        y = y + conv_w[j] * hp[:, j:j + seq]
    a, b = jnp.split(y, 2, axis=-1)
    return (jax.nn.silu(a) * b) @ w_down


def setup_inputs(seed: int = 0) -> dict:
    key = jax.random.key(seed)
    ks = jax.random.split(key, 19)

    def nrm(k, shape, scale):
        return jax.random.normal(k, shape, F32) * scale

    def gain(k, shape):
        return 1.0 + 0.05 * jax.random.normal(k, shape, F32)

    return {
        'x': nrm(ks[0], (BATCH, SEQ, D_MODEL), 1.0),
        'rel_bias': nrm(ks[1], (REL_BUCKETS, A_HEADS), 0.5),
        'hgrn_lb': nrm(ks[2], (DEPTH, C_HEADS * C_KEY_DIM), 0.5),
        'mix_norm': gain(ks[3], (DEPTH, D_MODEL)),
        'ffn_norm': gain(ks[4], (DEPTH, D_MODEL)),
        'final_norm': gain(ks[5], (D_MODEL,)),
        'ab_w_in': nrm(ks[6], (N_EVEN, D_MODEL, AB_IN), D_MODEL ** -0.5),
        'ab_idx_k_norm': gain(ks[7], (N_EVEN, IDX_DIM)),
        'ab_gmlp_norm': gain(ks[8], (N_EVEN, B_WIDTH)),
        'ab_w_s': nrm(ks[9], (N_EVEN, B_GROUPS, B_CHUNK, B_CHUNK), B_CHUNK ** -0.5),
        'ab_b_s': 1.0 + nrm(ks[10], (N_EVEN, B_GROUPS, B_CHUNK), 0.02),
        'ab_w_out': nrm(ks[11], (N_EVEN, A_WIDTH + B_WIDTH, D_MODEL), (A_WIDTH + B_WIDTH) ** -0.5),
        'c_w_in': nrm(ks[12], (N_ODD, D_MODEL, C_IN), D_MODEL ** -0.5),
        'c_out_norm': gain(ks[13], (N_ODD, C_VAL_DIM)),
        'c_w_out': nrm(ks[14], (N_ODD, C_WIDTH, D_MODEL), C_WIDTH ** -0.5),
        'ffn_w_up': nrm(ks[15], (DEPTH, D_MODEL, 2 * D_FF), D_MODEL ** -0.5),
        'ffn_conv_w': nrm(ks[16], (DEPTH, CONV_WIDTH, 2 * D_FF), CONV_WIDTH ** -0.5),
        'ffn_conv_b': nrm(ks[17], (DEPTH, 2 * D_FF), 0.02),
        'ffn_w_down': nrm(ks[18], (DEPTH, D_FF, D_MODEL), D_FF ** -0.5),
    }


def reference(x, rel_bias, hgrn_lb, mix_norm, ffn_norm, final_norm,
              ab_w_in, ab_idx_k_norm, ab_gmlp_norm, ab_w_s, ab_b_s, ab_w_out,
              c_w_in, c_out_norm, c_w_out,
              ffn_w_up, ffn_conv_w, ffn_conv_b, ffn_w_down):
    bsz, seq, _ = x.shape
    lb_all = jnp.cumsum(jax.nn.softmax(hgrn_lb.astype(F32), axis=0), axis=0)
    lb_all = lb_all - lb_all[0:1]
    for l in range(DEPTH):
        h = rms_norm(x, mix_norm[l])
        if l % 2 == 0:
            e = l // 2
            q, k, v, iq, ik, iw, u, vg = split_cols(h @ ab_w_in[e], AB_SPLITS)
            ik = rms_norm(ik, ab_idx_k_norm[e])
            y_a = dsa_attention(q.reshape(bsz, seq, A_HEADS, A_HEAD_DIM),
                                k.reshape(bsz, seq, A_HEADS, A_HEAD_DIM),
                                v.reshape(bsz, seq, A_HEADS, A_HEAD_DIM),
                                iq.reshape(bsz, seq, IDX_HEADS, IDX_DIM),
                                ik, iw, rel_bias)
            y_b = gmlp_spatial_gate(jax.nn.gelu(u), jax.nn.gelu(vg),
                                    ab_gmlp_norm[e], ab_w_s[e], ab_b_s[e])
            y = jnp.concatenate([y_a, y_b], axis=-1) @ ab_w_out[e]
        else:
            o_i = l // 2
            q, f, i, gate = split_cols(h @ c_w_in[o_i], C_SPLITS)
            o = hgrn2(q, f, i, lb_all[l])
            o = rms_norm(o, c_out_norm[o_i]).reshape(bsz, seq, C_WIDTH).astype(x.dtype)
            y = (o * jax.nn.silu(gate)) @ c_w_out[o_i]
        x = x + y
        x = x + conv_ffn(rms_norm(x, ffn_norm[l]), ffn_w_up[l], ffn_conv_w[l],
                         ffn_conv_b[l], ffn_w_down[l])
    return rms_norm(x, final_norm)
```

```python
import numpy as np
from contextlib import ExitStack, contextmanager
import concourse.bass as bass
import concourse.mybir as mybir
from concourse.bass_utils import run_bass_kernel_spmd

F32 = mybir.dt.float32
BF16 = mybir.dt.bfloat16
ALU = mybir.AluOpType
AF = mybir.ActivationFunctionType
AX = mybir.AxisListType

T = 2048
D = 1024
NT = 16
EPS = 1e-6
D_FF = 2816
NJ = 22
TOPK = 256
NBIS = 22
NEG = -30000.0


class Tok:
    __slots__ = ("sem", "key", "val", "clock")

    def __init__(self, sem, key, val, clock):
        self.sem, self.key, self.val, self.clock = sem, key, val, clock


class Reg:
    __slots__ = ("w", "r")

    def __init__(self):
        self.w = None
        self.r = {}


class Prog:
    def __init__(self, nc):
        self.nc = nc
        self.es = ExitStack()
        self.E = {"pe": nc.tensor, "act": nc.scalar, "dve": nc.vector, "pool": nc.gpsimd, "sp": nc.sync}
        self.csem = {e: self.es.enter_context(nc.semaphore("cs_" + e)) for e in ("pe", "act", "dve", "pool")}
        self.ccnt = {e: 0 for e in self.csem}
        self.known = {e: {} for e in self.E}
        self.dq = {}
        for q, n in (("sp", 24), ("pool", 16)):
            self.dq[q] = dict(
                sems=[self.es.enter_context(nc.semaphore("d_%s%d" % (q, i))) for i in range(n)],
                use=[0] * n, last=[None] * n, nxt=0)
        self.regs = {}
        self.last = {e: None for e in self.csem}
        self.phase_stack = None
        self.ninst = 0
        self.nwait = 0
        self.nsb = 0

    @staticmethod
    def key(x, sub=None):
        if isinstance(x, tuple):
            return x
        if isinstance(x, str):
            return (x, None)
        name = x.tensor.name if hasattr(x, "tensor") else x.name
        if sub and name in sub:
            return (name, sub[name])
        return (name, None)

    def _targets(self, k):
        name, s = k
        d = self.regs.setdefault(name, {})
        if s is None:
            if None not in d:
                d[None] = Reg()
            return list(d.values())
        if s not in d:
            d[s] = Reg()
        out = [d[s]]
        if None in d:
            out.append(d[None])
        return out

    def _wait(self, eng, tok):
        if tok is None:
            return
        if eng == "pe" and tok.key == "pe":
            return
        k = self.known[eng]
        if k.get(tok.key, 0) >= tok.val:
            return
        if tok.key in self.ccnt:
            assert tok.val <= self.ccnt[tok.key], ("wait on future inc", eng, tok.key, tok.val)
        self.E[eng].wait_ge(tok.sem, tok.val)
        self.nwait += 1
        for kk, vv in tok.clock.items():
            if k.get(kk, 0) < vv:
                k[kk] = vv

    def _sync(self, eng, reads, writes):
        for k in reads:
            for rg in self._targets(k):
                self._wait(eng, rg.w)
        for k in writes:
            for rg in self._targets(k):
                self._wait(eng, rg.w)
                for t in list(rg.r.values()):
                    self._wait(eng, t)

    def _record(self, tok, reads, writes):
        for k in reads:
            name, s = k
            rg = self.regs[name][s]
            old = rg.r.get(tok.key)
            if old is None or old.val < tok.val:
                rg.r[tok.key] = tok
        for k in writes:
            name, s = k
            rg = self.regs[name][s]
            rg.w = tok
            rg.r = {}

    def op(self, eng, fn, reads, writes, inc=True):
        reads = [self.key(r) for r in reads]
        writes = [self.key(w) for w in writes]
        self._sync(eng, reads, writes)
        inst = fn(self.E[eng])
        self.ninst += 1
        if inc:
            self.ccnt[eng] += 1
            inst.then_inc(self.csem[eng], 1)
            val = self.ccnt[eng]
        else:
            val = self.ccnt[eng] + 1
        clock = dict(self.known[eng])
        clock[eng] = max(clock.get(eng, 0), val)
        tok = Tok(self.csem[eng], eng, val, clock)
        self.last[eng] = tok
        self._record(tok, reads, writes)
        return tok

    def dma(self, q, out, in_, sub=None):
        reads = [self.key(in_, sub)]
        writes = [self.key(out, sub)]
        self._sync(q, reads, writes)
        d = self.dq[q]
        j = d["nxt"]
        d["nxt"] = (j + 1) % len(d["sems"])
        self._wait(q, d["last"][j])
        inst = self.E[q].dma_start(out=out, in_=in_)
        self.ninst += 1
        d["use"][j] += 1
        inst.then_inc(d["sems"][j], 16)
        key = ("d", q, j)
        val = 16 * d["use"][j]
        clock = dict(self.known[q])
        clock[key] = val
        tok = Tok(d["sems"][j], key, val, clock)
        d["last"][j] = tok
        self._record(tok, reads, writes)
        return tok

    def barrier(self):
        toks = [t for t in self.last.values() if t is not None]
        for d in self.dq.values():
            toks += [t for t in d["last"] if t is not None]
        for e in self.E:
            for t in toks:
                self._wait(e, t)

    def sb(self, name, shape, dtype, persistent=False):
        st = self.es if (persistent or self.phase_stack is None) else self.phase_stack
        self.nsb += 1
        return st.enter_context(self.nc.sbuf_tensor("s%d_%s" % (self.nsb, name), list(shape), dtype))

    @contextmanager
    def phase(self):
        prev = self.phase_stack
        self.phase_stack = ExitStack()
        try:
            yield
            self.barrier()
        finally:
            self.phase_stack.close()
            self.phase_stack = prev

    def _aps(self, sub, *xs):
        return [self.key(x, sub) for x in xs if x is not None and not isinstance(x, (int, float))]

    def mm(self, out, lhsT, rhs, start=True, stop=True, inc=None, sub=None):
        if inc is None:
            inc = stop
        return self.op("pe", lambda e: e.matmul(out, lhsT, rhs, start=start, stop=stop),
                       self._aps(sub, lhsT, rhs), self._aps(sub, out), inc=inc)

    def tr(self, out, in_, ident, inc=True, sub=None):
        return self.op("pe", lambda e: e.transpose(out, in_, ident),
                       self._aps(sub, in_, ident), self._aps(sub, out), inc=inc)

    def act(self, out, in_, func, bias=None, scale=None, accum_out=None, sub=None):
        kw = {}
        if bias is not None:
            kw["bias"] = bias
        if scale is not None:
            kw["scale"] = scale
        if accum_out is not None:
            kw["accum_out"] = accum_out
        return self.op("act", lambda e: e.activation(out=out, in_=in_, func=func, **kw),
                       self._aps(sub, in_, bias, scale), self._aps(sub, out, accum_out))

    def ts(self, eng, out, in0, s1, s2, op0, op1=None, accum_out=None, sub=None):
        kw = {}
        if op1 is not None:
            kw["op1"] = op1
        if accum_out is not None:
            kw["accum_out"] = accum_out
        return self.op(eng, lambda e: e.tensor_scalar(out=out, in0=in0, scalar1=s1, scalar2=s2, op0=op0, **kw),
                       self._aps(sub, in0, s1, s2), self._aps(sub, out, accum_out))

    def stt(self, out, in0, scalar, in1, op0, op1, sub=None):
        return self.op("dve", lambda e: e.scalar_tensor_tensor(out=out, in0=in0, scalar=scalar, in1=in1, op0=op0, op1=op1),
                       self._aps(sub, in0, scalar, in1), self._aps(sub, out))

    def tt(self, eng, out, in0, in1, op, sub=None):
        return self.op(eng, lambda e: e.tensor_tensor(out=out, in0=in0, in1=in1, op=op),
                       self._aps(sub, in0, in1), self._aps(sub, out))

    def copy(self, eng, out, in_, sub=None):
        if eng == "act":
            return self.act(out, in_, AF.Copy, sub=sub)
        return self.op(eng, lambda e: e.tensor_copy(out=out, in_=in_), self._aps(sub, in_), self._aps(sub, out))

    def memset(self, eng, ap, val, sub=None):
        return self.op(eng, lambda e: e.memset(ap, val), [], self._aps(sub, ap))

    def recip(self, out, in_, sub=None):
        return self.op("dve", lambda e: e.reciprocal(out=out, in_=in_), self._aps(sub, in_), self._aps(sub, out))

    def reduce(self, out, in_, op, sub=None):
        return self.op("dve", lambda e: e.tensor_reduce(out=out, in_=in_, axis=AX.X, op=op),
                       self._aps(sub, in_), self._aps(sub, out))

    def scan(self, out, d0, d1, initial, op0, op1, sub=None):
        return self.op("dve", lambda e: e.tensor_tensor_scan(out=out, data0=d0, data1=d1, initial=initial, op0=op0, op1=op1),
                       self._aps(sub, d0, d1), self._aps(sub, out))


class Ctx:
    pass


def rmsnorm_fm(p, c, g_ap, hT, t0, nt, nparts=128, kch=8, src=None, scale_div=None):
    src = c.xT if src is None else src
    div = float(scale_div if scale_div is not None else nparts * kch)
    for i, tb in enumerate(range(t0, t0 + nt, 512)):
        sq = c.sq[i % 2]
        p.act(sq[:nparts, :kch, :], src[:nparts, :kch, tb:tb + 512], AF.Square)
        ps = c.ps[c.psi % 8]
        c.psi += 1
        for k in range(kch):
            p.mm(ps[:nparts, :], c.ones_b[:nparts, :nparts], sq[:nparts, k, :], start=(k == 0), stop=(k == kch - 1))
        rs = c.rs[i % 2]
        p.act(rs[:nparts, :], ps[:nparts, :], AF.Sqrt, bias=c.eps_ap[:nparts, :], scale=1.0 / div)
        p.recip(rs[:nparts, :], rs[:nparts, :])
        for k in range(kch):
            p.stt(hT[:nparts, k, tb - t0:tb - t0 + 512], src[:nparts, k, tb:tb + 512], g_ap[:nparts, k:k + 1],
                  rs[:nparts, :], ALU.mult, ALU.mult)


def proj_stream(p, c, mode, hT, w_l, consume, kch=8, tsl=None, wname="wb"):
    nblk, _, _, cw = w_l.shape
    ntok = hT.shape[2] if tsl is None else tsl
    wbs = c.wb[wname]

    def load(b):
        p.dma("pool", wbs[b % 2][:, :kch, :cw], w_l[b])

    load(0)
    for b in range(nblk):
        if b + 1 < nblk:
            load(b + 1)
        wb = wbs[b % 2]
        if mode == "fm":
            for tb in range(ntok // 512):
                ps = c.ps[c.psi % 8]
                c.psi += 1
                for k in range(kch):
                    p.mm(ps[:cw, :], wb[:, k, :cw], hT[:, k, tb * 512:(tb + 1) * 512], start=(k == 0), stop=(k == kch - 1))
                consume(b, tb, ps)
        else:
            for tt in range(ntok // 128):
                ps = c.ps[c.psi % 8]
                c.psi += 1
                for k in range(kch):
                    p.mm(ps[:, :cw], hT[:, k, tt * 128:(tt + 1) * 128], wb[:, k, :cw], start=(k == 0), stop=(k == kch - 1))
                consume(b, tt, ps)


def layer0_mixer(p, c, W):
    with p.phase():
        c.nmT = p.sb("nmT", [128, 136, 128], BF16)
        with p.phase():
            c.ik2 = p.sb("ik2", [128, 1, T], F32)
            c.iw_sb = p.sb("iw_sb", [128, NT, 16], F32)
            with p.phase():
                hT = p.sb("hT", [128, 8, T], BF16)
                c.wb = {"wb": [p.sb("wb%d" % i, [128, 8, 512], BF16) for i in range(2)]}
                with p.phase():
                    c.sq = [p.sb("sq%d" % i, [128, 8, 512], BF16) for i in range(2)]
                    c.rs = [p.sb("rs%d" % i, [128, 512], F32) for i in range(2)]
                    rmsnorm_fm(p, c, c.g_mix[0], hT, 0, T)
                    layer0_inproj_a(p, c, W, hT)
                layer0_gmlp(p, c, W, hT)
            if c.stop_after == "gmlp":
                return
            layer0_indexer(p, c, W)
        if c.stop_after == "indexer":
            return
        layer0_attention(p, c, W)
    if c.stop_after == "attn":
        return
    with p.phase():
        yT = p.sb("yT", [128, 8, T], BF16)
        for k in range(8):
            p.dma("sp", yT[:, k, :], c.yT_d[k])
        out_proj_residual(p, c, W["w_out0"], yT)


def layer0_inproj_a(p, c, W, hT):
    if True:
        stg = [p.sb("stg%d" % i, [128, T], BF16) for i in range(2)]
        cnt = [0]

        def cons_fm(dst, scale=None):
            def f(b, tb, ps):
                st = stg[b % 2]
                eng = "act" if (cnt[0] % 2 == 0) else "dve"
                cnt[0] += 1
                if scale is None:
                    p.copy(eng, st[:, tb * 512:(tb + 1) * 512], ps[:, :])
                elif eng == "act":
                    p.act(st[:, tb * 512:(tb + 1) * 512], ps[:, :], AF.Copy, scale=scale)
                else:
                    p.ts("dve", st[:, tb * 512:(tb + 1) * 512], ps[:, :], scale, None, ALU.mult)
                if tb == 3:
                    p.dma("sp", dst[b], st[:, :])
            return f

        proj_stream(p, c, "fm", hT, W["w_q"], cons_fm(c.qT_d, 0.125))
        proj_stream(p, c, "fm", hT, W["w_k"], cons_fm(c.kT_d))
        proj_stream(p, c, "fm", hT, W["w_iq"], cons_fm(c.iqT_d))

        def cons_ik(b, tb, ps):
            p.copy("act", c.ik2[:, 0, tb * 512:(tb + 1) * 512], ps[:, :])
        proj_stream(p, c, "fm", hT, W["w_ik2"], cons_ik)

        vst = [p.sb("vst%d" % i, [128, 512], BF16) for i in range(2)]

        def cons_v(b, tt, ps):
            st = vst[tt % 2]
            p.copy("act" if tt % 2 == 0 else "dve", st[:, :], ps[:, :])
            p.dma("sp", c.v_d[tt * 128:(tt + 1) * 128, :], st[:, :])
        proj_stream(p, c, "tm", hT, W["w_v"], cons_v)

        def cons_iw(b, tt, ps):
            p.copy("dve", c.iw_sb[:, tt, :], ps[:, 0:16])
        proj_stream(p, c, "tm", hT, W["w_iw"], cons_iw)


def layer0_gmlp(p, c, W, hT):
    if True:
        u_all = p.sb("u_all", [128, NT, 512], BF16)
        vn_all = p.sb("vn_all", [128, NT, 512], BF16)
        vgf = [p.sb("vgf%d" % i, [128, 512], F32) for i in range(2)]
        junk = p.sb("junk", [128, 512], BF16)
        ssv = p.sb("ssv", [128, NT], F32)
        rsv = p.sb("rsv", [128, NT], F32)
        gn_bc = p.sb("gn_bc", [128, 512], F32)
        p.dma("sp", gn_bc[:, :], W["gmlp_norm"].partition_broadcast(128))

        def cons_u(b, tt, ps):
            p.act(u_all[:, tt, :], ps[:, :], AF.Gelu_apprx_tanh)
        proj_stream(p, c, "tm", hT, W["w_u"], cons_u)

        def cons_vg(b, tt, ps):
            vg = vgf[tt % 2]
            p.act(vg[:, :], ps[:, :], AF.Gelu_apprx_tanh)
            p.act(junk[:, :], vg[:, :], AF.Square, accum_out=ssv[:, tt:tt + 1])
            p.act(rsv[:, tt:tt + 1], ssv[:, tt:tt + 1], AF.Sqrt, bias=c.eps_ap[:, :], scale=1.0 / 512)
            p.recip(rsv[:, tt:tt + 1], rsv[:, tt:tt + 1])
            p.stt(vn_all[:, tt, :], vg[:, :], rsv[:, tt:tt + 1], gn_bc[:, :], ALU.mult, ALU.mult)
        proj_stream(p, c, "tm", hT, W["w_vg"], cons_vg)

        wsT = p.sb("wsT", [128, 8, 128], BF16)
        p.dma("pool", wsT[:, :, :], W["w_sT"])
        p.memset("dve", wsT[64:128, :, 0:64], 0.0)
        bs8 = p.sb("bs8", [8, 128], BF16)
        p.dma("pool", bs8[:, :], W["b_s"])
        e8 = p.sb("e8", [8, 512], BF16)
        p.dma("pool", e8[:, :], W["e8"])
        ybs = [p.sb("ybs%d" % i, [128, 512], BF16) for i in range(2)]
        ybT = [p.sb("ybT%d" % i, [128, 512], BF16) for i in range(2)]
        for n in range(NT):
            ps = c.ps[c.psi % 8]
            c.psi += 1
            for g in range(8):
                p.mm(ps[:, g * 64:(g + 1) * 64], wsT[:, g, :], vn_all[:, n, g * 64:(g + 1) * 64], start=True, stop=False, inc=False)
                p.mm(ps[:, g * 64:(g + 1) * 64], bs8[:, :], e8[:, g * 64:(g + 1) * 64], start=False, stop=True, inc=(g == 7))
            yb = ybs[n % 2]
            p.tt("dve", yb[:, :], ps[:, :], u_all[:, n, :], ALU.mult)
            pst = c.psb[c.psi % 8]
            pstn = c.ps[c.psi % 8]
            c.psi += 1
            for cc in range(4):
                p.op("pe", lambda e, cc=cc: e.transpose(pst[:, cc * 128:(cc + 1) * 128], yb[:, cc * 128:(cc + 1) * 128], c.ident_b[:, :]),
                     [p.key(yb), p.key(c.ident_b)], [p.key(pstn)], inc=(cc == 3))
            yst = ybT[n % 2]
            p.copy("act", yst[:, :], pst[:, 0:512])
            p.dma("sp", c.yT_d[4:8, :, n * 128:(n + 1) * 128].rearrange("c p t -> p c t"),
                  yst[:, :].rearrange("p (a b) -> p a b", a=4))


def tri(b):
    return b * (b + 1) // 2


def layer0_indexer(p, c, W):
    with p.phase():
        ikn = p.sb("ikn", [128, 1, T], BF16)
        gk2 = p.sb("gk2", [128, 1], F32)
        p.dma("sp", gk2[:, :], W["gk2"])
        with p.phase():
            c.sq = [p.sb("isq%d" % i, [128, 1, 512], BF16) for i in range(2)]
            c.rs = [p.sb("irs%d" % i, [128, 512], F32) for i in range(2)]
            rmsnorm_fm(p, c, gk2, ikn, 0, T, nparts=128, kch=1, src=c.ik2, scale_div=128)
        iqz = [p.sb("iqz%d" % i, [128, 8, 2, 128], BF16) for i in range(2)]
        for i in range(2):
            p.memset("pool", iqz[i][:, :, :, :], 0.0)
        acc = [p.sb("acc%d" % i, [128, T], F32) for i in range(2)]
        rr = [p.sb("rr%d" % i, [128, 512], F32) for i in range(3)]
        nm = [p.sb("nm%d" % i, [128, T], BF16) for i in range(2)]
        junk = p.sb("junkc", [128, T], BF16)
        pow2 = p.sb("pow2", [128, NBIS], F32)
        p.dma("sp", pow2[:, :], W["pow2"])
        st = p.sb("bis", [128, NT, 8], F32)
        wtab = p.sb("wtab", [128, NT, NBIS], F32)
        ri = 0
        ei = 0
        for b in range(NT):
            L = 128 * (b + 1)
            nmb = nm[b % 2]
            if b < 2:
                p.memset("pool", nmb[:, :L], 0.0)
                p.memset("pool", nmb[0:64, L - 64:L], NEG)
            else:
                iz = iqz[b % 2]
                for par in range(2):
                    p.dma("sp", iz[par * 64:(par + 1) * 64, :, par, :],
                          c.iqT_d[:, par * 64:(par + 1) * 64, b * 128:(b + 1) * 128].rearrange("m p t -> p m t"))
                ac = acc[b % 2]
                nkb = (L + 511) // 512
                for h in range(16):
                    m, par = divmod(h, 2)
                    for kb in range(nkb):
                        n = min(512, L - kb * 512)
                        ps = c.ps[c.psi % 8]
                        c.psi += 1
                        p.mm(ps[:, :n], iz[:, m, par, :], ikn[:, 0, kb * 512:kb * 512 + n])
                        r = rr[ri % 3]
                        ri += 1
                        p.act(r[:, :n], ps[:, :n], AF.Relu)
                        dst = ac[:, kb * 512:kb * 512 + n]
                        if h == 0:
                            p.ts("dve", dst, r[:, :n], c.iw_sb[:, b, 0:1], None, ALU.mult)
                        else:
                            p.stt(dst, r[:, :n], c.iw_sb[:, b, h:h + 1], dst, ALU.mult, ALU.add)
                mx, mn, w0, cand, cnt, tq = [st[:, b, i:i + 1] for i in range(6)]
                p.reduce(mx, ac[:, :L], ALU.max)
                p.reduce(mn, ac[:, :L], ALU.min)
                p.memset("dve", ac[0:64, L - 64:L], -1e30)
                p.tt("dve", w0, mx, mn, ALU.subtract)
                wt = wtab[:, b, :]
                p.ts("dve", wt, pow2[:, :], w0, None, ALU.mult)
                p.tt("dve", cand, mn, wtab[:, b, 0:1], ALU.add)
                for i in range(NBIS):
                    p.ts("dve", junk[:, :L], ac[:, :L], cand, 0.0, ALU.is_ge, ALU.add, accum_out=cnt)
                    last = (i == NBIS - 1)
                    p.ts("dve", tq, cnt, float(TOPK), (1.0 if last else 0.5), ALU.is_ge, ALU.subtract)
                    p.stt(cand, tq, wtab[:, b, i:i + 1], cand, ALU.mult, ALU.add)
                p.ts("dve", nmb[:, :L], ac[:, :L], cand, NEG, ALU.is_lt, ALU.mult)
            for a0 in range(0, b + 1, 4):
                g = min(4, b + 1 - a0)
                pst = c.psb[c.psi % 8]
                pstn = c.ps[c.psi % 8]
                c.psi += 1
                for j in range(g):
                    a = a0 + j
                    p.op("pe", lambda e, j=j, a=a: e.transpose(pst[:, j * 128:(j + 1) * 128], nmb[:, a * 128:(a + 1) * 128], c.ident_b[:, :]),
                         [p.key(nmb), p.key(c.ident_b)], [p.key(pstn)], inc=(j == g - 1))
                eng = "act" if ei % 2 == 0 else "dve"
                ei += 1
                o_ap = c.nmT[:, tri(b) + a0:tri(b) + a0 + g, :]
                i_ap = pst[:, 0:g * 128].rearrange("p (a b) -> p a b", a=g)
                if eng == "act":
                    p.op("act", lambda e: e.activation(out=o_ap, in_=i_ap, func=AF.Copy), [p.key(pstn)], [p.key(c.nmT, {c.nmT.name: b})])
                else:
                    p.op("dve", lambda e: e.tensor_copy(out=o_ap, in_=i_ap), [p.key(pstn)], [p.key(c.nmT, {c.nmT.name: b})])


def layer0_attention(p, c, W):
    with p.phase():
        kT = p.sb("kT", [128, 4, T], BF16)
        for m in range(4):
            p.dma("sp", kT[:, m, :], c.kT_d[m])
        qzs = [p.sb("qz%d" % i, [128, 4, 2, 128], BF16) for i in range(2)]
        for i in range(2):
            p.memset("pool", qzs[i][:, :, :, :], 0.0)
        va = p.sb("va", [128, NT, 8, 65], BF16)
        p.memset("pool", va[:, :, :, 64:65], 1.0)
        for tt in range(NT):
            p.dma("sp", va[:, tt, :, 0:64], c.v_d[tt * 128:(tt + 1) * 128, :].rearrange("p (h d) -> p h d", h=8))
        oh1 = p.sb("oh1", [32, 383], F32)
        p.dma("sp", oh1[:, :], W["oh1"])
        rb = p.sb("rb", [32, 8], F32)
        p.dma("sp", rb[:, :], W["rel_bias"])
        cb = p.sb("cb", [128, 8], F32)
        p.dma("sp", cb[:, :], W["rel_bias"][15:16, :].partition_broadcast(128))
        Tb = p.sb("Tb", [128, 2, 8, 128], BF16)
        for kind in range(2):
            for t0 in range(0, 128, 64):
                ps = c.ps[c.psi % 8]
                c.psi += 1
                for t in range(t0, t0 + 64):
                    base = (255 - t) if kind == 0 else (127 - t)
                    p.mm(ps[:, (t - t0) * 8:(t - t0) * 8 + 8], oh1[:, base:base + 128], rb[:, :], start=True, stop=True,
                         inc=(t == t0 + 63))
                p.tt("dve", Tb[:, kind, :, t0:t0 + 64], ps[:, 0:512].rearrange("p (t h) -> p h t", h=8),
                     cb[:, :].unsqueeze(2).to_broadcast([128, 8, 64]), ALU.subtract)
        ya = [p.sb("ya%d" % i, [128, 512], BF16) for i in range(2)]
        PT = [p.sb("PT%d" % i, [128, 512], BF16) for i in range(3)]
        rec = p.sb("rec", [128, 16], F32)
        si = 0
        yaT = [p.sb("yaT%d" % i, [128, 512], BF16) for i in range(2)]
        for b in range(NT):
            qz = qzs[b % 2]
            for par in range(2):
                p.dma("sp", qz[par * 64:(par + 1) * 64, :, par, :],
                      c.qT_d[:, par * 64:(par + 1) * 64, b * 128:(b + 1) * 128].rearrange("m p t -> p m t"))
            for h in range(8):
                m = h // 2
                po = c.ps[4 + (b * 8 + h) % 2]
                for a0 in range(0, b + 1, 4):
                    grp = list(range(a0, min(a0 + 4, b + 1)))
                    ps = c.ps[si % 4]
                    pt = PT[si % 3]
                    si += 1
                    for j, a in enumerate(grp):
                        near = (a >= b - 1)
                        lastj = (j == len(grp) - 1)
                        o = ps[:, j * 128:(j + 1) * 128]
                        p.mm(o, kT[:, m, a * 128:(a + 1) * 128], qz[:, h // 2, h % 2, :], start=True, stop=False, inc=False)
                        p.mm(o, c.ident_b[:, :], c.nmT[:, tri(b) + a, :], start=False, stop=(not near), inc=((not near) and lastj))
                        if near:
                            kind = 0 if a == b else 1
                            p.mm(o, c.ident_b[:, :], Tb[:, kind, h, :], start=False, stop=True, inc=lastj)
                    n = len(grp) * 128
                    p.act(pt[:, :n], ps[:, :n], AF.Exp, bias=cb[:, h:h + 1])
                    for j, a in enumerate(grp):
                        p.mm(po[:, 0:65], pt[:, j * 128:(j + 1) * 128], va[:, a, h, :], start=(a == 0), stop=(a == b))
                ri = (b * 8 + h) % 16
                p.recip(rec[:, ri:ri + 1], po[:, 64:65])
                p.act(ya[b % 2][:, h * 64:(h + 1) * 64], po[:, 0:64], AF.Copy, scale=rec[:, ri:ri + 1])
            pst = c.psb[6 + b % 2]
            pstn = c.ps[6 + b % 2]
            yab = ya[b % 2]
            for cc in range(4):
                p.op("pe", lambda e, cc=cc: e.transpose(pst[:, cc * 128:(cc + 1) * 128], yab[:, cc * 128:(cc + 1) * 128], c.ident_b[:, :]),
                     [p.key(yab), p.key(c.ident_b)], [p.key(pstn)], inc=(cc == 3))
            yst = yaT[b % 2]
            p.copy("dve", yst[:, :], pst[:, 0:512])
            p.dma("sp", c.yT_d[0:4, :, b * 128:(b + 1) * 128].rearrange("c p t -> p c t"),
                  yst[:, :].rearrange("p (a b) -> p a b", a=4))


def out_proj_residual(p, c, w_l, src):
    kch = w_l.shape[2]
    with p.phase():
        c.wb = {"wb": [p.sb("wbo%d" % i, [128, kch, 128], BF16) for i in range(2)]}

        def cons(b, tb, ps):
            p.tt("dve", c.xT[:, b, tb * 512:(tb + 1) * 512], ps[:, :], c.xT[:, b, tb * 512:(tb + 1) * 512], ALU.add)
        proj_stream(p, c, "fm", src, w_l, cons, kch=kch)


def conv_ffn(p, c, W, l):
    w_up = W["w_up%d" % l]
    w_dn = W["w_dn%d" % l]
    with p.phase():
        cw = p.sb("cw", [128, 44, 3], F32)
        cbs = p.sb("cbs", [128, 44], F32)
        p.dma("sp", cw[:, :, :], W["ffn_cw%d" % l])
        p.dma("sp", cbs[:, :], W["ffn_cb%d" % l])
        halo = p.sb("halo", [128, 44, 2], F32)
        for half in range(2):
            t0 = half * 1024
            with p.phase():
                hT = p.sb("hTf", [128, 8, 1024], BF16)
                gT = p.sb("gT", [128, NJ, 1024], BF16)
                with p.phase():
                    c.sq = [p.sb("fsq%d" % i, [128, 8, 512], BF16) for i in range(2)]
                    c.rs = [p.sb("frs%d" % i, [128, 512], F32) for i in range(2)]
                    rmsnorm_fm(p, c, c.g_ffn[l], hT, t0, 1024)
                wu = [[p.sb("wu%d_%d" % (sd, i), [128, 8, 128], BF16) for i in range(2)] for sd in range(2)]
                ya = [p.sb("fya%d" % i, [128, 512], F32) for i in range(3)]
                yb = [p.sb("fyb%d" % i, [128, 512], F32) for i in range(3)]
                sa = [p.sb("fsa%d" % i, [128, 512], F32) for i in range(2)]
                hl = [p.sb("fhl%d" % i, [128, 2], F32) for i in range(4)]
                hli = 0

                def load(j):
                    p.dma("pool", wu[0][j % 2][:, :, :], w_up[j])
                    p.dma("pool", wu[1][j % 2][:, :, :], w_up[NJ + j])

                load(0)
                it = 0
                for j in range(NJ):
                    if j + 1 < NJ:
                        load(j + 1)
                    for tb in range(2):
                        ys = []
                        for sd in range(2):
                            m = sd * NJ + j
                            ps = c.ps[c.psi % 8]
                            c.psi += 1
                            wt = wu[sd][j % 2]
                            for k in range(8):
                                p.mm(ps[:, :], wt[:, k, :], hT[:, k, tb * 512:(tb + 1) * 512], start=(k == 0), stop=(k == 7))
                            y = (ya if sd == 0 else yb)[it % 3]
                            p.act(y[:, :], ps[:, :], AF.Identity, bias=cbs[:, m:m + 1], scale=cw[:, m, 2:3])
                            p.stt(y[:, 1:512], ps[:, 0:511], cw[:, m, 1:2], y[:, 1:512], ALU.mult, ALU.add)
                            p.stt(y[:, 2:512], ps[:, 0:510], cw[:, m, 0:1], y[:, 2:512], ALU.mult, ALU.add)
                            first = (half == 0 and tb == 0)
                            if not first:
                                if tb == 0:
                                    hsrc = halo[:, m, :]
                                else:
                                    hsrc = hprev[sd][:, :]
                                p.stt(y[:, 0:1], hsrc[:, 1:2], cw[:, m, 1:2], y[:, 0:1], ALU.mult, ALU.add)
                                p.stt(y[:, 0:2], hsrc[:, 0:2], cw[:, m, 0:1], y[:, 0:2], ALU.mult, ALU.add)
                            if tb == 0:
                                if sd == 0:
                                    hprev = [None, None]
                                hprev[sd] = hl[hli % 4]
                                hli += 1
                                p.copy("act", hprev[sd][:, :], ps[:, 510:512])
                            elif half == 0:
                                p.copy("act", halo[:, m, :], ps[:, 510:512])
                            ys.append(y)
                        sg = sa[it % 2]
                        p.act(sg[:, :], ys[0][:, :], AF.Silu)
                        p.tt("pool", gT[:, j, tb * 512:(tb + 1) * 512], sg[:, :], ys[1][:, :], ALU.mult)
                        it += 1
                wd = [p.sb("wd%d" % i, [128, NJ, 128], BF16) for i in range(2)]
                p.dma("pool", wd[0][:, :, :], w_dn[0])
                for dc in range(8):
                    if dc + 1 < 8:
                        p.dma("pool", wd[(dc + 1) % 2][:, :, :], w_dn[dc + 1])
                    for tb in range(2):
                        ps = c.ps[c.psi % 8]
                        c.psi += 1
                        for j in range(NJ):
                            p.mm(ps[:, :], wd[dc % 2][:, j, :], gT[:, j, tb * 512:(tb + 1) * 512], start=(j == 0), stop=(j == NJ - 1))
                        sl = c.xT[:, dc, t0 + tb * 512:t0 + (tb + 1) * 512]
                        p.tt("dve", sl, ps[:, :], sl, ALU.add)


def hgrn_mixer(p, c, W):
    with p.phase():
        hT = p.sb("hT1", [128, 8, T], BF16)
        c.wb = {"wb": [p.sb("wbh%d" % i, [128, 8, 512], BF16) for i in range(2)]}
        lbr = p.sb("lbr", [128, 2, 8], F32)
        p.dma("sp", lbr[:, :, :], W["hgrn_lb"])
        lb = p.sb("lb", [128, 8], F32)
        oml = p.sb("oml", [128, 8], F32)
        p.tt("dve", lb[:, :], lbr[:, 1, :], lbr[:, 0, :], ALU.subtract)
        p.act(lb[:, :], lb[:, :], AF.Sigmoid)
        p.ts("dve", oml[:, :], lb[:, :], -1.0, 1.0, ALU.mult, ALU.add)
        with p.phase():
            c.sq = [p.sb("hsq%d" % i, [128, 8, 512], BF16) for i in range(2)]
            c.rs = [p.sb("hrs%d" % i, [128, 512], F32) for i in range(2)]
            rmsnorm_fm(p, c, c.g_mix[1], hT, 0, T)
        stg = [p.sb("hstg%d" % i, [128, T], BF16) for i in range(2)]

        def cons_silu(dst):
            def f(b, tb, ps):
                st = stg[b % 2]
                p.act(st[:, tb * 512:(tb + 1) * 512], ps[:, :], AF.Silu)
                if tb == 3:
                    p.dma("sp", dst[b], st[:, :])
            return f
        proj_stream(p, c, "fm", hT, W["w_cq"], cons_silu(c.qh_d))
        proj_stream(p, c, "fm", hT, W["w_cg"], cons_silu(c.gate_d))

        gst = [p.sb("gst%d" % i, [128, T], F32) for i in range(2)]
        sgt = [p.sb("sgt%d" % i, [128, 512], F32) for i in range(2)]

        def cons_f(b, tb, ps):
            g = gst[b % 2]
            sg = sgt[tb % 2]
            p.act(sg[:, :], ps[:, :], AF.Sigmoid)
            p.ts("dve", g[:, tb * 512:(tb + 1) * 512], sg[:, :], oml[:, b:b + 1], lb[:, b:b + 1], ALU.mult, ALU.add)
            if tb == 3:
                st = stg[b % 2]
                p.ts("dve", st[:, :], g[:, :], -1.0, 1.0, ALU.mult, ALU.add)
                p.dma("sp", c.kk_d[b], st[:, :])
                p.act(g[:, :], g[:, :], AF.Ln)
                p.dma("sp", c.lg_d[b], g[:, :])
        proj_stream(p, c, "fm", hT, W["w_cf"], cons_f)

        ist = [p.sb("ist%d" % i, [128, 512], BF16) for i in range(2)]

        def cons_i(b, tt, ps):
            st = ist[tt % 2]
            p.copy("act" if tt % 2 == 0 else "dve", st[:, :], ps[:, :])
            p.dma("sp", c.i_d[tt * 128:(tt + 1) * 128, b * 512:(b + 1) * 512], st[:, :])
        proj_stream(p, c, "tm", hT, W["w_ci"], cons_i)
    if c.stop_after == "hproj":
        return
    with p.phase():
        oT_all = p.sb("oT_all", [128, 8, T], BF16)
        with p.phase():
            cm = p.sb("cm", [128, 32, 64], F32)
            p.memset("pool", cm[:, :, :], 1.0)
            p.memset("pool", cm[:, :, 0:1], 0.0)
            tri64 = p.sb("tri64", [64, 64], F32)
            p.dma("sp", tri64[:, :], W["tri64"])
            cn = p.sb("cn", [128, 1], F32)
            p.dma("sp", cn[:, :], W["c_out_norm"])
            lg = p.sb("lg", [128, T], F32)
            bb = p.sb("bb", [128, T], F32)
            eb = p.sb("eb", [128, T], F32)
            t1 = p.sb("t1", [128, T], F32)
            qh = p.sb("qh", [128, T], BF16)
            kk = p.sb("kk", [128, T], BF16)
            Qb = p.sb("Qb", [128, T], BF16)
            Kbb = p.sb("Kbb", [128, T], BF16)
            KlT = p.sb("KlT", [128, T], BF16)
            Klc = p.sb("Klc", [64, 32, 128], BF16)
            ic = p.sb("ic", [64, 32, 128], BF16)
            oT = p.sb("oT", [128, T], F32)
            gt = p.sb("gt", [128, T], BF16)
            Sf = p.sb("Sf", [128, 128], F32)
            Sb = [p.sb("Sb%d" % i, [128, 128], BF16) for i in range(2)]
            ATs = [p.sb("ATs%d" % i, [64, 64], BF16) for i in range(3)]
            osq = [p.sb("osq%d" % i, [128, 512], BF16) for i in range(2)]
            ors = [p.sb("ors%d" % i, [128, 512], F32) for i in range(2)]
            on = [p.sb("on%d" % i, [128, 512], F32) for i in range(2)]
            for h in range(8):
                p.dma("sp", lg[:, :], c.lg_d[h])
                p.dma("sp", qh[:, :], c.qh_d[h])
                p.dma("sp", kk[:, :], c.kk_d[h])
                p.dma("sp", gt[:, :], c.gate_d[h])
                p.dma("sp", ic[:, :, :], c.i_d[:, h * 128:(h + 1) * 128].rearrange("(n s) e -> s n e", s=64))
                p.scan(bb[:, :], cm[:, :, :].rearrange("p a b -> p (a b)"), lg[:, :], 0.0, ALU.mult, ALU.add)
                p.act(eb[:, :], bb[:, :], AF.Exp)
                p.act(t1[:, :], bb[:, :], AF.Exp, scale=-1.0)
                p.tt("pool", Qb[:, :], qh[:, :], eb[:, :], ALU.mult)
                p.tt("dve", t1[:, :], kk[:, :], t1[:, :], ALU.mult)
                p.copy("pool", Kbb[:, :], t1[:, :])
                ebv = eb[:, :].rearrange("p (a b) -> p a b", b=64)
                p.tt("dve", KlT[:, :].rearrange("p (a b) -> p a b", b=64), t1[:, :].rearrange("p (a b) -> p a b", b=64),
                     ebv[:, :, 63:64].to_broadcast([128, 32, 64]), ALU.mult)
                for g8 in range(4):
                    pst = c.psb[c.psi % 8]
                    pstn = c.ps[c.psi % 8]
                    c.psi += 1
                    for j in range(8):
                        n = g8 * 8 + j
                        p.op("pe", lambda e, j=j, n=n: e.transpose(pst[0:64, j * 128:(j + 1) * 128], KlT[:, n * 64:(n + 1) * 64], c.ident_b[:, :]),
                             [p.key(KlT), p.key(c.ident_b)], [p.key(pstn)], inc=(j == 7))
                    p.op("act", lambda e, g8=g8: e.activation(out=Klc[:, g8 * 8:(g8 + 1) * 8, :],
                                                              in_=pst[0:64, :].rearrange("p (a b) -> p a b", a=8), func=AF.Copy),
                         [p.key(pstn)], [p.key(Klc)])
                po = None
                for n in range(32):
                    cs = slice(n * 64, (n + 1) * 64)
                    if n < 31:
                        psS = c.ps[4 + n % 3]
                        p.mm(psS[:, 0:128], Klc[:, n, :], ic[:, n, :])
                    psA = c.ps[n % 2]
                    p.mm(psA[0:64, 0:64], Kbb[:, cs], Qb[:, cs])
                    at = ATs[n % 3]
                    p.tt("dve", at[:, :], psA[0:64, 0:64], tri64[:, :], ALU.mult)
                    if n % 8 == 0:
                        po = c.ps[2 + (n // 8) % 2]
                    oc = po[:, (n % 8) * 64:(n % 8 + 1) * 64]
                    p.mm(oc, ic[:, n, :], at[:, :], start=True, stop=(n == 0), inc=(n == 0))
                    if n > 0:
                        p.mm(oc, Sb[(n - 1) % 2][:, :], Qb[:, cs], start=False, stop=True, inc=True)
                    if n % 8 == 7:
                        p.copy("act", oT[:, (n // 8) * 512:(n // 8 + 1) * 512], po[:, :])
                    if n < 31:
                        if n == 0:
                            p.copy("dve", Sf[:, :], psS[:, 0:128])
                        else:
                            p.stt(Sf[:, :], Sf[:, :], eb[:, n * 64 + 63:n * 64 + 64], psS[:, 0:128], ALU.mult, ALU.add)
                        p.copy("act", Sb[n % 2][:, :], Sf[:, :])
                for tb in range(4):
                    ts_ = slice(tb * 512, (tb + 1) * 512)
                    sq = osq[tb % 2]
                    p.act(sq[:, :], oT[:, ts_], AF.Square)
                    ps = c.ps[6 + tb % 2]
                    p.mm(ps[:, :], c.ones_b[:, :], sq[:, :])
                    rs = ors[tb % 2]
                    p.act(rs[:, :], ps[:, :], AF.Sqrt, bias=c.eps_ap[:, :], scale=1.0 / 128)
                    p.recip(rs[:, :], rs[:, :])
                    o2 = on[tb % 2]
                    p.stt(o2[:, :], oT[:, ts_], cn[:, 0:1], rs[:, :], ALU.mult, ALU.mult)
                    p.tt("pool", oT_all[:, h, ts_], o2[:, :], gt[:, ts_], ALU.mult)
        if c.stop_after == "hrec":
            if "oT_dbg" in c.dbg:
                od = c.dscr("oT_dbg", [8, 128, T], BF16)
                for k in range(8):
                    p.dma("sp", od[k], oT_all[:, k, :])
            return
        out_proj_residual(p, c, W["w_out1"], oT_all)


def final_norm(p, c, out):
    with p.phase():
        c.sq = [p.sb("nsq%d" % i, [128, 8, 512], BF16) for i in range(2)]
        c.rs = [p.sb("nrs%d" % i, [128, 512], F32) for i in range(2)]
        ot = [p.sb("fot%d" % i, [128, 8, 512], F32) for i in range(2)]
        for i, tb in enumerate(range(0, T, 512)):
            sq = c.sq[i % 2]
            p.act(sq[:, :, :], c.xT[:, :, tb:tb + 512], AF.Square)
            ps = c.ps[c.psi % 8]
            c.psi += 1
            for k in range(8):
                p.mm(ps[:, :], c.ones_b[:, :], sq[:, k, :], start=(k == 0), stop=(k == 7))
            rs = c.rs[i % 2]
            p.act(rs[:, :], ps[:, :], AF.Sqrt, bias=c.eps_ap[:, :], scale=1.0 / D)
            p.recip(rs[:, :], rs[:, :])
            o = ot[i % 2]
            for k in range(8):
                p.stt(o[:, k, :], c.xT[:, k, tb:tb + 512], c.g_fin[:, k:k + 1], rs[:, :], ALU.mult, ALU.mult)
            p.dma("sp", out[:, :, tb:tb + 512].rearrange("c p t -> p c t"), o[:, :, :])

def lay(Wm, cw):
    K, N = Wm.shape
    return np.ascontiguousarray(Wm.reshape(K // 128, 128, N // cw, cw).transpose(2, 1, 0, 3))


def build(stop_after=None, dbg=()):
    nc = bass.Bass("TRN2", target_bir_lowering=False)
    p = Prog(nc)
    c = Ctx()
    W = {}

    def din(name, shape, dt=F32):
        W[name] = nc.dram_tensor(name, list(shape), dt, kind="ExternalInput").ap()
        return W[name]

    def dscr(name, shape, dt):
        kind = "ExternalOutput" if name in dbg else "Internal"
        return nc.dram_tensor(name, list(shape), dt, kind=kind).ap()

    din("xT_in", [8, 128, T])
    din("ident", [128, 128])
    din("g_mix", [2, 128, 8]); din("g_ffn", [2, 128, 8]); din("g_fin", [128, 8])
    din("w_q", [4, 128, 8, 128]); din("w_k", [4, 128, 8, 128]); din("w_iq", [8, 128, 8, 128])
    din("w_ik2", [1, 128, 8, 128]); din("w_v", [1, 128, 8, 512]); din("w_iw", [1, 128, 8, 16])
    din("w_u", [1, 128, 8, 512]); din("w_vg", [1, 128, 8, 512])
    din("gmlp_norm", [1, 512]); din("w_sT", [128, 8, 128]); din("b_s", [8, 128]); din("e8", [8, 512])
    din("gk2", [128, 1]); din("pow2", [128, NBIS]); din("oh1", [32, 383]); din("rel_bias", [32, 8])
    din("w_out0", [8, 128, 8, 128])
    for l in range(2):
        din("w_up%d" % l, [44, 128, 8, 128]); din("w_dn%d" % l, [8, 128, NJ, 128])
        din("ffn_cw%d" % l, [128, 44, 3]); din("ffn_cb%d" % l, [128, 44])
    din("w_cq", [8, 128, 8, 128]); din("w_cf", [8, 128, 8, 128]); din("w_cg", [8, 128, 8, 128]); din("w_ci", [2, 128, 8, 512])
    din("hgrn_lb", [128, 2, 8]); din("tri64", [64, 64]); din("c_out_norm", [128, 1]); din("w_out1", [8, 128, 8, 128])
    out = nc.dram_tensor("outT", [8, 128, T], F32, kind="ExternalOutput").ap()

    c.qT_d = dscr("qT_d", [4, 128, T], BF16)
    c.kT_d = dscr("kT_d", [4, 128, T], BF16)
    c.iqT_d = dscr("iqT_d", [8, 128, T], BF16)
    c.v_d = dscr("v_d", [T, 512], BF16)
    c.yT_d = dscr("yT_d", [8, 128, T], BF16)
    c.qh_d = dscr("qh_d", [8, 128, T], BF16)
    c.gate_d = dscr("gate_d", [8, 128, T], BF16)
    c.kk_d = dscr("kk_d", [8, 128, T], BF16)
    c.lg_d = dscr("lg_d", [8, 128, T], F32)
    c.i_d = dscr("i_d", [T, 1024], BF16)

    c.xT = p.sb("xT", [128, 8, T], F32, True)
    c.ident_f = p.sb("ident_f", [128, 128], F32, True)
    c.ident_b = p.sb("ident_b", [128, 128], BF16, True)
    c.ones_b = p.sb("ones_b", [128, 128], BF16, True)
    c.eps_ap = p.sb("eps_ap", [128, 1], F32, True)
    gm = p.sb("g_mix_sb", [128, 2, 8], F32, True)
    gf = p.sb("g_ffn_sb", [128, 2, 8], F32, True)
    gfin = p.sb("g_fin_sb", [128, 8], F32, True)
    c.ps = [p.es.enter_context(nc.psum_tensor("ps%d" % i, [128, 512], F32)) for i in range(8)]
    c.psb = [t[:, :].bitcast(BF16) for t in c.ps]
    c.psi = 0
    c.stop_after = stop_after
    c.dbg = dbg
    c.dscr = dscr

    for k in range(8):
        p.dma("sp", c.xT[:, k, :], W["xT_in"][k])
    p.dma("sp", c.ident_f[:, :], W["ident"])
    p.dma("pool", c.ident_b[:, :], W["ident"])
    p.memset("dve", c.ones_b[:, :], 1.0)
    p.memset("dve", c.eps_ap[:, :], EPS)
    for l in range(2):
        p.dma("sp", gm[:, l, :], W["g_mix"][l])
        p.dma("sp", gf[:, l, :], W["g_ffn"][l])
    p.dma("sp", gfin[:, :], W["g_fin"])
    c.g_mix = [gm[:, l, :] for l in range(2)]
    c.g_ffn = [gf[:, l, :] for l in range(2)]
    c.g_fin = gfin

    layer0_mixer(p, c, W)
    stages = ["gmlp", "indexer", "attn", "mix0", "ffn0", "hproj", "hrec", "mix1", "ffn1", None]
    si_ = stages.index(stop_after)
    if si_ >= stages.index("ffn0"):
        conv_ffn(p, c, W, 0)
    if si_ >= stages.index("hproj"):
        hgrn_mixer(p, c, W)
    if si_ >= stages.index("ffn1"):
        conv_ffn(p, c, W, 1)
    if stop_after is None:
        final_norm(p, c, out)
        p.barrier()
        return nc, p, c

    for k in range(8):
        p.dma("sp", out[k], c.xT[:, k, :])
    p.barrier()
    return nc, p, c


def t5_onehot():
    rel = np.arange(-255, 128)
    half, max_exact = 16, 8
    ret = np.where(rel > 0, half, 0)
    n = np.abs(rel)
    nf = np.maximum(n, max_exact).astype(np.float32)
    large = max_exact + (np.log(nf / np.float32(max_exact)) / np.float32(np.log(128 / 8)) * np.float32(half - max_exact)).astype(np.int32)
    large = np.minimum(large, half - 1)
    bucket = ret + np.where(n < max_exact, n, large)
    oh = np.zeros((32, 383), np.float32)
    oh[bucket, np.arange(383)] = 1.0
    return oh


def host_inputs(inp, b):
    f = np.float32
    m = {}
    x = inp["x"][b]
    m["xT_in"] = np.ascontiguousarray(x.T.reshape(8, 128, T))
    m["ident"] = np.eye(128, dtype=f)
    m["g_mix"] = np.ascontiguousarray(inp["mix_norm"].reshape(2, 8, 128).transpose(0, 2, 1))
    m["g_ffn"] = np.ascontiguousarray(inp["ffn_norm"].reshape(2, 8, 128).transpose(0, 2, 1))
    m["g_fin"] = np.ascontiguousarray(inp["final_norm"].reshape(8, 128).T)
    Wi = inp["ab_w_in"][0]
    m["w_q"] = lay(Wi[:, 0:512], 128)
    m["w_k"] = lay(Wi[:, 512:1024], 128)
    m["w_v"] = lay(Wi[:, 1024:1536], 512)
    m["w_iq"] = lay(Wi[:, 1536:2560], 128)
    m["w_ik2"] = lay(np.concatenate([Wi[:, 2560:2624], Wi[:, 2560:2624]], axis=1), 128)
    m["w_iw"] = lay(Wi[:, 2624:2640], 16)
    m["w_u"] = lay(Wi[:, 2640:3152], 512)
    m["w_vg"] = lay(Wi[:, 3152:3664], 512)
    m["gmlp_norm"] = np.ascontiguousarray(inp["ab_gmlp_norm"][0].reshape(1, 512))
    m["w_sT"] = np.ascontiguousarray(inp["ab_w_s"][0].transpose(2, 0, 1))
    m["b_s"] = np.ascontiguousarray(inp["ab_b_s"][0])
    e8 = np.zeros((8, 512), f)
    for g in range(8):
        e8[g, g * 64:(g + 1) * 64] = 1.0
    m["e8"] = e8
    m["gk2"] = np.concatenate([inp["ab_idx_k_norm"][0], inp["ab_idx_k_norm"][0]]).reshape(128, 1)
    m["pow2"] = np.tile((0.5 ** np.arange(1, NBIS + 1)).astype(f)[None, :], (128, 1))
    m["oh1"] = t5_onehot()
    m["rel_bias"] = inp["rel_bias"]
    m["w_out0"] = lay(inp["ab_w_out"][0], 128)
    Wc = inp["c_w_in"][0]
    m["w_cq"] = lay(Wc[:, 0:1024], 128)
    m["w_cf"] = lay(Wc[:, 1024:2048], 128)
    m["w_ci"] = lay(Wc[:, 2048:3072], 512)
    m["w_cg"] = lay(Wc[:, 3072:4096], 128)
    m["hgrn_lb"] = inp["hgrn_lb"].reshape(2, 8, 128).transpose(2, 0, 1)
    m["tri64"] = np.triu(np.ones((64, 64), f))
    m["c_out_norm"] = inp["c_out_norm"][0].reshape(128, 1)
    m["w_out1"] = lay(inp["c_w_out"][0], 128)
    for l in range(2):
        m["w_up%d" % l] = lay(inp["ffn_w_up"][l], 128)
        m["w_dn%d" % l] = lay(inp["ffn_w_down"][l], 128)
        m["ffn_cw%d" % l] = inp["ffn_conv_w"][l].reshape(3, 44, 128).transpose(2, 1, 0)
        m["ffn_cb%d" % l] = inp["ffn_conv_b"][l].reshape(44, 128).T
    return {k: np.ascontiguousarray(v, dtype=f) for k, v in m.items()}


def kernel(**inputs):
    inp = {k: np.asarray(v) for k, v in inputs.items()}
    nc, p, c = build()
    in_maps = [host_inputs(inp, b) for b in range(8)]
    res = run_bass_kernel_spmd(nc, in_maps, core_ids=list(range(8)))
    outs = [np.asarray(r["outT"]).reshape(D, T).T for r in res.results]
    return np.stack(outs, axis=0).astype(np.float32)
```

```python
import numpy as np
from contextlib import ExitStack, contextmanager
import concourse.bass as bass
import concourse.mybir as mybir
from concourse.bass_utils import run_bass_kernel_spmd

F32 = mybir.dt.float32
BF16 = mybir.dt.bfloat16
ALU = mybir.AluOpType
AF = mybir.ActivationFunctionType
AX = mybir.AxisListType

T = 2048
D = 1024
NT = 16
EPS = 1e-6
D_FF = 2816
NJ = 22
TOPK = 256
NBIS = 16
NEG = -30000.0


class Tok:
    __slots__ = ("sem", "key", "val", "clock")

    def __init__(self, sem, key, val, clock):
        self.sem, self.key, self.val, self.clock = sem, key, val, clock


class Reg:
    __slots__ = ("w", "r")

    def __init__(self):
        self.w = None
        self.r = {}


class Prog:
    def __init__(self, nc):
        self.nc = nc
        self.es = ExitStack()
        self.E = {"pe": nc.tensor, "act": nc.scalar, "dve": nc.vector, "pool": nc.gpsimd, "sp": nc.sync}
        self.csem = {e: self.es.enter_context(nc.semaphore("cs_" + e)) for e in ("pe", "act", "dve", "pool")}
        self.ccnt = {e: 0 for e in self.csem}
        self.known = {e: {} for e in self.E}
        self.dq = {}
        for q, n in (("sp", 24), ("pool", 16)):
            self.dq[q] = dict(
                sems=[self.es.enter_context(nc.semaphore("d_%s%d" % (q, i))) for i in range(n)],
                use=[0] * n, last=[None] * n, nxt=0)
        self.regs = {}
        self.last = {e: None for e in self.csem}
        self.phase_stack = None
        self.ninst = 0
        self.nwait = 0
        self.nsb = 0

    @staticmethod
    def key(x, sub=None):
        if isinstance(x, (tuple, list)):
            return x
        if isinstance(x, str):
            return (x, None)
        name = x.tensor.name if hasattr(x, "tensor") else x.name
        if name == "psum":
            sz = mybir.dt.size(x.dtype)
            apl = list(x.ap)
            off = (x.offset % apl[0][0]) * sz
            ext = sum((cnt - 1) * abs(st) for st, cnt in apl[1:]) * sz + sz
            return [("psum", bk) for bk in range(off // 2048, (off + ext - 1) // 2048 + 1)]
        if sub and name in sub:
            return (name, sub[name])
        return (name, None)

    def _targets(self, k):
        name, s = k
        d = self.regs.setdefault(name, {})
        if s is None:
            if None not in d:
                d[None] = Reg()
            return list(d.values())
        if s not in d:
            d[s] = Reg()
        out = [d[s]]
        if None in d:
            out.append(d[None])
        return out

    def _wait(self, eng, tok):
        if tok is None:
            return
        if eng == "pe" and tok.key == "pe":
            return
        k = self.known[eng]
        if k.get(tok.key, 0) >= tok.val:
            return
        if tok.key in self.ccnt:
            assert tok.val <= self.ccnt[tok.key], ("wait on future inc", eng, tok.key, tok.val)
        self.E[eng].wait_ge(tok.sem, tok.val)
        self.nwait += 1
        for kk, vv in tok.clock.items():
            if k.get(kk, 0) < vv:
                k[kk] = vv

    def _sync(self, eng, reads, writes):
        for k in reads:
            for rg in self._targets(k):
                self._wait(eng, rg.w)
        for k in writes:
            for rg in self._targets(k):
                self._wait(eng, rg.w)
                for t in list(rg.r.values()):
                    self._wait(eng, t)

    def _record(self, tok, reads, writes):
        for k in reads:
            name, s = k
            rg = self.regs[name][s]
            old = rg.r.get(tok.key)
            if old is None or old.val < tok.val:
                rg.r[tok.key] = tok
        for k in writes:
            name, s = k
            rg = self.regs[name][s]
            rg.w = tok
            rg.r = {}

    def _flat(self, ks):
        out = []
        for k in ks:
            k = self.key(k)
            if isinstance(k, list):
                out += k
            else:
                out.append(k)
        return out

    def op(self, eng, fn, reads, writes, inc=True):
        reads = self._flat(reads)
        writes = self._flat(writes)
        self._sync(eng, reads, writes)
        inst = fn(self.E[eng])
        self.ninst += 1
        if inc:
            self.ccnt[eng] += 1
            inst.then_inc(self.csem[eng], 1)
            val = self.ccnt[eng]
        else:
            val = self.ccnt[eng] + 1
        clock = dict(self.known[eng])
        clock[eng] = max(clock.get(eng, 0), val)
        tok = Tok(self.csem[eng], eng, val, clock)
        self.last[eng] = tok
        self._record(tok, reads, writes)
        return tok

    def dma(self, q, out, in_, sub=None):
        reads = self._flat([self.key(in_, sub)])
        writes = self._flat([self.key(out, sub)])
        self._sync(q, reads, writes)
        d = self.dq[q]
        j = d["nxt"]
        d["nxt"] = (j + 1) % len(d["sems"])
        self._wait(q, d["last"][j])
        inst = self.E[q].dma_start(out=out, in_=in_)
        self.ninst += 1
        d["use"][j] += 1
        inst.then_inc(d["sems"][j], 16)
        key = ("d", q, j)
        val = 16 * d["use"][j]
        clock = dict(self.known[q])
        clock[key] = val
        tok = Tok(d["sems"][j], key, val, clock)
        d["last"][j] = tok
        self._record(tok, reads, writes)
        return tok

    def barrier(self):
        toks = [t for t in self.last.values() if t is not None]
        for d in self.dq.values():
            toks += [t for t in d["last"] if t is not None]
        for e in self.E:
            for t in toks:
                self._wait(e, t)

    def sb(self, name, shape, dtype, persistent=False):
        st = self.es if (persistent or self.phase_stack is None) else self.phase_stack
        self.nsb += 1
        return st.enter_context(self.nc.sbuf_tensor("s%d_%s" % (self.nsb, name), list(shape), dtype))

    @contextmanager
    def phase(self):
        prev = self.phase_stack
        self.phase_stack = ExitStack()
        try:
            yield
            self.barrier()
        finally:
            self.phase_stack.close()
            self.phase_stack = prev

    def _aps(self, sub, *xs):
        return [self.key(x, sub) for x in xs if x is not None and not isinstance(x, (int, float))]

    def mm(self, out, lhsT, rhs, start=True, stop=True, inc=None, sub=None):
        if inc is None:
            inc = stop
        return self.op("pe", lambda e: e.matmul(out, lhsT, rhs, start=start, stop=stop),
                       self._aps(sub, lhsT, rhs), self._aps(sub, out), inc=inc)

    def tr(self, out, in_, ident, inc=True, sub=None):
        return self.op("pe", lambda e: e.transpose(out, in_, ident),
                       self._aps(sub, in_, ident), self._aps(sub, out), inc=inc)

    def act(self, out, in_, func, bias=None, scale=None, accum_out=None, sub=None):
        kw = {}
        if bias is not None:
            kw["bias"] = bias
        if scale is not None:
            kw["scale"] = scale
        if accum_out is not None:
            kw["accum_out"] = accum_out
        return self.op("act", lambda e: e.activation(out=out, in_=in_, func=func, **kw),
                       self._aps(sub, in_, bias, scale), self._aps(sub, out, accum_out))

    def ts(self, eng, out, in0, s1, s2, op0, op1=None, accum_out=None, sub=None):
        kw = {}
        if op1 is not None:
            kw["op1"] = op1
        if accum_out is not None:
            kw["accum_out"] = accum_out
        return self.op(eng, lambda e: e.tensor_scalar(out=out, in0=in0, scalar1=s1, scalar2=s2, op0=op0, **kw),
                       self._aps(sub, in0, s1, s2), self._aps(sub, out, accum_out))

    def stt(self, out, in0, scalar, in1, op0, op1, sub=None):
        return self.op("dve", lambda e: e.scalar_tensor_tensor(out=out, in0=in0, scalar=scalar, in1=in1, op0=op0, op1=op1),
                       self._aps(sub, in0, scalar, in1), self._aps(sub, out))

    def tt(self, eng, out, in0, in1, op, sub=None):
        return self.op(eng, lambda e: e.tensor_tensor(out=out, in0=in0, in1=in1, op=op),
                       self._aps(sub, in0, in1), self._aps(sub, out))

    def copy(self, eng, out, in_, sub=None):
        if eng == "act":
            return self.act(out, in_, AF.Copy, sub=sub)
        return self.op(eng, lambda e: e.tensor_copy(out=out, in_=in_), self._aps(sub, in_), self._aps(sub, out))

    def memset(self, eng, ap, val, sub=None):
        return self.op(eng, lambda e: e.memset(ap, val), [], self._aps(sub, ap))

    def recip(self, out, in_, sub=None):
        return self.op("dve", lambda e: e.reciprocal(out=out, in_=in_), self._aps(sub, in_), self._aps(sub, out))

    def reduce(self, out, in_, op, sub=None):
        return self.op("dve", lambda e: e.tensor_reduce(out=out, in_=in_, axis=AX.X, op=op),
                       self._aps(sub, in_), self._aps(sub, out))

    def scan(self, out, d0, d1, initial, op0, op1, sub=None):
        return self.op("dve", lambda e: e.tensor_tensor_scan(out=out, data0=d0, data1=d1, initial=initial, op0=op0, op1=op1),
                       self._aps(sub, d0, d1), self._aps(sub, out))


class Ctx:
    pass


def rmsnorm_fm(p, c, g_ap, hT, t0, nt, nparts=128, kch=8, src=None, scale_div=None):
    src = c.xT if src is None else src
    div = float(scale_div if scale_div is not None else nparts * kch)
    for i, tb in enumerate(range(t0, t0 + nt, 512)):
        sq = c.sq[i % 2]
        p.act(sq[:nparts, :kch, :], src[:nparts, :kch, tb:tb + 512], AF.Square)
        ps = c.ps[c.psi % 8]
        c.psi += 1
        for k in range(kch):
            p.mm(ps[:nparts, :], c.ones_b[:nparts, :nparts], sq[:nparts, k, :], start=(k == 0), stop=(k == kch - 1))
        rs = c.rs[i % 2]
        p.act(rs[:nparts, :], ps[:nparts, :], AF.Sqrt, bias=c.eps_ap[:nparts, :], scale=1.0 / div)
        p.recip(rs[:nparts, :], rs[:nparts, :])
        for k in range(kch):
            p.stt(hT[:nparts, k, tb - t0:tb - t0 + 512], src[:nparts, k, tb:tb + 512], g_ap[:nparts, k:k + 1],
                  rs[:nparts, :], ALU.mult, ALU.mult)


def proj_stream(p, c, mode, hT, w_l, consume, kch=8, tsl=None, wname="wb"):
    nblk, _, _, cw = w_l.shape
    ntok = hT.shape[2] if tsl is None else tsl
    wbs = c.wb[wname]

    def load(b):
        p.dma("pool", wbs[b % 2][:, :kch, :cw], w_l[b])

    load(0)
    for b in range(nblk):
        if b + 1 < nblk:
            load(b + 1)
        wb = wbs[b % 2]
        if mode == "fm":
            for tb in range(ntok // 512):
                ps = c.ps[c.psi % 8]
                c.psi += 1
                for k in range(kch):
                    p.mm(ps[:cw, :], wb[:, k, :cw], hT[:, k, tb * 512:(tb + 1) * 512], start=(k == 0), stop=(k == kch - 1))
                consume(b, tb, ps)
        else:
            for tt in range(ntok // 128):
                ps = c.ps[c.psi % 8]
                c.psi += 1
                for k in range(kch):
                    p.mm(ps[:, :cw], hT[:, k, tt * 128:(tt + 1) * 128], wb[:, k, :cw], start=(k == 0), stop=(k == kch - 1))
                consume(b, tt, ps)


def layer0_mixer(p, c, W):
    with p.phase():
        c.nmT = p.sb("nmT", [128, 136, 128], BF16)
        with p.phase():
            c.ik2 = p.sb("ik2", [128, 1, T], F32)
            c.iw_sb = p.sb("iw_sb", [128, NT, 16], F32)
            with p.phase():
                hT = p.sb("hT", [128, 8, T], BF16)
                c.wb = {"wb": [p.sb("wb%d" % i, [128, 8, 512], BF16) for i in range(2)]}
                with p.phase():
                    c.sq = [p.sb("sq%d" % i, [128, 8, 512], BF16) for i in range(2)]
                    c.rs = [p.sb("rs%d" % i, [128, 512], F32) for i in range(2)]
                    rmsnorm_fm(p, c, c.g_mix[0], hT, 0, T)
                    layer0_inproj_a(p, c, W, hT)
                layer0_gmlp(p, c, W, hT)
            if c.stop_after == "gmlp":
                return
            layer0_indexer(p, c, W)
        if c.stop_after == "indexer":
            return
        layer0_attention(p, c, W)
    if c.stop_after == "attn":
        return
    with p.phase():
        yT = p.sb("yT", [128, 8, T], BF16)
        for k in range(8):
            p.dma("sp", yT[:, k, :], c.yT_d[k])
        out_proj_residual(p, c, W["w_out0"], yT)


def layer0_inproj_a(p, c, W, hT):
    if True:
        stg = [p.sb("stg%d" % i, [128, T], BF16) for i in range(2)]
        cnt = [0]

        def cons_fm(dst, scale=None):
            def f(b, tb, ps):
                st = stg[b % 2]
                eng = "act" if (cnt[0] % 2 == 0) else "dve"
                cnt[0] += 1
                if scale is None:
                    p.copy(eng, st[:, tb * 512:(tb + 1) * 512], ps[:, :])
                elif eng == "act":
                    p.act(st[:, tb * 512:(tb + 1) * 512], ps[:, :], AF.Copy, scale=scale)
                else:
                    p.ts("dve", st[:, tb * 512:(tb + 1) * 512], ps[:, :], scale, None, ALU.mult)
                if tb == 3:
                    p.dma("sp", dst[b], st[:, :])
            return f

        proj_stream(p, c, "fm", hT, W["w_q"], cons_fm(c.qT_d, 0.125))
        proj_stream(p, c, "fm", hT, W["w_k"], cons_fm(c.kT_d))
        proj_stream(p, c, "fm", hT, W["w_iq"], cons_fm(c.iqT_d))

        def cons_ik(b, tb, ps):
            p.copy("act", c.ik2[:, 0, tb * 512:(tb + 1) * 512], ps[:, :])
        proj_stream(p, c, "fm", hT, W["w_ik2"], cons_ik)

        vst = [p.sb("vst%d" % i, [128, 512], BF16) for i in range(2)]

        def cons_v(b, tt, ps):
            st = vst[tt % 2]
            p.copy("act" if tt % 2 == 0 else "dve", st[:, :], ps[:, :])
            p.dma("sp", c.v_d[tt * 128:(tt + 1) * 128, :], st[:, :])
        proj_stream(p, c, "tm", hT, W["w_v"], cons_v)

        def cons_iw(b, tt, ps):
            p.copy("dve", c.iw_sb[:, tt, :], ps[:, 0:16])
        proj_stream(p, c, "tm", hT, W["w_iw"], cons_iw)


def layer0_gmlp(p, c, W, hT):
    if True:
        u_all = p.sb("u_all", [128, NT, 512], BF16)
        vn_all = p.sb("vn_all", [128, NT, 512], BF16)
        vgf = [p.sb("vgf%d" % i, [128, 512], F32) for i in range(2)]
        junk = p.sb("junk", [128, 512], BF16)
        ssv = p.sb("ssv", [128, NT], F32)
        rsv = p.sb("rsv", [128, NT], F32)
        gn_bc = p.sb("gn_bc", [128, 512], F32)
        p.dma("sp", gn_bc[:, :], W["gmlp_norm"].partition_broadcast(128))

        def cons_u(b, tt, ps):
            p.act(u_all[:, tt, :], ps[:, :], AF.Gelu_apprx_tanh)
        proj_stream(p, c, "tm", hT, W["w_u"], cons_u)

        def cons_vg(b, tt, ps):
            vg = vgf[tt % 2]
            p.act(vg[:, :], ps[:, :], AF.Gelu_apprx_tanh)
            p.act(junk[:, :], vg[:, :], AF.Square, accum_out=ssv[:, tt:tt + 1])
            p.act(rsv[:, tt:tt + 1], ssv[:, tt:tt + 1], AF.Sqrt, bias=c.eps_ap[:, :], scale=1.0 / 512)
            p.recip(rsv[:, tt:tt + 1], rsv[:, tt:tt + 1])
            p.stt(vn_all[:, tt, :], vg[:, :], rsv[:, tt:tt + 1], gn_bc[:, :], ALU.mult, ALU.mult)
        proj_stream(p, c, "tm", hT, W["w_vg"], cons_vg)

        wsT = p.sb("wsT", [128, 8, 128], BF16)
        p.dma("pool", wsT[:, :, :], W["w_sT"])
        p.memset("dve", wsT[64:128, :, 0:64], 0.0)
        bs8 = p.sb("bs8", [8, 128], BF16)
        p.dma("pool", bs8[:, :], W["b_s"])
        e8 = p.sb("e8", [8, 512], BF16)
        p.dma("pool", e8[:, :], W["e8"])
        ybs = [p.sb("ybs%d" % i, [128, 512], BF16) for i in range(2)]
        ybT = [p.sb("ybT%d" % i, [128, 512], BF16) for i in range(2)]
        for n in range(NT):
            ps = c.ps[c.psi % 8]
            c.psi += 1
            for g in range(8):
                p.mm(ps[:, g * 64:(g + 1) * 64], wsT[:, g, :], vn_all[:, n, g * 64:(g + 1) * 64], start=True, stop=False, inc=False)
                p.mm(ps[:, g * 64:(g + 1) * 64], bs8[:, :], e8[:, g * 64:(g + 1) * 64], start=False, stop=True, inc=(g == 7))
            yb = ybs[n % 2]
            p.tt("dve", yb[:, :], ps[:, :], u_all[:, n, :], ALU.mult)
            pst = c.psb[c.psi % 8]
            pstn = c.ps[c.psi % 8]
            c.psi += 1
            for cc in range(4):
                p.op("pe", lambda e, cc=cc: e.transpose(pst[:, cc * 128:(cc + 1) * 128], yb[:, cc * 128:(cc + 1) * 128], c.ident_b[:, :]),
                     [p.key(yb), p.key(c.ident_b)], [p.key(pstn)], inc=(cc == 3))
            yst = ybT[n % 2]
            p.copy("act", yst[:, :], pst[:, 0:512])
            p.dma("sp", c.yT_d[4:8, :, n * 128:(n + 1) * 128].rearrange("c p t -> p c t"),
                  yst[:, :].rearrange("p (a b) -> p a b", a=4))


def tri(b):
    return b * (b + 1) // 2


def layer0_indexer(p, c, W):
    with p.phase():
        ikn = p.sb("ikn", [128, 1, T], BF16)
        gk2 = p.sb("gk2", [128, 1], F32)
        p.dma("sp", gk2[:, :], W["gk2"])
        with p.phase():
            c.sq = [p.sb("isq%d" % i, [128, 1, 512], BF16) for i in range(2)]
            c.rs = [p.sb("irs%d" % i, [128, 512], F32) for i in range(2)]
            rmsnorm_fm(p, c, gk2, ikn, 0, T, nparts=128, kch=1, src=c.ik2, scale_div=128)
        wabs = p.sb("wabs", [128, NT, 16], F32)
        sgn = p.sb("sgn", [128, NT, 16], F32)
        p.ts("dve", wabs[:, :, :], c.iw_sb[:, :, :], -1.0, None, ALU.mult)
        p.tt("dve", wabs[:, :, :], wabs[:, :, :], c.iw_sb[:, :, :], ALU.max)
        p.ts("dve", sgn[:, :, :], c.iw_sb[:, :, :], 0.0, 2.0, ALU.is_ge, ALU.mult)
        p.ts("dve", sgn[:, :, :], sgn[:, :, :], -1.0, None, ALU.add)
        iqz = [p.sb("iqz%d" % i, [128, 8, 2, 128], BF16) for i in range(2)]
        for i in range(2):
            p.memset("pool", iqz[i][:, :, :, :], 0.0)
        Dsg = [p.sb("Dsg%d" % i, [128, 16, 128], BF16) for i in range(4)]
        acc = [p.sb("acc%d" % i, [128, T], F32) for i in range(4)]
        rr = [p.sb("rr%d" % i, [128, 1024], BF16) for i in range(3)]
        nm = [p.sb("nm%d" % i, [128, T], BF16) for i in range(4)]
        junk = [p.sb("junkc%d" % i, [128, T], BF16) for i in range(2)]
        pow2 = p.sb("pow2", [128, NBIS], F32)
        p.dma("sp", pow2[:, :], W["pow2"])
        st = [p.sb("bis%d" % b, [128, 8], F32) for b in range(NT)]
        wtab = [p.sb("wtab%d" % b, [128, NBIS], F32) for b in range(NT)]
        cnt_ = {"ri": 0, "ei": 0, "si": 0}

        def jobs_for(b):
            L = 128 * (b + 1)
            units = [(0, min(L, 1024))] + ([(1024, L)] if L > 1024 else [])
            return [(b, k0, k1, h) for (k0, k1) in units for h in range(16)]

        def load_iq(b):
            iz = iqz[b % 2]
            for par in range(2):
                p.dma("sp", iz[par * 64:(par + 1) * 64, :, par, :],
                      c.iqT_d[:, par * 64:(par + 1) * 64, b * 128:(b + 1) * 128].rearrange("m p t -> p m t"))

        def emit_score(job):
            b, k0, k1, h = job
            n = k1 - k0
            nb = (n + 511) // 512
            m, par = divmod(h, 2)
            slot = 1 + cnt_["si"] % 3
            cnt_["si"] += 1
            psc = c.psum[:, slot * 1024:slot * 1024 + n]
            for kb in range(nb):
                c0, c1 = kb * 512, min(n, (kb + 1) * 512)
                p.mm(psc[:, c0:c1], iqz[b % 2][:, m, par, :], ikn[:, 0, k0 + c0:k0 + c1])
            return psc

        def emit_rest(job, psc):
            b, k0, k1, h = job
            n = k1 - k0
            nb = (n + 511) // 512
            pacc = c.psum[:, 0:n]
            r = rr[cnt_["ri"] % 3]
            cnt_["ri"] += 1
            p.act(r[:, :n], psc, AF.Relu, scale=wabs[:, b, h:h + 1])
            for kb in range(nb):
                c0, c1 = kb * 512, min(n, (kb + 1) * 512)
                p.mm(pacc[:, c0:c1], Dsg[b % 4][:, h, :], r[:, c0:c1], start=(h == 0), stop=(h == 15))
            if h == 15:
                p.copy("act", acc[b % 4][:, k0:k1], pacc)

        def scores(bs):
            jobs = []
            for b in bs:
                load_iq(b)
                jobs += jobs_for(b)
            psc = emit_score(jobs[0])
            for i, job in enumerate(jobs):
                nxt = emit_score(jobs[i + 1]) if i + 1 < len(jobs) else None
                emit_rest(job, psc)
                psc = nxt

        def build_dsg(b):
            for h in range(16):
                p.ts("dve", Dsg[b % 4][:, h, :], c.ident_b[:, :], sgn[:, b, h:h + 1], None, ALU.mult)

        def bisect(bs):
            S = {}
            for i_, b in enumerate(bs):
                L = 128 * (b + 1)
                ac = acc[b % 4]
                mx, mn, w0, cand, cnt, tq = [st[b][:, i:i + 1] for i in range(6)]
                S[b] = (L, ac, cand, cnt, tq, junk[i_ % 2])
                p.reduce(mx, ac[:, :L], ALU.max)
                p.reduce(mn, ac[:, :L], ALU.min)
                p.memset("dve", ac[0:64, L - 64:L], -1e30)
                p.tt("dve", w0, mx, mn, ALU.subtract)
                p.ts("dve", wtab[b][:, :], pow2[:, :], w0, None, ALU.mult)
                p.tt("dve", cand, mn, wtab[b][:, 0:1], ALU.add)
            for i in range(NBIS):
                last = (i == NBIS - 1)
                for b in bs:
                    L, ac, cand, cnt, tq, jk = S[b]
                    p.ts("dve", jk[:, :L], ac[:, :L], cand, 0.0, ALU.is_ge, ALU.add, accum_out=cnt)
                for b in bs:
                    L, ac, cand, cnt, tq, jk = S[b]
                    p.ts("dve", tq, cnt, float(TOPK), (1.0 if last else 0.5), ALU.is_ge, ALU.subtract)
                for b in bs:
                    L, ac, cand, cnt, tq, jk = S[b]
                    p.stt(cand, tq, wtab[b][:, i:i + 1], cand, ALU.mult, ALU.add)
            for b in bs:
                L, ac, cand, cnt, tq, jk = S[b]
                p.ts("dve", nm[b % 4][:, :L], ac[:, :L], cand, NEG, ALU.is_lt, ALU.mult)

        def transposes(b):
            nmb = nm[b % 4]
            for a0 in range(0, b + 1, 4):
                g = min(4, b + 1 - a0)
                slot = 1 + cnt_["si"] % 3
                cnt_["si"] += 1
                pstn = c.ps[2 * slot]
                pst = c.psb[2 * slot]
                for j in range(g):
                    a = a0 + j
                    p.op("pe", lambda e, j=j, a=a: e.transpose(pst[:, j * 128:(j + 1) * 128], nmb[:, a * 128:(a + 1) * 128], c.ident_b[:, :]),
                         [p.key(nmb), p.key(c.ident_b)], [p.key(pstn)], inc=(j == g - 1))
                o_ap = c.nmT[:, tri(b) + a0:tri(b) + a0 + g, :]
                i_ap = pst[:, 0:g * 128].rearrange("p (a b) -> p a b", a=g)
                p.op("act", lambda e: e.activation(out=o_ap, in_=i_ap, func=AF.Copy), [p.key(pstn)], [p.key(c.nmT, {c.nmT.name: b})])

        for b in range(2):
            L = 128 * (b + 1)
            p.memset("pool", nm[b][:, :L], 0.0)
            p.memset("pool", nm[b][0:64, L - 64:L], NEG)
        prev = [0, 1]
        build_dsg(2)
        build_dsg(3)
        for b0 in range(2, NT, 2):
            bs = [b0, b0 + 1]
            scores(bs)
            for b in prev:
                transposes(b)
            if b0 + 2 < NT:
                build_dsg(b0 + 2)
                build_dsg(b0 + 3)
            bisect(bs)
            prev = bs
        for b in prev:
            transposes(b)


def layer0_attention(p, c, W):
    with p.phase():
        kT = p.sb("kT", [128, 4, T], BF16)
        for m in range(4):
            p.dma("sp", kT[:, m, :], c.kT_d[m])
        qzs = [p.sb("qz%d" % i, [128, 4, 2, 128], BF16) for i in range(2)]
        for i in range(2):
            p.memset("pool", qzs[i][:, :, :, :], 0.0)
        va = p.sb("va", [128, NT, 8, 65], BF16)
        p.memset("pool", va[:, :, :, 64:65], 1.0)
        for tt in range(NT):
            p.dma("sp", va[:, tt, :, 0:64], c.v_d[tt * 128:(tt + 1) * 128, :].rearrange("p (h d) -> p h d", h=8))
        oh1 = p.sb("oh1", [32, 383], F32)
        p.dma("sp", oh1[:, :], W["oh1"])
        rb = p.sb("rb", [32, 8], F32)
        p.dma("sp", rb[:, :], W["rel_bias"])
        cb = p.sb("cb", [128, 8], F32)
        p.dma("sp", cb[:, :], W["rel_bias"][15:16, :].partition_broadcast(128))
        Tb = p.sb("Tb", [128, 2, 8, 128], BF16)
        for kind in range(2):
            for t0 in range(0, 128, 64):
                ps = c.ps[c.psi % 8]
                c.psi += 1
                for t in range(t0, t0 + 64):
                    base = (255 - t) if kind == 0 else (127 - t)
                    p.mm(ps[:, (t - t0) * 8:(t - t0) * 8 + 8], oh1[:, base:base + 128], rb[:, :], start=True, stop=True,
                         inc=(t == t0 + 63))
                p.tt("dve", Tb[:, kind, :, t0:t0 + 64], ps[:, 0:512].rearrange("p (t h) -> p h t", h=8),
                     cb[:, :].unsqueeze(2).to_broadcast([128, 8, 64]), ALU.subtract)
        ya = [p.sb("ya%d" % i, [128, 512], BF16) for i in range(2)]
        PT = [p.sb("PT%d" % i, [128, 1024], BF16) for i in range(3)]
        rec = p.sb("rec", [128, 16], F32)
        yaT = [p.sb("yaT%d" % i, [128, 512], BF16) for i in range(2)]
        si = 0
        pend = []

        def normalize(b, h, po):
            ri = (b * 8 + h) % 16
            p.recip(rec[:, ri:ri + 1], po[:, 64:65])
            p.act(ya[b % 2][:, h * 64:(h + 1) * 64], po[:, 0:64], AF.Copy, scale=rec[:, ri:ri + 1])
            if h == 7:
                pstn = c.ps[6 + b % 2]
                pst = c.psb[6 + b % 2]
                yab = ya[b % 2]
                for cc in range(4):
                    p.op("pe", lambda e, cc=cc: e.transpose(pst[:, cc * 128:(cc + 1) * 128], yab[:, cc * 128:(cc + 1) * 128], c.ident_b[:, :]),
                         [p.key(yab), p.key(c.ident_b)], [p.key(pstn)], inc=(cc == 3))
                yst = yaT[b % 2]
                p.copy("dve", yst[:, :], pst[:, 0:512])
                p.dma("sp", c.yT_d[0:4, :, b * 128:(b + 1) * 128].rearrange("c p t -> p c t"),
                      yst[:, :].rearrange("p (a b) -> p a b", a=4))

        jobs = [(b, h, a0) for b in range(NT) for h in range(8) for a0 in range(0, b + 1, 8)]

        def emit_score(job, idx):
            b, h, a0 = job
            qz = qzs[b % 2]
            if h == 0 and a0 == 0:
                for par in range(2):
                    p.dma("sp", qz[par * 64:(par + 1) * 64, :, par, :],
                          c.qT_d[:, par * 64:(par + 1) * 64, b * 128:(b + 1) * 128].rearrange("m p t -> p m t"))
            m = h // 2
            grp = list(range(a0, min(a0 + 8, b + 1)))
            n = len(grp) * 128
            slot = idx % 2
            ps = c.psum[:, slot * 1024:slot * 1024 + n]
            for j, a in enumerate(grp):
                near = (a >= b - 1)
                lastj = (j == len(grp) - 1)
                o = ps[:, j * 128:(j + 1) * 128]
                p.mm(o, kT[:, m, a * 128:(a + 1) * 128], qz[:, h // 2, h % 2, :], start=True, stop=False, inc=False)
                p.mm(o, c.ident_b[:, :], c.nmT[:, tri(b) + a, :], start=False, stop=(not near), inc=((not near) and lastj))
                if near:
                    kind = 0 if a == b else 1
                    p.mm(o, c.ident_b[:, :], Tb[:, kind, h, :], start=False, stop=True, inc=lastj)
            return ps

        def emit_rest(job, idx, ps):
            nonlocal pend
            b, h, a0 = job
            grp = list(range(a0, min(a0 + 8, b + 1)))
            n = len(grp) * 128
            po = c.ps[4 + (b * 8 + h) % 2]
            pt = PT[idx % 3]
            p.act(pt[:, :n], ps, AF.Exp, bias=cb[:, h:h + 1])
            for j, a in enumerate(grp):
                p.mm(po[:, 0:65], pt[:, j * 128:(j + 1) * 128], va[:, a, h, :], start=(a == 0), stop=(a == b))
            if grp[-1] == b:
                for f in pend:
                    f()
                pend = [lambda b=b, h=h, po=po: normalize(b, h, po)]

        ps = emit_score(jobs[0], 0)
        for i, job in enumerate(jobs):
            nxt = emit_score(jobs[i + 1], i + 1) if i + 1 < len(jobs) else None
            emit_rest(job, i, ps)
            ps = nxt
        for f in pend:
            f()


def out_proj_residual(p, c, w_l, src):
    kch = w_l.shape[2]
    with p.phase():
        c.wb = {"wb": [p.sb("wbo%d" % i, [128, kch, 128], BF16) for i in range(2)]}

        def cons(b, tb, ps):
            p.tt("dve", c.xT[:, b, tb * 512:(tb + 1) * 512], ps[:, :], c.xT[:, b, tb * 512:(tb + 1) * 512], ALU.add)
        proj_stream(p, c, "fm", src, w_l, cons, kch=kch)


def conv_ffn(p, c, W, l):
    w_up = W["w_up%d" % l]
    w_dn = W["w_dn%d" % l]
    with p.phase():
        cw = p.sb("cw", [128, 44, 3], F32)
        cbs = p.sb("cbs", [128, 44], F32)
        p.dma("sp", cw[:, :, :], W["ffn_cw%d" % l])
        p.dma("sp", cbs[:, :], W["ffn_cb%d" % l])
        halo = p.sb("halo", [128, 44, 2], F32)
        for half in range(2):
            t0 = half * 1024
            with p.phase():
                hT = p.sb("hTf", [128, 8, 1024], BF16)
                gT = p.sb("gT", [128, NJ, 1024], BF16)
                with p.phase():
                    c.sq = [p.sb("fsq%d" % i, [128, 8, 512], BF16) for i in range(2)]
                    c.rs = [p.sb("frs%d" % i, [128, 512], F32) for i in range(2)]
                    rmsnorm_fm(p, c, c.g_ffn[l], hT, t0, 1024)
                wu = [[p.sb("wu%d_%d" % (sd, i), [128, 8, 128], BF16) for i in range(2)] for sd in range(2)]
                ya = [p.sb("fya%d" % i, [128, 512], F32) for i in range(3)]
                yb = [p.sb("fyb%d" % i, [128, 512], F32) for i in range(3)]
                sa = [p.sb("fsa%d" % i, [128, 512], F32) for i in range(2)]
                hl = [p.sb("fhl%d" % i, [128, 2], F32) for i in range(4)]
                hli = 0

                def load(j):
                    p.dma("pool", wu[0][j % 2][:, :, :], w_up[j])
                    p.dma("pool", wu[1][j % 2][:, :, :], w_up[NJ + j])

                load(0)
                it = 0
                for j in range(NJ):
                    if j + 1 < NJ:
                        load(j + 1)
                    for tb in range(2):
                        ys = []
                        for sd in range(2):
                            m = sd * NJ + j
                            ps = c.ps[c.psi % 8]
                            c.psi += 1
                            wt = wu[sd][j % 2]
                            for k in range(8):
                                p.mm(ps[:, :], wt[:, k, :], hT[:, k, tb * 512:(tb + 1) * 512], start=(k == 0), stop=(k == 7))
                            y = (ya if sd == 0 else yb)[it % 3]
                            p.act(y[:, :], ps[:, :], AF.Identity, bias=cbs[:, m:m + 1], scale=cw[:, m, 2:3])
                            p.stt(y[:, 1:512], ps[:, 0:511], cw[:, m, 1:2], y[:, 1:512], ALU.mult, ALU.add)
                            p.stt(y[:, 2:512], ps[:, 0:510], cw[:, m, 0:1], y[:, 2:512], ALU.mult, ALU.add)
                            first = (half == 0 and tb == 0)
                            if not first:
                                if tb == 0:
                                    hsrc = halo[:, m, :]
                                else:
                                    hsrc = hprev[sd][:, :]
                                p.stt(y[:, 0:1], hsrc[:, 1:2], cw[:, m, 1:2], y[:, 0:1], ALU.mult, ALU.add)
                                p.stt(y[:, 0:2], hsrc[:, 0:2], cw[:, m, 0:1], y[:, 0:2], ALU.mult, ALU.add)
                            if tb == 0:
                                if sd == 0:
                                    hprev = [None, None]
                                hprev[sd] = hl[hli % 4]
                                hli += 1
                                p.copy("act", hprev[sd][:, :], ps[:, 510:512])
                            elif half == 0:
                                p.copy("act", halo[:, m, :], ps[:, 510:512])
                            ys.append(y)
                        sg = sa[it % 2]
                        p.act(sg[:, :], ys[0][:, :], AF.Silu)
                        p.tt("pool", gT[:, j, tb * 512:(tb + 1) * 512], sg[:, :], ys[1][:, :], ALU.mult)
                        it += 1
                wd = [p.sb("wd%d" % i, [128, NJ, 128], BF16) for i in range(2)]
                p.dma("pool", wd[0][:, :, :], w_dn[0])
                for dc in range(8):
                    if dc + 1 < 8:
                        p.dma("pool", wd[(dc + 1) % 2][:, :, :], w_dn[dc + 1])
                    for tb in range(2):
                        ps = c.ps[c.psi % 8]
                        c.psi += 1
                        for j in range(NJ):
                            p.mm(ps[:, :], wd[dc % 2][:, j, :], gT[:, j, tb * 512:(tb + 1) * 512], start=(j == 0), stop=(j == NJ - 1))
                        sl = c.xT[:, dc, t0 + tb * 512:t0 + (tb + 1) * 512]
                        p.tt("dve", sl, ps[:, :], sl, ALU.add)


def hgrn_mixer(p, c, W):
    with p.phase():
        hT = p.sb("hT1", [128, 8, T], BF16)
        c.wb = {"wb": [p.sb("wbh%d" % i, [128, 8, 512], BF16) for i in range(2)]}
        lbr = p.sb("lbr", [128, 2, 8], F32)
        p.dma("sp", lbr[:, :, :], W["hgrn_lb"])
        lb = p.sb("lb", [128, 8], F32)
        oml = p.sb("oml", [128, 8], F32)
        p.tt("dve", lb[:, :], lbr[:, 1, :], lbr[:, 0, :], ALU.subtract)
        p.act(lb[:, :], lb[:, :], AF.Sigmoid)
        p.ts("dve", oml[:, :], lb[:, :], -1.0, 1.0, ALU.mult, ALU.add)
        with p.phase():
            c.sq = [p.sb("hsq%d" % i, [128, 8, 512], BF16) for i in range(2)]
            c.rs = [p.sb("hrs%d" % i, [128, 512], F32) for i in range(2)]
            rmsnorm_fm(p, c, c.g_mix[1], hT, 0, T)
        stg = [p.sb("hstg%d" % i, [128, T], BF16) for i in range(2)]

        def cons_silu(dst):
            def f(b, tb, ps):
                st = stg[b % 2]
                p.act(st[:, tb * 512:(tb + 1) * 512], ps[:, :], AF.Silu)
                if tb == 3:
                    p.dma("sp", dst[b], st[:, :])
            return f
        proj_stream(p, c, "fm", hT, W["w_cq"], cons_silu(c.qh_d))
        proj_stream(p, c, "fm", hT, W["w_cg"], cons_silu(c.gate_d))

        gst = [p.sb("gst%d" % i, [128, T], F32) for i in range(2)]
        sgt = [p.sb("sgt%d" % i, [128, 512], F32) for i in range(2)]

        def cons_f(b, tb, ps):
            g = gst[b % 2]
            sg = sgt[tb % 2]
            p.act(sg[:, :], ps[:, :], AF.Sigmoid)
            p.ts("dve", g[:, tb * 512:(tb + 1) * 512], sg[:, :], oml[:, b:b + 1], lb[:, b:b + 1], ALU.mult, ALU.add)
            if tb == 3:
                st = stg[b % 2]
                p.ts("dve", st[:, :], g[:, :], -1.0, 1.0, ALU.mult, ALU.add)
                p.dma("sp", c.kk_d[b], st[:, :])
                p.act(g[:, :], g[:, :], AF.Ln)
                p.dma("sp", c.lg_d[b], g[:, :])
        proj_stream(p, c, "fm", hT, W["w_cf"], cons_f)

        ist = [p.sb("ist%d" % i, [128, 512], BF16) for i in range(2)]

        def cons_i(b, tt, ps):
            st = ist[tt % 2]
            p.copy("act" if tt % 2 == 0 else "dve", st[:, :], ps[:, :])
            p.dma("sp", c.i_d[tt * 128:(tt + 1) * 128, b * 512:(b + 1) * 512], st[:, :])
        proj_stream(p, c, "tm", hT, W["w_ci"], cons_i)
    if c.stop_after == "hproj":
        return
    with p.phase():
        oT_all = p.sb("oT_all", [128, 8, T], BF16)
        with p.phase():
            cm = p.sb("cm", [128, 32, 64], F32)
            p.memset("pool", cm[:, :, :], 1.0)
            p.memset("pool", cm[:, :, 0:1], 0.0)
            tri64 = p.sb("tri64", [64, 64], F32)
            p.dma("sp", tri64[:, :], W["tri64"])
            cn = p.sb("cn", [128, 1], F32)
            p.dma("sp", cn[:, :], W["c_out_norm"])
            lg = p.sb("lg", [128, T], F32)
            bb = p.sb("bb", [128, T], F32)
            eb = p.sb("eb", [128, T], F32)
            t1 = p.sb("t1", [128, T], F32)
            qh = p.sb("qh", [128, T], BF16)
            kk = p.sb("kk", [128, T], BF16)
            Qb = p.sb("Qb", [128, T], BF16)
            Kbb = p.sb("Kbb", [128, T], BF16)
            KlT = p.sb("KlT", [128, T], BF16)
            Klc = p.sb("Klc", [64, 32, 128], BF16)
            ic = p.sb("ic", [64, 32, 128], BF16)
            oT = p.sb("oT", [128, T], F32)
            gt = p.sb("gt", [128, T], BF16)
            Sf = p.sb("Sf", [128, 128], F32)
            Sb = [p.sb("Sb%d" % i, [128, 128], BF16) for i in range(2)]
            ATs = [p.sb("ATs%d" % i, [64, 64], BF16) for i in range(3)]
            osq = [p.sb("osq%d" % i, [128, 512], BF16) for i in range(2)]
            ors = [p.sb("ors%d" % i, [128, 512], F32) for i in range(2)]
            on = [p.sb("on%d" % i, [128, 512], F32) for i in range(2)]
            for h in range(8):
                p.dma("sp", lg[:, :], c.lg_d[h])
                p.dma("sp", qh[:, :], c.qh_d[h])
                p.dma("sp", kk[:, :], c.kk_d[h])
                p.dma("sp", gt[:, :], c.gate_d[h])
                p.dma("sp", ic[:, :, :], c.i_d[:, h * 128:(h + 1) * 128].rearrange("(n s) e -> s n e", s=64))
                p.scan(bb[:, :], cm[:, :, :].rearrange("p a b -> p (a b)"), lg[:, :], 0.0, ALU.mult, ALU.add)
                p.act(eb[:, :], bb[:, :], AF.Exp)
                p.act(t1[:, :], bb[:, :], AF.Exp, scale=-1.0)
                p.tt("pool", Qb[:, :], qh[:, :], eb[:, :], ALU.mult)
                p.tt("dve", t1[:, :], kk[:, :], t1[:, :], ALU.mult)
                p.copy("pool", Kbb[:, :], t1[:, :])
                ebv = eb[:, :].rearrange("p (a b) -> p a b", b=64)
                p.tt("dve", KlT[:, :].rearrange("p (a b) -> p a b", b=64), t1[:, :].rearrange("p (a b) -> p a b", b=64),
                     ebv[:, :, 63:64].to_broadcast([128, 32, 64]), ALU.mult)
                for g8 in range(4):
                    pst = c.psb[c.psi % 8]
                    pstn = c.ps[c.psi % 8]
                    c.psi += 1
                    for j in range(8):
                        n = g8 * 8 + j
                        p.op("pe", lambda e, j=j, n=n: e.transpose(pst[0:64, j * 128:(j + 1) * 128], KlT[:, n * 64:(n + 1) * 64], c.ident_b[:, :]),
                             [p.key(KlT), p.key(c.ident_b)], [p.key(pstn)], inc=(j == 7))
                    p.op("act", lambda e, g8=g8: e.activation(out=Klc[:, g8 * 8:(g8 + 1) * 8, :],
                                                              in_=pst[0:64, :].rearrange("p (a b) -> p a b", a=8), func=AF.Copy),
                         [p.key(pstn)], [p.key(Klc)])
                po = None
                for n in range(32):
                    cs = slice(n * 64, (n + 1) * 64)
                    if n < 31:
                        psS = c.ps[4 + n % 3]
                        p.mm(psS[:, 0:128], Klc[:, n, :], ic[:, n, :])
                    psA = c.ps[n % 2]
                    p.mm(psA[0:64, 0:64], Kbb[:, cs], Qb[:, cs])
                    at = ATs[n % 3]
                    p.tt("dve", at[:, :], psA[0:64, 0:64], tri64[:, :], ALU.mult)
                    if n % 8 == 0:
                        po = c.ps[2 + (n // 8) % 2]
                    oc = po[:, (n % 8) * 64:(n % 8 + 1) * 64]
                    p.mm(oc, ic[:, n, :], at[:, :], start=True, stop=(n == 0), inc=(n == 0))
                    if n > 0:
                        p.mm(oc, Sb[(n - 1) % 2][:, :], Qb[:, cs], start=False, stop=True, inc=True)
                    if n % 8 == 7:
                        p.copy("act", oT[:, (n // 8) * 512:(n // 8 + 1) * 512], po[:, :])
                    if n < 31:
                        if n == 0:
                            p.copy("dve", Sf[:, :], psS[:, 0:128])
                        else:
                            p.stt(Sf[:, :], Sf[:, :], eb[:, n * 64 + 63:n * 64 + 64], psS[:, 0:128], ALU.mult, ALU.add)
                        p.copy("act", Sb[n % 2][:, :], Sf[:, :])
                for tb in range(4):
                    ts_ = slice(tb * 512, (tb + 1) * 512)
                    sq = osq[tb % 2]
                    p.act(sq[:, :], oT[:, ts_], AF.Square)
                    ps = c.ps[6 + tb % 2]
                    p.mm(ps[:, :], c.ones_b[:, :], sq[:, :])
                    rs = ors[tb % 2]
                    p.act(rs[:, :], ps[:, :], AF.Sqrt, bias=c.eps_ap[:, :], scale=1.0 / 128)
                    p.recip(rs[:, :], rs[:, :])
                    o2 = on[tb % 2]
                    p.stt(o2[:, :], oT[:, ts_], cn[:, 0:1], rs[:, :], ALU.mult, ALU.mult)
                    p.tt("pool", oT_all[:, h, ts_], o2[:, :], gt[:, ts_], ALU.mult)
        if c.stop_after == "hrec":
            if "oT_dbg" in c.dbg:
                od = c.dscr("oT_dbg", [8, 128, T], BF16)
                for k in range(8):
                    p.dma("sp", od[k], oT_all[:, k, :])
            return
        out_proj_residual(p, c, W["w_out1"], oT_all)


def final_norm(p, c, out):
    with p.phase():
        c.sq = [p.sb("nsq%d" % i, [128, 8, 512], BF16) for i in range(2)]
        c.rs = [p.sb("nrs%d" % i, [128, 512], F32) for i in range(2)]
        ot = [p.sb("fot%d" % i, [128, 8, 512], F32) for i in range(2)]
        for i, tb in enumerate(range(0, T, 512)):
            sq = c.sq[i % 2]
            p.act(sq[:, :, :], c.xT[:, :, tb:tb + 512], AF.Square)
            ps = c.ps[c.psi % 8]
            c.psi += 1
            for k in range(8):
                p.mm(ps[:, :], c.ones_b[:, :], sq[:, k, :], start=(k == 0), stop=(k == 7))
            rs = c.rs[i % 2]
            p.act(rs[:, :], ps[:, :], AF.Sqrt, bias=c.eps_ap[:, :], scale=1.0 / D)
            p.recip(rs[:, :], rs[:, :])
            o = ot[i % 2]
            for k in range(8):
                p.stt(o[:, k, :], c.xT[:, k, tb:tb + 512], c.g_fin[:, k:k + 1], rs[:, :], ALU.mult, ALU.mult)
            p.dma("sp", out[:, :, tb:tb + 512].rearrange("c p t -> p c t"), o[:, :, :])

def lay(Wm, cw):
    K, N = Wm.shape
    return np.ascontiguousarray(Wm.reshape(K // 128, 128, N // cw, cw).transpose(2, 1, 0, 3))


def build(stop_after=None, dbg=()):
    nc = bass.Bass("TRN2", target_bir_lowering=False)
    p = Prog(nc)
    c = Ctx()
    W = {}

    def din(name, shape, dt=F32):
        W[name] = nc.dram_tensor(name, list(shape), dt, kind="ExternalInput").ap()
        return W[name]

    def dscr(name, shape, dt):
        kind = "ExternalOutput" if name in dbg else "Internal"
        return nc.dram_tensor(name, list(shape), dt, kind=kind).ap()

    din("xT_in", [8, 128, T])
    din("ident", [128, 128])
    din("g_mix", [2, 128, 8]); din("g_ffn", [2, 128, 8]); din("g_fin", [128, 8])
    din("w_q", [4, 128, 8, 128]); din("w_k", [4, 128, 8, 128]); din("w_iq", [8, 128, 8, 128])
    din("w_ik2", [1, 128, 8, 128]); din("w_v", [1, 128, 8, 512]); din("w_iw", [1, 128, 8, 16])
    din("w_u", [1, 128, 8, 512]); din("w_vg", [1, 128, 8, 512])
    din("gmlp_norm", [1, 512]); din("w_sT", [128, 8, 128]); din("b_s", [8, 128]); din("e8", [8, 512])
    din("gk2", [128, 1]); din("pow2", [128, NBIS]); din("oh1", [32, 383]); din("rel_bias", [32, 8])
    din("w_out0", [8, 128, 8, 128])
    for l in range(2):
        din("w_up%d" % l, [44, 128, 8, 128]); din("w_dn%d" % l, [8, 128, NJ, 128])
        din("ffn_cw%d" % l, [128, 44, 3]); din("ffn_cb%d" % l, [128, 44])
    din("w_cq", [8, 128, 8, 128]); din("w_cf", [8, 128, 8, 128]); din("w_cg", [8, 128, 8, 128]); din("w_ci", [2, 128, 8, 512])
    din("hgrn_lb", [128, 2, 8]); din("tri64", [64, 64]); din("c_out_norm", [128, 1]); din("w_out1", [8, 128, 8, 128])
    out = nc.dram_tensor("outT", [8, 128, T], F32, kind="ExternalOutput").ap()

    c.qT_d = dscr("qT_d", [4, 128, T], BF16)
    c.kT_d = dscr("kT_d", [4, 128, T], BF16)
    c.iqT_d = dscr("iqT_d", [8, 128, T], BF16)
    c.v_d = dscr("v_d", [T, 512], BF16)
    c.yT_d = dscr("yT_d", [8, 128, T], BF16)
    c.qh_d = dscr("qh_d", [8, 128, T], BF16)
    c.gate_d = dscr("gate_d", [8, 128, T], BF16)
    c.kk_d = dscr("kk_d", [8, 128, T], BF16)
    c.lg_d = dscr("lg_d", [8, 128, T], F32)
    c.i_d = dscr("i_d", [T, 1024], BF16)

    c.xT = p.sb("xT", [128, 8, T], F32, True)
    c.ident_f = p.sb("ident_f", [128, 128], F32, True)
    c.ident_b = p.sb("ident_b", [128, 128], BF16, True)
    c.ones_b = p.sb("ones_b", [128, 128], BF16, True)
    c.eps_ap = p.sb("eps_ap", [128, 1], F32, True)
    gm = p.sb("g_mix_sb", [128, 2, 8], F32, True)
    gf = p.sb("g_ffn_sb", [128, 2, 8], F32, True)
    gfin = p.sb("g_fin_sb", [128, 8], F32, True)
    c.psum = p.es.enter_context(nc.psum_tensor("psum", [128, 4096], F32))
    c.ps = [c.psum[:, i * 512:(i + 1) * 512] for i in range(8)]
    c.psb = [t.bitcast(BF16) for t in c.ps]
    c.psi = 0
    c.stop_after = stop_after
    c.dbg = dbg
    c.dscr = dscr

    for k in range(8):
        p.dma("sp", c.xT[:, k, :], W["xT_in"][k])
    p.dma("sp", c.ident_f[:, :], W["ident"])
    p.dma("pool", c.ident_b[:, :], W["ident"])
    p.memset("dve", c.ones_b[:, :], 1.0)
    p.memset("dve", c.eps_ap[:, :], EPS)
    for l in range(2):
        p.dma("sp", gm[:, l, :], W["g_mix"][l])
        p.dma("sp", gf[:, l, :], W["g_ffn"][l])
    p.dma("sp", gfin[:, :], W["g_fin"])
    c.g_mix = [gm[:, l, :] for l in range(2)]
    c.g_ffn = [gf[:, l, :] for l in range(2)]
    c.g_fin = gfin

    layer0_mixer(p, c, W)
    stages = ["gmlp", "indexer", "attn", "mix0", "ffn0", "hproj", "hrec", "mix1", "ffn1", None]
    si_ = stages.index(stop_after)
    if si_ >= stages.index("ffn0"):
        conv_ffn(p, c, W, 0)
    if si_ >= stages.index("hproj"):
        hgrn_mixer(p, c, W)
    if si_ >= stages.index("ffn1"):
        conv_ffn(p, c, W, 1)
    if stop_after is None:
        final_norm(p, c, out)
        p.barrier()
        return nc, p, c

    for k in range(8):
        p.dma("sp", out[k], c.xT[:, k, :])
    p.barrier()
    return nc, p, c


def t5_onehot():
    rel = np.arange(-255, 128)
    half, max_exact = 16, 8
    ret = np.where(rel > 0, half, 0)
    n = np.abs(rel)
    nf = np.maximum(n, max_exact).astype(np.float32)
    large = max_exact + (np.log(nf / np.float32(max_exact)) / np.float32(np.log(128 / 8)) * np.float32(half - max_exact)).astype(np.int32)
    large = np.minimum(large, half - 1)
    bucket = ret + np.where(n < max_exact, n, large)
    oh = np.zeros((32, 383), np.float32)
    oh[bucket, np.arange(383)] = 1.0
    return oh


def host_inputs(inp, b):
    f = np.float32
    m = {}
    x = inp["x"][b]
    m["xT_in"] = np.ascontiguousarray(x.T.reshape(8, 128, T))
    m["ident"] = np.eye(128, dtype=f)
    m["g_mix"] = np.ascontiguousarray(inp["mix_norm"].reshape(2, 8, 128).transpose(0, 2, 1))
    m["g_ffn"] = np.ascontiguousarray(inp["ffn_norm"].reshape(2, 8, 128).transpose(0, 2, 1))
    m["g_fin"] = np.ascontiguousarray(inp["final_norm"].reshape(8, 128).T)
    Wi = inp["ab_w_in"][0]
    m["w_q"] = lay(Wi[:, 0:512], 128)
    m["w_k"] = lay(Wi[:, 512:1024], 128)
    m["w_v"] = lay(Wi[:, 1024:1536], 512)
    m["w_iq"] = lay(Wi[:, 1536:2560], 128)
    m["w_ik2"] = lay(np.concatenate([Wi[:, 2560:2624], Wi[:, 2560:2624]], axis=1), 128)
    m["w_iw"] = lay(Wi[:, 2624:2640], 16)
    m["w_u"] = lay(Wi[:, 2640:3152], 512)
    m["w_vg"] = lay(Wi[:, 3152:3664], 512)
    m["gmlp_norm"] = np.ascontiguousarray(inp["ab_gmlp_norm"][0].reshape(1, 512))
    m["w_sT"] = np.ascontiguousarray(inp["ab_w_s"][0].transpose(2, 0, 1))
    m["b_s"] = np.ascontiguousarray(inp["ab_b_s"][0])
    e8 = np.zeros((8, 512), f)
    for g in range(8):
        e8[g, g * 64:(g + 1) * 64] = 1.0
    m["e8"] = e8
    m["gk2"] = np.concatenate([inp["ab_idx_k_norm"][0], inp["ab_idx_k_norm"][0]]).reshape(128, 1)
    m["pow2"] = np.tile((0.5 ** np.arange(1, NBIS + 1)).astype(f)[None, :], (128, 1))
    m["oh1"] = t5_onehot()
    m["rel_bias"] = inp["rel_bias"]
    m["w_out0"] = lay(inp["ab_w_out"][0], 128)
    Wc = inp["c_w_in"][0]
    m["w_cq"] = lay(Wc[:, 0:1024], 128)
    m["w_cf"] = lay(Wc[:, 1024:2048], 128)
    m["w_ci"] = lay(Wc[:, 2048:3072], 512)
    m["w_cg"] = lay(Wc[:, 3072:4096], 128)
    m["hgrn_lb"] = inp["hgrn_lb"].reshape(2, 8, 128).transpose(2, 0, 1)
    m["tri64"] = np.triu(np.ones((64, 64), f))
    m["c_out_norm"] = inp["c_out_norm"][0].reshape(128, 1)
    m["w_out1"] = lay(inp["c_w_out"][0], 128)
    for l in range(2):
        m["w_up%d" % l] = lay(inp["ffn_w_up"][l], 128)
        m["w_dn%d" % l] = lay(inp["ffn_w_down"][l], 128)
        m["ffn_cw%d" % l] = inp["ffn_conv_w"][l].reshape(3, 44, 128).transpose(2, 1, 0)
        m["ffn_cb%d" % l] = inp["ffn_conv_b"][l].reshape(44, 128).T
    return {k: np.ascontiguousarray(v, dtype=f) for k, v in m.items()}


def kernel(**inputs):
    inp = {k: np.asarray(v) for k, v in inputs.items()}
    nc, p, c = build()
    in_maps = [host_inputs(inp, b) for b in range(8)]
    res = run_bass_kernel_spmd(nc, in_maps, core_ids=list(range(8)))
    outs = [np.asarray(r["outT"]).reshape(D, T).T for r in res.results]
    return np.stack(outs, axis=0).astype(np.float32)
```

```python
import numpy as np
from contextlib import ExitStack, contextmanager
import concourse.bass as bass
import concourse.mybir as mybir
from concourse.bass_utils import run_bass_kernel_spmd

F32 = mybir.dt.float32
BF16 = mybir.dt.bfloat16
ALU = mybir.AluOpType
AF = mybir.ActivationFunctionType
AX = mybir.AxisListType

T = 2048
D = 1024
NT = 16
EPS = 1e-6
D_FF = 2816
NJ = 22
TOPK = 256
NBIS = 16
NEG = -30000.0


class Tok:
    __slots__ = ("sem", "key", "val", "clock")

    def __init__(self, sem, key, val, clock):
        self.sem, self.key, self.val, self.clock = sem, key, val, clock


class Reg:
    __slots__ = ("w", "r")

    def __init__(self):
        self.w = None
        self.r = {}


class Prog:
    def __init__(self, nc):
        self.nc = nc
        self.es = ExitStack()
        self.E = {"pe": nc.tensor, "act": nc.scalar, "dve": nc.vector, "pool": nc.gpsimd, "sp": nc.sync}
        self.csem = {e: self.es.enter_context(nc.semaphore("cs_" + e)) for e in ("pe", "act", "dve", "pool")}
        self.ccnt = {e: 0 for e in self.csem}
        self.known = {e: {} for e in self.E}
        self.dq = {}
        for q, n in (("sp", 24), ("pool", 16)):
            self.dq[q] = dict(
                sems=[self.es.enter_context(nc.semaphore("d_%s%d" % (q, i))) for i in range(n)],
                use=[0] * n, last=[None] * n, nxt=0)
        self.regs = {}
        self.last = {e: None for e in self.csem}
        self.phase_stack = None
        self.ninst = 0
        self.nwait = 0
        self.nsb = 0

    @staticmethod
    def key(x, sub=None):
        if isinstance(x, (tuple, list)):
            return x
        if isinstance(x, str):
            return (x, None)
        name = x.tensor.name if hasattr(x, "tensor") else x.name
        if name == "psum":
            sz = mybir.dt.size(x.dtype)
            apl = list(x.ap)
            off = (x.offset % apl[0][0]) * sz
            ext = sum((cnt - 1) * abs(st) for st, cnt in apl[1:]) * sz + sz
            return [("psum", bk) for bk in range(off // 2048, (off + ext - 1) // 2048 + 1)]
        if sub and name in sub:
            return (name, sub[name])
        return (name, None)

    def _targets(self, k):
        name, s = k
        d = self.regs.setdefault(name, {})
        if s is None:
            if None not in d:
                d[None] = Reg()
            return list(d.values())
        if s not in d:
            d[s] = Reg()
        out = [d[s]]
        if None in d:
            out.append(d[None])
        return out

    def _wait(self, eng, tok):
        if tok is None:
            return
        if eng == "pe" and tok.key == "pe":
            return
        k = self.known[eng]
        if k.get(tok.key, 0) >= tok.val:
            return
        if tok.key in self.ccnt:
            assert tok.val <= self.ccnt[tok.key], ("wait on future inc", eng, tok.key, tok.val)
        self.E[eng].wait_ge(tok.sem, tok.val)
        self.nwait += 1
        for kk, vv in tok.clock.items():
            if k.get(kk, 0) < vv:
                k[kk] = vv

    def _sync(self, eng, reads, writes):
        for k in reads:
            for rg in self._targets(k):
                self._wait(eng, rg.w)
        for k in writes:
            for rg in self._targets(k):
                self._wait(eng, rg.w)
                for t in list(rg.r.values()):
                    self._wait(eng, t)

    def _record(self, tok, reads, writes):
        for k in reads:
            name, s = k
            rg = self.regs[name][s]
            old = rg.r.get(tok.key)
            if old is None or old.val < tok.val:
                rg.r[tok.key] = tok
        for k in writes:
            name, s = k
            rg = self.regs[name][s]
            rg.w = tok
            rg.r = {}

    def _flat(self, ks):
        out = []
        for k in ks:
            k = self.key(k)
            if isinstance(k, list):
                out += k
            else:
                out.append(k)
        return out

    def op(self, eng, fn, reads, writes, inc=True):
        reads = self._flat(reads)
        writes = self._flat(writes)
        self._sync(eng, reads, writes)
        inst = fn(self.E[eng])
        self.ninst += 1
        if inc:
            self.ccnt[eng] += 1
            inst.then_inc(self.csem[eng], 1)
            val = self.ccnt[eng]
        else:
            val = self.ccnt[eng] + 1
        clock = dict(self.known[eng])
        clock[eng] = max(clock.get(eng, 0), val)
        tok = Tok(self.csem[eng], eng, val, clock)
        self.last[eng] = tok
        self._record(tok, reads, writes)
        return tok

    def dma(self, q, out, in_, sub=None):
        reads = self._flat([self.key(in_, sub)])
        writes = self._flat([self.key(out, sub)])
        self._sync(q, reads, writes)
        d = self.dq[q]
        j = d["nxt"]
        d["nxt"] = (j + 1) % len(d["sems"])
        self._wait(q, d["last"][j])
        inst = self.E[q].dma_start(out=out, in_=in_)
        self.ninst += 1
        d["use"][j] += 1
        inst.then_inc(d["sems"][j], 16)
        key = ("d", q, j)
        val = 16 * d["use"][j]
        clock = dict(self.known[q])
        clock[key] = val
        tok = Tok(d["sems"][j], key, val, clock)
        d["last"][j] = tok
        self._record(tok, reads, writes)
        return tok

    def barrier(self):
        toks = [t for t in self.last.values() if t is not None]
        for d in self.dq.values():
            toks += [t for t in d["last"] if t is not None]
        for e in self.E:
            for t in toks:
                self._wait(e, t)

    def sb(self, name, shape, dtype, persistent=False):
        st = self.es if (persistent or self.phase_stack is None) else self.phase_stack
        self.nsb += 1
        return st.enter_context(self.nc.sbuf_tensor("s%d_%s" % (self.nsb, name), list(shape), dtype))

    @contextmanager
    def phase(self):
        prev = self.phase_stack
        self.phase_stack = ExitStack()
        try:
            yield
            self.barrier()
        finally:
            self.phase_stack.close()
            self.phase_stack = prev

    def _aps(self, sub, *xs):
        return [self.key(x, sub) for x in xs if x is not None and not isinstance(x, (int, float))]

    def mm(self, out, lhsT, rhs, start=True, stop=True, inc=None, sub=None):
        if inc is None:
            inc = stop
        return self.op("pe", lambda e: e.matmul(out, lhsT, rhs, start=start, stop=stop),
                       self._aps(sub, lhsT, rhs), self._aps(sub, out), inc=inc)

    def tr(self, out, in_, ident, inc=True, sub=None):
        return self.op("pe", lambda e: e.transpose(out, in_, ident),
                       self._aps(sub, in_, ident), self._aps(sub, out), inc=inc)

    def act(self, out, in_, func, bias=None, scale=None, accum_out=None, sub=None):
        kw = {}
        if bias is not None:
            kw["bias"] = bias
        if scale is not None:
            kw["scale"] = scale
        if accum_out is not None:
            kw["accum_out"] = accum_out
        return self.op("act", lambda e: e.activation(out=out, in_=in_, func=func, **kw),
                       self._aps(sub, in_, bias, scale), self._aps(sub, out, accum_out))

    def ts(self, eng, out, in0, s1, s2, op0, op1=None, accum_out=None, sub=None):
        kw = {}
        if op1 is not None:
            kw["op1"] = op1
        if accum_out is not None:
            kw["accum_out"] = accum_out
        return self.op(eng, lambda e: e.tensor_scalar(out=out, in0=in0, scalar1=s1, scalar2=s2, op0=op0, **kw),
                       self._aps(sub, in0, s1, s2), self._aps(sub, out, accum_out))

    def stt(self, out, in0, scalar, in1, op0, op1, sub=None):
        return self.op("dve", lambda e: e.scalar_tensor_tensor(out=out, in0=in0, scalar=scalar, in1=in1, op0=op0, op1=op1),
                       self._aps(sub, in0, scalar, in1), self._aps(sub, out))

    def tt(self, eng, out, in0, in1, op, sub=None):
        return self.op(eng, lambda e: e.tensor_tensor(out=out, in0=in0, in1=in1, op=op),
                       self._aps(sub, in0, in1), self._aps(sub, out))

    def copy(self, eng, out, in_, sub=None):
        if eng == "act":
            return self.act(out, in_, AF.Copy, sub=sub)
        return self.op(eng, lambda e: e.tensor_copy(out=out, in_=in_), self._aps(sub, in_), self._aps(sub, out))

    def memset(self, eng, ap, val, sub=None):
        return self.op(eng, lambda e: e.memset(ap, val), [], self._aps(sub, ap))

    def recip(self, out, in_, sub=None):
        return self.op("dve", lambda e: e.reciprocal(out=out, in_=in_), self._aps(sub, in_), self._aps(sub, out))

    def reduce(self, out, in_, op, sub=None):
        return self.op("dve", lambda e: e.tensor_reduce(out=out, in_=in_, axis=AX.X, op=op),
                       self._aps(sub, in_), self._aps(sub, out))

    def scan(self, out, d0, d1, initial, op0, op1, sub=None):
        return self.op("dve", lambda e: e.tensor_tensor_scan(out=out, data0=d0, data1=d1, initial=initial, op0=op0, op1=op1),
                       self._aps(sub, d0, d1), self._aps(sub, out))


class Ctx:
    pass


def rmsnorm_fm(p, c, g_ap, hT, t0, nt, nparts=128, kch=8, src=None, scale_div=None):
    src = c.xT if src is None else src
    div = float(scale_div if scale_div is not None else nparts * kch)
    for i, tb in enumerate(range(t0, t0 + nt, 512)):
        sq = c.sq[i % 2]
        sub = {hT.name: (tb - t0) // 512}
        if src is c.xT:
            sub[c.xT.name] = tb // 512
        p.act(sq[:nparts, :kch, :], src[:nparts, :kch, tb:tb + 512], AF.Square, sub=sub)
        ps = c.ps[c.psi % 8]
        c.psi += 1
        for k in range(kch):
            p.mm(ps[:nparts, :], c.ones_b[:nparts, :nparts], sq[:nparts, k, :], start=(k == 0), stop=(k == kch - 1))
        rs = c.rs[i % 2]
        p.act(rs[:nparts, :], ps[:nparts, :], AF.Ln, bias=c.eps_ap[:nparts, :], scale=1.0 / div)
        p.act(rs[:nparts, :], rs[:nparts, :], AF.Exp, scale=-0.5)
        for k in range(kch):
            p.stt(hT[:nparts, k, tb - t0:tb - t0 + 512], src[:nparts, k, tb:tb + 512], g_ap[:nparts, k:k + 1],
                  rs[:nparts, :], ALU.mult, ALU.mult, sub=sub)


def proj_stream(p, c, mode, hT, w_l, consume, kch=8, tsl=None, wname="wb"):
    nblk, _, _, cw = w_l.shape
    ntok = hT.shape[2] if tsl is None else tsl
    wbs = c.wb[wname]

    def load(b):
        p.dma("pool", wbs[b % 2][:, :kch, :cw], w_l[b])

    load(0)
    for b in range(nblk):
        if b + 1 < nblk:
            load(b + 1)
        wb = wbs[b % 2]
        if mode == "fm":
            for tb in range(ntok // 512):
                ps = c.ps[c.psi % 8]
                c.psi += 1
                for k in range(kch):
                    p.mm(ps[:cw, :], wb[:, k, :cw], hT[:, k, tb * 512:(tb + 1) * 512], start=(k == 0), stop=(k == kch - 1),
                         sub={hT.name: tb})
                consume(b, tb, ps)
        else:
            for tt in range(ntok // 128):
                ps = c.ps[c.psi % 8]
                c.psi += 1
                for k in range(kch):
                    p.mm(ps[:, :cw], hT[:, k, tt * 128:(tt + 1) * 128], wb[:, k, :cw], start=(k == 0), stop=(k == kch - 1),
                         sub={hT.name: tt // 4})
                consume(b, tt, ps)


def layer0_mixer(p, c, W):
    with p.phase():
        c.nmT = p.sb("nmT", [128, 136, 128], BF16)
        with p.phase():
            c.ik2 = p.sb("ik2", [128, 1, T], F32)
            c.iw_sb = p.sb("iw_sb", [128, NT, 16], F32)
            with p.phase():
                hT = p.sb("hT", [128, 8, T], BF16)
                c.wb = {"wb": [p.sb("wb%d" % i, [128, 8, 512], BF16) for i in range(2)]}
                with p.phase():
                    c.sq = [p.sb("sq%d" % i, [128, 8, 512], BF16) for i in range(2)]
                    c.rs = [p.sb("rs%d" % i, [128, 512], F32) for i in range(2)]
                    rmsnorm_fm(p, c, c.g_mix[0], hT, 0, T)
                    layer0_inproj_a(p, c, W, hT)
                layer0_gmlp(p, c, W, hT)
            if c.stop_after == "gmlp":
                return
            layer0_indexer(p, c, W)
        if c.stop_after == "indexer":
            return
        layer0_attention(p, c, W)
    if c.stop_after == "attn":
        return
    with p.phase():
        yT = p.sb("yT", [128, 8, T], BF16)
        for k in range(8):
            p.dma("sp", yT[:, k, :], c.yT_d[k])
        out_proj_residual(p, c, W["w_out0"], yT)


def layer0_inproj_a(p, c, W, hT):
    if True:
        stg = [p.sb("stg%d" % i, [128, T], BF16) for i in range(2)]
        cnt = [0]

        def cons_fm(dst, scale=None):
            def f(b, tb, ps):
                st = stg[b % 2]
                eng = "act" if (cnt[0] % 2 == 0) else "dve"
                cnt[0] += 1
                if scale is None:
                    p.copy(eng, st[:, tb * 512:(tb + 1) * 512], ps[:, :])
                elif eng == "act":
                    p.act(st[:, tb * 512:(tb + 1) * 512], ps[:, :], AF.Copy, scale=scale)
                else:
                    p.ts("dve", st[:, tb * 512:(tb + 1) * 512], ps[:, :], scale, None, ALU.mult)
                if tb == 3:
                    p.dma("sp", dst[b], st[:, :])
            return f

        proj_stream(p, c, "fm", hT, W["w_q"], cons_fm(c.qT_d, 0.125))
        proj_stream(p, c, "fm", hT, W["w_k"], cons_fm(c.kT_d))
        proj_stream(p, c, "fm", hT, W["w_iq"], cons_fm(c.iqT_d))

        def cons_ik(b, tb, ps):
            p.copy("act", c.ik2[:, 0, tb * 512:(tb + 1) * 512], ps[:, :])
        proj_stream(p, c, "fm", hT, W["w_ik2"], cons_ik)

        vst = [p.sb("vst%d" % i, [128, 512], BF16) for i in range(2)]

        def cons_v(b, tt, ps):
            st = vst[tt % 2]
            p.copy("act" if tt % 2 == 0 else "dve", st[:, :], ps[:, :])
            p.dma("sp", c.v_d[tt * 128:(tt + 1) * 128, :], st[:, :])
        proj_stream(p, c, "tm", hT, W["w_v"], cons_v)

        def cons_iw(b, tt, ps):
            p.copy("dve", c.iw_sb[:, tt, :], ps[:, 0:16])
        proj_stream(p, c, "tm", hT, W["w_iw"], cons_iw)


def layer0_gmlp(p, c, W, hT):
    if True:
        u_all = p.sb("u_all", [128, NT, 512], BF16)
        vn_all = p.sb("vn_all", [128, NT, 512], BF16)
        vgf = [p.sb("vgf%d" % i, [128, 512], F32) for i in range(2)]
        junk = p.sb("junk", [128, 512], BF16)
        ssv = p.sb("ssv", [128, NT], F32)
        rsv = p.sb("rsv", [128, NT], F32)
        gn_bc = p.sb("gn_bc", [128, 512], F32)
        p.dma("sp", gn_bc[:, :], W["gmlp_norm"].partition_broadcast(128))

        def cons_u(b, tt, ps):
            p.act(u_all[:, tt, :], ps[:, :], AF.Gelu_apprx_tanh)
        proj_stream(p, c, "tm", hT, W["w_u"], cons_u)

        def cons_vg(b, tt, ps):
            vg = vgf[tt % 2]
            p.act(vg[:, :], ps[:, :], AF.Gelu_apprx_tanh)
            p.act(junk[:, :], vg[:, :], AF.Square, accum_out=ssv[:, tt:tt + 1])
            p.act(rsv[:, tt:tt + 1], ssv[:, tt:tt + 1], AF.Sqrt, bias=c.eps_ap[:, :], scale=1.0 / 512)
            p.recip(rsv[:, tt:tt + 1], rsv[:, tt:tt + 1])
            p.stt(vn_all[:, tt, :], vg[:, :], rsv[:, tt:tt + 1], gn_bc[:, :], ALU.mult, ALU.mult)
        proj_stream(p, c, "tm", hT, W["w_vg"], cons_vg)

        wsT = p.sb("wsT", [128, 8, 128], BF16)
        p.dma("pool", wsT[:, :, :], W["w_sT"])
        p.memset("dve", wsT[64:128, :, 0:64], 0.0)
        bs8 = p.sb("bs8", [8, 128], BF16)
        p.dma("pool", bs8[:, :], W["b_s"])
        e8 = p.sb("e8", [8, 512], BF16)
        p.dma("pool", e8[:, :], W["e8"])
        ybs = [p.sb("ybs%d" % i, [128, 512], BF16) for i in range(2)]
        ybT = [p.sb("ybT%d" % i, [128, 512], BF16) for i in range(2)]
        for n in range(NT):
            ps = c.ps[c.psi % 8]
            c.psi += 1
            for g in range(8):
                p.mm(ps[:, g * 64:(g + 1) * 64], wsT[:, g, :], vn_all[:, n, g * 64:(g + 1) * 64], start=True, stop=False, inc=False)
                p.mm(ps[:, g * 64:(g + 1) * 64], bs8[:, :], e8[:, g * 64:(g + 1) * 64], start=False, stop=True, inc=(g == 7))
            yb = ybs[n % 2]
            p.tt("dve", yb[:, :], ps[:, :], u_all[:, n, :], ALU.mult)
            pst = c.psb[c.psi % 8]
            pstn = c.ps[c.psi % 8]
            c.psi += 1
            for cc in range(4):
                p.op("pe", lambda e, cc=cc: e.transpose(pst[:, cc * 128:(cc + 1) * 128], yb[:, cc * 128:(cc + 1) * 128], c.ident_b[:, :]),
                     [p.key(yb), p.key(c.ident_b)], [p.key(pstn)], inc=(cc == 3))
            yst = ybT[n % 2]
            p.copy("act", yst[:, :], pst[:, 0:512])
            p.dma("sp", c.yT_d[4:8, :, n * 128:(n + 1) * 128].rearrange("c p t -> p c t"),
                  yst[:, :].rearrange("p (a b) -> p a b", a=4))


def tri(b):
    return b * (b + 1) // 2


def layer0_indexer(p, c, W):
    with p.phase():
        ikn = p.sb("ikn", [128, 1, T], BF16)
        gk2 = p.sb("gk2", [128, 1], F32)
        p.dma("sp", gk2[:, :], W["gk2"])
        with p.phase():
            c.sq = [p.sb("isq%d" % i, [128, 1, 512], BF16) for i in range(2)]
            c.rs = [p.sb("irs%d" % i, [128, 512], F32) for i in range(2)]
            rmsnorm_fm(p, c, gk2, ikn, 0, T, nparts=128, kch=1, src=c.ik2, scale_div=128)
        wabs = p.sb("wabs", [128, NT, 16], F32)
        sgn = p.sb("sgn", [128, NT, 16], F32)
        p.ts("dve", wabs[:, :, :], c.iw_sb[:, :, :], -1.0, None, ALU.mult)
        p.tt("dve", wabs[:, :, :], wabs[:, :, :], c.iw_sb[:, :, :], ALU.max)
        p.ts("dve", sgn[:, :, :], c.iw_sb[:, :, :], 0.0, 2.0, ALU.is_ge, ALU.mult)
        p.ts("dve", sgn[:, :, :], sgn[:, :, :], -1.0, None, ALU.add)
        iqz = [p.sb("iqz%d" % i, [128, 8, 2, 128], BF16) for i in range(2)]
        for i in range(2):
            p.memset("pool", iqz[i][:, :, :, :], 0.0)
        Dsg = [p.sb("Dsg%d" % i, [128, 16, 128], BF16) for i in range(4)]
        acc = [p.sb("acc%d" % i, [128, T], F32) for i in range(4)]
        rr = [p.sb("rr%d" % i, [128, 1024], BF16) for i in range(3)]
        nm = [p.sb("nm%d" % i, [128, T], BF16) for i in range(4)]
        junk = [p.sb("junkc%d" % i, [128, T], BF16) for i in range(2)]
        pow2 = p.sb("pow2", [128, NBIS], F32)
        p.dma("sp", pow2[:, :], W["pow2"])
        st = [p.sb("bis%d" % b, [128, 8], F32) for b in range(NT)]
        wtab = [p.sb("wtab%d" % b, [128, NBIS], F32) for b in range(NT)]
        cnt_ = {"ri": 0, "ei": 0, "si": 0}

        def jobs_for(b):
            L = 128 * (b + 1)
            units = [(0, min(L, 1024))] + ([(1024, L)] if L > 1024 else [])
            return [(b, k0, k1, h) for (k0, k1) in units for h in range(16)]

        def load_iq(b):
            iz = iqz[b % 2]
            for par in range(2):
                p.dma("sp", iz[par * 64:(par + 1) * 64, :, par, :],
                      c.iqT_d[:, par * 64:(par + 1) * 64, b * 128:(b + 1) * 128].rearrange("m p t -> p m t"))

        def emit_score(job):
            b, k0, k1, h = job
            n = k1 - k0
            nb = (n + 511) // 512
            m, par = divmod(h, 2)
            slot = 1 + cnt_["si"] % 3
            cnt_["si"] += 1
            psc = c.psum[:, slot * 1024:slot * 1024 + n]
            for kb in range(nb):
                c0, c1 = kb * 512, min(n, (kb + 1) * 512)
                p.mm(psc[:, c0:c1], iqz[b % 2][:, m, par, :], ikn[:, 0, k0 + c0:k0 + c1])
            return psc

        def emit_rest(job, psc):
            b, k0, k1, h = job
            n = k1 - k0
            nb = (n + 511) // 512
            pacc = c.psum[:, 0:n]
            r = rr[cnt_["ri"] % 3]
            cnt_["ri"] += 1
            p.act(r[:, :n], psc, AF.Relu, scale=wabs[:, b, h:h + 1])
            for kb in range(nb):
                c0, c1 = kb * 512, min(n, (kb + 1) * 512)
                p.mm(pacc[:, c0:c1], Dsg[b % 4][:, h, :], r[:, c0:c1], start=(h == 0), stop=(h == 15))
            if h == 15:
                p.copy("act", acc[b % 4][:, k0:k1], pacc)

        def scores(bs):
            jobs = []
            for b in bs:
                load_iq(b)
                jobs += jobs_for(b)
            psc = emit_score(jobs[0])
            for i, job in enumerate(jobs):
                nxt = emit_score(jobs[i + 1]) if i + 1 < len(jobs) else None
                emit_rest(job, psc)
                psc = nxt

        def build_dsg(b):
            for h in range(16):
                p.ts("dve", Dsg[b % 4][:, h, :], c.ident_b[:, :], sgn[:, b, h:h + 1], None, ALU.mult)

        def bisect(bs):
            S = {}
            for i_, b in enumerate(bs):
                L = 128 * (b + 1)
                ac = acc[b % 4]
                mx, mn, w0, cand, cnt, tq = [st[b][:, i:i + 1] for i in range(6)]
                S[b] = (L, ac, cand, cnt, tq, junk[i_ % 2])
                p.reduce(mx, ac[:, :L], ALU.max)
                p.reduce(mn, ac[:, :L], ALU.min)
                p.memset("dve", ac[0:64, L - 64:L], -1e30)
                p.tt("dve", w0, mx, mn, ALU.subtract)
                p.ts("dve", wtab[b][:, :], pow2[:, :], w0, None, ALU.mult)
                p.tt("dve", cand, mn, wtab[b][:, 0:1], ALU.add)
            for i in range(NBIS):
                last = (i == NBIS - 1)
                for b in bs:
                    L, ac, cand, cnt, tq, jk = S[b]
                    p.ts("dve", jk[:, :L], ac[:, :L], cand, 0.0, ALU.is_ge, ALU.add, accum_out=cnt)
                for b in bs:
                    L, ac, cand, cnt, tq, jk = S[b]
                    p.ts("dve", tq, cnt, float(TOPK), (1.0 if last else 0.5), ALU.is_ge, ALU.subtract)
                for b in bs:
                    L, ac, cand, cnt, tq, jk = S[b]
                    p.stt(cand, tq, wtab[b][:, i:i + 1], cand, ALU.mult, ALU.add)
            for b in bs:
                L, ac, cand, cnt, tq, jk = S[b]
                p.ts("dve", nm[b % 4][:, :L], ac[:, :L], cand, NEG, ALU.is_lt, ALU.mult)

        def transposes(b):
            nmb = nm[b % 4]
            for a0 in range(0, b + 1, 4):
                g = min(4, b + 1 - a0)
                slot = 1 + cnt_["si"] % 3
                cnt_["si"] += 1
                pstn = c.ps[2 * slot]
                pst = c.psb[2 * slot]
                for j in range(g):
                    a = a0 + j
                    p.op("pe", lambda e, j=j, a=a: e.transpose(pst[:, j * 128:(j + 1) * 128], nmb[:, a * 128:(a + 1) * 128], c.ident_b[:, :]),
                         [p.key(nmb), p.key(c.ident_b)], [p.key(pstn)], inc=(j == g - 1))
                o_ap = c.nmT[:, tri(b) + a0:tri(b) + a0 + g, :]
                i_ap = pst[:, 0:g * 128].rearrange("p (a b) -> p a b", a=g)
                p.op("act", lambda e: e.activation(out=o_ap, in_=i_ap, func=AF.Copy), [p.key(pstn)], [p.key(c.nmT, {c.nmT.name: b})])

        for b in range(2):
            L = 128 * (b + 1)
            p.memset("pool", nm[b][:, :L], 0.0)
            p.memset("pool", nm[b][0:64, L - 64:L], NEG)
        prev = [0, 1]
        build_dsg(2)
        build_dsg(3)
        for b0 in range(2, NT, 2):
            bs = [b0, b0 + 1]
            scores(bs)
            for b in prev:
                transposes(b)
            if b0 + 2 < NT:
                build_dsg(b0 + 2)
                build_dsg(b0 + 3)
            bisect(bs)
            prev = bs
        for b in prev:
            transposes(b)


def layer0_attention(p, c, W):
    with p.phase():
        kT = p.sb("kT", [128, 4, T], BF16)
        for m in range(4):
            p.dma("sp", kT[:, m, :], c.kT_d[m])
        qzs = [p.sb("qz%d" % i, [128, 4, 2, 128], BF16) for i in range(2)]
        for i in range(2):
            p.memset("pool", qzs[i][:, :, :, :], 0.0)
        va = p.sb("va", [128, NT, 8, 65], BF16)
        p.memset("pool", va[:, :, :, 64:65], 1.0)
        for tt in range(NT):
            p.dma("sp", va[:, tt, :, 0:64], c.v_d[tt * 128:(tt + 1) * 128, :].rearrange("p (h d) -> p h d", h=8))
        oh1 = p.sb("oh1", [32, 383], F32)
        p.dma("sp", oh1[:, :], W["oh1"])
        rb = p.sb("rb", [32, 8], F32)
        p.dma("sp", rb[:, :], W["rel_bias"])
        cb = p.sb("cb", [128, 8], F32)
        p.dma("sp", cb[:, :], W["rel_bias"][15:16, :].partition_broadcast(128))
        Tb = p.sb("Tb", [128, 2, 8, 128], BF16)
        for kind in range(2):
            for t0 in range(0, 128, 64):
                ps = c.ps[c.psi % 8]
                c.psi += 1
                for t in range(t0, t0 + 64):
                    base = (255 - t) if kind == 0 else (127 - t)
                    p.mm(ps[:, (t - t0) * 8:(t - t0) * 8 + 8], oh1[:, base:base + 128], rb[:, :], start=True, stop=True,
                         inc=(t == t0 + 63))
                p.tt("dve", Tb[:, kind, :, t0:t0 + 64], ps[:, 0:512].rearrange("p (t h) -> p h t", h=8),
                     cb[:, :].unsqueeze(2).to_broadcast([128, 8, 64]), ALU.subtract)
        ya = [p.sb("ya%d" % i, [128, 512], BF16) for i in range(2)]
        PT = [p.sb("PT%d" % i, [128, 1024], BF16) for i in range(3)]
        rec = p.sb("rec", [128, 16], F32)
        yaT = [p.sb("yaT%d" % i, [128, 512], BF16) for i in range(2)]
        si = 0
        pend = []

        def normalize(b, h, po):
            ri = (b * 8 + h) % 16
            p.recip(rec[:, ri:ri + 1], po[:, 64:65])
            p.act(ya[b % 2][:, h * 64:(h + 1) * 64], po[:, 0:64], AF.Copy, scale=rec[:, ri:ri + 1])
            if h == 7:
                pstn = c.ps[6 + b % 2]
                pst = c.psb[6 + b % 2]
                yab = ya[b % 2]
                for cc in range(4):
                    p.op("pe", lambda e, cc=cc: e.transpose(pst[:, cc * 128:(cc + 1) * 128], yab[:, cc * 128:(cc + 1) * 128], c.ident_b[:, :]),
                         [p.key(yab), p.key(c.ident_b)], [p.key(pstn)], inc=(cc == 3))
                yst = yaT[b % 2]
                p.copy("dve", yst[:, :], pst[:, 0:512])
                p.dma("sp", c.yT_d[0:4, :, b * 128:(b + 1) * 128].rearrange("c p t -> p c t"),
                      yst[:, :].rearrange("p (a b) -> p a b", a=4))

        jobs = [(b, h, a0) for b in range(NT) for h in range(8) for a0 in range(0, b + 1, 8)]

        def emit_score(job, idx):
            b, h, a0 = job
            qz = qzs[b % 2]
            if h == 0 and a0 == 0:
                for par in range(2):
                    p.dma("sp", qz[par * 64:(par + 1) * 64, :, par, :],
                          c.qT_d[:, par * 64:(par + 1) * 64, b * 128:(b + 1) * 128].rearrange("m p t -> p m t"))
            m = h // 2
            grp = list(range(a0, min(a0 + 8, b + 1)))
            n = len(grp) * 128
            slot = idx % 2
            ps = c.psum[:, slot * 1024:slot * 1024 + n]
            for j, a in enumerate(grp):
                near = (a >= b - 1)
                lastj = (j == len(grp) - 1)
                o = ps[:, j * 128:(j + 1) * 128]
                p.mm(o, kT[:, m, a * 128:(a + 1) * 128], qz[:, h // 2, h % 2, :], start=True, stop=False, inc=False)
                p.mm(o, c.ident_b[:, :], c.nmT[:, tri(b) + a, :], start=False, stop=(not near), inc=((not near) and lastj))
                if near:
                    kind = 0 if a == b else 1
                    p.mm(o, c.ident_b[:, :], Tb[:, kind, h, :], start=False, stop=True, inc=lastj)
            return ps

        def emit_rest(job, idx, ps):
            nonlocal pend
            b, h, a0 = job
            grp = list(range(a0, min(a0 + 8, b + 1)))
            n = len(grp) * 128
            po = c.ps[4 + (b * 8 + h) % 2]
            pt = PT[idx % 3]
            p.act(pt[:, :n], ps, AF.Exp, bias=cb[:, h:h + 1])
            for j, a in enumerate(grp):
                p.mm(po[:, 0:65], pt[:, j * 128:(j + 1) * 128], va[:, a, h, :], start=(a == 0), stop=(a == b))
            if grp[-1] == b:
                for f in pend:
                    f()
                pend = [lambda b=b, h=h, po=po: normalize(b, h, po)]

        ps = emit_score(jobs[0], 0)
        for i, job in enumerate(jobs):
            nxt = emit_score(jobs[i + 1], i + 1) if i + 1 < len(jobs) else None
            emit_rest(job, i, ps)
            ps = nxt
        for f in pend:
            f()


def out_proj_residual(p, c, w_l, src):
    kch = w_l.shape[2]
    with p.phase():
        c.wb = {"wb": [p.sb("wbo%d" % i, [128, kch, 128], BF16) for i in range(2)]}

        def cons(b, tb, ps):
            p.tt("dve", c.xT[:, b, tb * 512:(tb + 1) * 512], ps[:, :], c.xT[:, b, tb * 512:(tb + 1) * 512], ALU.add)
        proj_stream(p, c, "fm", src, w_l, cons, kch=kch)


def conv_ffn(p, c, W, l):
    w_up = W["w_up%d" % l]
    w_dn = W["w_dn%d" % l]
    with p.phase():
        cw = p.sb("cw", [128, 44, 3], F32)
        cbs = p.sb("cbs", [128, 44], F32)
        p.dma("sp", cw[:, :, :], W["ffn_cw%d" % l])
        p.dma("sp", cbs[:, :], W["ffn_cb%d" % l])
        halo = p.sb("halo", [128, 44, 2], F32)
        for half in range(2):
            t0 = half * 1024
            with p.phase():
                hT = p.sb("hTf", [128, 8, 1024], BF16)
                gT = p.sb("gT", [128, NJ, 1024], BF16)
                with p.phase():
                    c.sq = [p.sb("fsq%d" % i, [128, 8, 512], BF16) for i in range(2)]
                    c.rs = [p.sb("frs%d" % i, [128, 512], F32) for i in range(2)]
                    rmsnorm_fm(p, c, c.g_ffn[l], hT, t0, 1024)
                wu = [[p.sb("wu%d_%d" % (sd, i), [128, 8, 128], BF16) for i in range(2)] for sd in range(2)]
                ya = [p.sb("fya%d" % i, [128, 512], F32) for i in range(3)]
                yb = [p.sb("fyb%d" % i, [128, 512], F32) for i in range(3)]
                sa = [p.sb("fsa%d" % i, [128, 512], F32) for i in range(2)]
                hl = [p.sb("fhl%d" % i, [128, 2], F32) for i in range(4)]
                hli = 0

                def load(j):
                    p.dma("pool", wu[0][j % 2][:, :, :], w_up[j])
                    p.dma("pool", wu[1][j % 2][:, :, :], w_up[NJ + j])

                load(0)
                it = 0
                for j in range(NJ):
                    if j + 1 < NJ:
                        load(j + 1)
                    for tb in range(2):
                        ys = []
                        for sd in range(2):
                            m = sd * NJ + j
                            ps = c.ps[c.psi % 8]
                            c.psi += 1
                            wt = wu[sd][j % 2]
                            for k in range(8):
                                p.mm(ps[:, :], wt[:, k, :], hT[:, k, tb * 512:(tb + 1) * 512], start=(k == 0), stop=(k == 7),
                                     sub={hT.name: tb})
                            y = (ya if sd == 0 else yb)[it % 3]
                            p.act(y[:, :], ps[:, :], AF.Identity, bias=cbs[:, m:m + 1], scale=cw[:, m, 2:3])
                            p.stt(y[:, 1:512], ps[:, 0:511], cw[:, m, 1:2], y[:, 1:512], ALU.mult, ALU.add)
                            p.stt(y[:, 2:512], ps[:, 0:510], cw[:, m, 0:1], y[:, 2:512], ALU.mult, ALU.add)
                            first = (half == 0 and tb == 0)
                            if not first:
                                if tb == 0:
                                    hsrc = halo[:, m, :]
                                else:
                                    hsrc = hprev[sd][:, :]
                                p.stt(y[:, 0:1], hsrc[:, 1:2], cw[:, m, 1:2], y[:, 0:1], ALU.mult, ALU.add)
                                p.stt(y[:, 0:2], hsrc[:, 0:2], cw[:, m, 0:1], y[:, 0:2], ALU.mult, ALU.add)
                            if tb == 0:
                                if sd == 0:
                                    hprev = [None, None]
                                hprev[sd] = hl[hli % 4]
                                hli += 1
                                p.copy("act", hprev[sd][:, :], ps[:, 510:512])
                            elif half == 0:
                                p.copy("act", halo[:, m, :], ps[:, 510:512])
                            ys.append(y)
                        sg = sa[it % 2]
                        p.act(sg[:, :], ys[0][:, :], AF.Silu)
                        p.tt("pool", gT[:, j, tb * 512:(tb + 1) * 512], sg[:, :], ys[1][:, :], ALU.mult)
                        it += 1
                wd = [p.sb("wd%d" % i, [128, NJ, 128], BF16) for i in range(2)]
                p.dma("pool", wd[0][:, :, :], w_dn[0])
                for dc in range(8):
                    if dc + 1 < 8:
                        p.dma("pool", wd[(dc + 1) % 2][:, :, :], w_dn[dc + 1])
                    for tb in range(2):
                        ps = c.ps[c.psi % 8]
                        c.psi += 1
                        for j in range(NJ):
                            p.mm(ps[:, :], wd[dc % 2][:, j, :], gT[:, j, tb * 512:(tb + 1) * 512], start=(j == 0), stop=(j == NJ - 1))
                        sl = c.xT[:, dc, t0 + tb * 512:t0 + (tb + 1) * 512]
                        p.tt("dve", sl, ps[:, :], sl, ALU.add)


def hgrn_mixer(p, c, W):
    with p.phase():
        hT = p.sb("hT1", [128, 8, T], BF16)
        c.wb = {"wb": [p.sb("wbh%d" % i, [128, 8, 512], BF16) for i in range(2)]}
        lbr = p.sb("lbr", [128, 2, 8], F32)
        p.dma("sp", lbr[:, :, :], W["hgrn_lb"])
        lb = p.sb("lb", [128, 8], F32)
        oml = p.sb("oml", [128, 8], F32)
        p.tt("dve", lb[:, :], lbr[:, 1, :], lbr[:, 0, :], ALU.subtract)
        p.act(lb[:, :], lb[:, :], AF.Sigmoid)
        p.ts("dve", oml[:, :], lb[:, :], -1.0, 1.0, ALU.mult, ALU.add)
        with p.phase():
            c.sq = [p.sb("hsq%d" % i, [128, 8, 512], BF16) for i in range(2)]
            c.rs = [p.sb("hrs%d" % i, [128, 512], F32) for i in range(2)]
            rmsnorm_fm(p, c, c.g_mix[1], hT, 0, T)
        stg = [p.sb("hstg%d" % i, [128, T], BF16) for i in range(2)]

        def cons_silu(dst):
            def f(b, tb, ps):
                st = stg[b % 2]
                p.act(st[:, tb * 512:(tb + 1) * 512], ps[:, :], AF.Silu)
                if tb == 3:
                    p.dma("sp", dst[b], st[:, :])
            return f
        proj_stream(p, c, "fm", hT, W["w_cq"], cons_silu(c.qh_d))
        proj_stream(p, c, "fm", hT, W["w_cg"], cons_silu(c.gate_d))

        gst = [p.sb("gst%d" % i, [128, T], F32) for i in range(2)]
        sgt = [p.sb("sgt%d" % i, [128, 512], F32) for i in range(2)]

        def cons_f(b, tb, ps):
            g = gst[b % 2]
            sg = sgt[tb % 2]
            p.act(sg[:, :], ps[:, :], AF.Sigmoid)
            p.ts("dve", g[:, tb * 512:(tb + 1) * 512], sg[:, :], oml[:, b:b + 1], lb[:, b:b + 1], ALU.mult, ALU.add)
            if tb == 3:
                st = stg[b % 2]
                p.ts("dve", st[:, :], g[:, :], -1.0, 1.0, ALU.mult, ALU.add)
                p.dma("sp", c.kk_d[b], st[:, :])
                p.act(g[:, :], g[:, :], AF.Ln)
                p.dma("sp", c.lg_d[b], g[:, :])
        proj_stream(p, c, "fm", hT, W["w_cf"], cons_f)

        ist = [p.sb("ist%d" % i, [128, 512], BF16) for i in range(2)]

        def cons_i(b, tt, ps):
            st = ist[tt % 2]
            p.copy("act" if tt % 2 == 0 else "dve", st[:, :], ps[:, :])
            p.dma("sp", c.i_d[tt * 128:(tt + 1) * 128, b * 512:(b + 1) * 512], st[:, :])
        proj_stream(p, c, "tm", hT, W["w_ci"], cons_i)
    if c.stop_after == "hproj":
        return
    with p.phase():
        cm = p.sb("cm", [128, 32, 64], BF16)
        p.memset("pool", cm[:, :, :], 1.0)
        p.memset("pool", cm[:, :, 0:1], 0.0)
        tri64 = p.sb("tri64", [64, 64], F32)
        p.dma("sp", tri64[:, :], W["tri64"])
        cn = p.sb("cn", [128, 1], F32)
        p.dma("sp", cn[:, :], W["c_out_norm"])
        lg = p.sb("lg", [128, T], F32)
        bb = p.sb("bb", [128, T], F32)
        eb = p.sb("eb", [128, T], F32)
        t1 = p.sb("t1", [128, T], F32)
        qh = p.sb("qh", [128, T], BF16)
        kk = p.sb("kk", [128, T], BF16)
        KlT = p.sb("KlT", [128, T], BF16)
        Qb = [p.sb("Qb%d" % i, [128, T], BF16) for i in range(2)]
        Sb = [p.sb("Sb%d" % i, [128, 128], BF16) for i in range(2)]
        Kbb = [p.sb("Kbb%d" % i, [128, T], BF16) for i in range(2)]
        Klc = [p.sb("Klc%d" % i, [64, 32, 128], BF16) for i in range(2)]
        ic = [p.sb("ic%d" % i, [64, 32, 128], BF16) for i in range(2)]
        ebl = [p.sb("ebl%d" % i, [128, 32], F32) for i in range(2)]
        gt = [p.sb("gt%d" % i, [128, T], BF16) for i in range(2)]
        oTt = [p.sb("oTt%d" % i, [128, 512], F32) for i in range(2)]
        Sf = [p.sb("Sf%d" % i, [128, 128], F32) for i in range(2)]
        ATs = [p.sb("ATs%d" % i, [64, 64], BF16) for i in range(3)]
        osq = [p.sb("osq%d" % i, [128, 512], BF16) for i in range(2)]
        ors = [p.sb("ors%d" % i, [128, 512], F32) for i in range(2)]
        on = [p.sb("on%d" % i, [128, 512], F32) for i in range(2)]
        ost = [p.sb("ost%d" % i, [128, 512], BF16) for i in range(2)]

        def precompute_steps(h):
            u = h % 2
            ebv = eb[:, :].rearrange("p (a b) -> p a b", b=64)
            st = []

            def loads():
                p.dma("sp", lg[:, :], c.lg_d[h])
                p.dma("sp", qh[:, :], c.qh_d[h])
                p.dma("sp", kk[:, :], c.kk_d[h])
                p.dma("sp", gt[u][:, :], c.gate_d[h])
                p.dma("sp", ic[u][:, :, :], c.i_d[:, h * 128:(h + 1) * 128].rearrange("(n s) e -> s n e", s=64))
            st.append(loads)
            st.append(lambda: p.scan(bb[:, :], cm[:, :, :].rearrange("p a b -> p (a b)"), lg[:, :], 0.0, ALU.mult, ALU.add))
            st.append(lambda: p.act(eb[:, :], bb[:, :], AF.Exp))
            st.append(lambda: p.act(t1[:, :], bb[:, :], AF.Exp, scale=-1.0))
            st.append(lambda: p.tt("pool", Qb[u][:, :], qh[:, :], eb[:, :], ALU.mult))
            st.append(lambda: p.tt("pool", t1[:, :], kk[:, :], t1[:, :], ALU.mult))
            st.append(lambda: p.copy("act", ebl[u][:, :], ebv[:, :, 63]))
            st.append(lambda: p.copy("act", Kbb[u][:, :], t1[:, :]))
            st.append(lambda: p.tt("pool", KlT[:, :].rearrange("p (a b) -> p a b", b=64), t1[:, :].rearrange("p (a b) -> p a b", b=64),
                                   ebv[:, :, 63:64].to_broadcast([128, 32, 64]), ALU.mult))

            def trs(g8):
                pstn = c.ps[7]
                pst = c.psb[7]
                for j in range(8):
                    n = g8 * 8 + j
                    p.op("pe", lambda e, j=j, n=n: e.transpose(pst[0:64, j * 128:(j + 1) * 128], KlT[:, n * 64:(n + 1) * 64], c.ident_b[:, :]),
                         [p.key(KlT), p.key(c.ident_b)], [p.key(pstn)], inc=(j == 7))
                p.op("act", lambda e: e.activation(out=Klc[u][:, g8 * 8:(g8 + 1) * 8, :],
                                                   in_=pst[0:64, :].rearrange("p (a b) -> p a b", a=8), func=AF.Copy),
                     [p.key(pstn)], [p.key(Klc[u])])
            for g8 in range(4):
                st.append(lambda g8=g8: trs(g8))
            return st

        def chunkloop(h, bg):
            u = h % 2

            def front(n):
                cs = slice(n * 64, (n + 1) * 64)
                if n < 31:
                    psS = c.ps[4 + n % 3]
                    p.mm(psS[:, 0:128], Klc[u][:, n, :], ic[u][:, n, :])
                psA = c.ps[n % 2]
                p.mm(psA[0:64, 0:64], Kbb[u][:, cs], Qb[u][:, cs])
                p.tt("dve", ATs[n % 3][:, :], psA[0:64, 0:64], tri64[:, :], ALU.mult)

            def outnorm_steps(tb, po):
                ts_ = slice(tb * 512, (tb + 1) * 512)
                oT = oTt[tb % 2]
                sq = osq[tb % 2]
                rs = ors[tb % 2]
                o2 = on[tb % 2]
                o3 = ost[tb % 2]
                ps = c.ps[7]

                def s1():
                    p.copy("act", oT[:, :], po[:, :])
                    p.act(sq[:, :], oT[:, :], AF.Square)

                def s2():
                    p.mm(ps[:, :], c.ones_b[:, :], sq[:, :])

                def s3():
                    p.act(rs[:, :], ps[:, :], AF.Ln, bias=c.eps_ap[:, :], scale=1.0 / 128)
                    p.act(rs[:, :], rs[:, :], AF.Exp, scale=-0.5)
                    p.stt(o2[:, :], oT[:, :], cn[:, 0:1], rs[:, :], ALU.mult, ALU.mult)
                    p.tt("pool", o3[:, :], o2[:, :], gt[u][:, ts_], ALU.mult)
                    p.dma("pool", c.oT_d[h][:, ts_], o3[:, :])
                return [s1, s2, s3]

            front(0)
            po = None
            for n in range(32):
                cs = slice(n * 64, (n + 1) * 64)
                if n + 1 < 32:
                    front(n + 1)
                if n % 8 == 0:
                    po = c.ps[2 + (n // 8) % 2]
                oc = po[:, (n % 8) * 64:(n % 8 + 1) * 64]
                p.mm(oc, ic[u][:, n, :], ATs[n % 3][:, :], start=True, stop=(n == 0), inc=(n == 0))
                if n > 0:
                    p.mm(oc, Sb[(n - 1) % 2][:, :], Qb[u][:, cs], start=False, stop=True, inc=True)
                if n < 31:
                    psS = c.ps[4 + n % 3]
                    if n == 0:
                        p.copy("dve", Sf[0][:, :], psS[:, 0:128])
                    else:
                        p.stt(Sf[n % 2][:, :], Sf[(n - 1) % 2][:, :], ebl[u][:, n:n + 1], psS[:, 0:128], ALU.mult, ALU.add)
                    p.copy("act", Sb[n % 2][:, :], Sf[n % 2][:, :])
                if n % 8 == 7:
                    for f in reversed(outnorm_steps(n // 8, po)):
                        bg.insert(0, f)
                if bg:
                    bg.pop(0)()
            while bg:
                bg.pop(0)()

        for f in precompute_steps(0):
            f()
        for h in range(8):
            bg = precompute_steps(h + 1) if h + 1 < 8 else []
            chunkloop(h, bg)
    if c.stop_after == "hrec":
        return
    with p.phase():
        oTa = p.sb("oTa", [128, 8, T], BF16)
        for k in range(8):
            p.dma("sp", oTa[:, k, :], c.oT_d[k])
        out_proj_residual(p, c, W["w_out1"], oTa)


def final_norm(p, c, out):
    with p.phase():
        c.sq = [p.sb("nsq%d" % i, [128, 8, 512], BF16) for i in range(2)]
        c.rs = [p.sb("nrs%d" % i, [128, 512], F32) for i in range(2)]
        ot = [p.sb("fot%d" % i, [128, 8, 512], F32) for i in range(2)]
        for i, tb in enumerate(range(0, T, 512)):
            sq = c.sq[i % 2]
            p.act(sq[:, :, :], c.xT[:, :, tb:tb + 512], AF.Square)
            ps = c.ps[c.psi % 8]
            c.psi += 1
            for k in range(8):
                p.mm(ps[:, :], c.ones_b[:, :], sq[:, k, :], start=(k == 0), stop=(k == 7))
            rs = c.rs[i % 2]
            p.act(rs[:, :], ps[:, :], AF.Ln, bias=c.eps_ap[:, :], scale=1.0 / D)
            p.act(rs[:, :], rs[:, :], AF.Exp, scale=-0.5)
            o = ot[i % 2]
            for k in range(8):
                p.stt(o[:, k, :], c.xT[:, k, tb:tb + 512], c.g_fin[:, k:k + 1], rs[:, :], ALU.mult, ALU.mult)
            p.dma("sp", out[:, :, tb:tb + 512].rearrange("c p t -> p c t"), o[:, :, :])

def lay(Wm, cw):
    K, N = Wm.shape
    return np.ascontiguousarray(Wm.reshape(K // 128, 128, N // cw, cw).transpose(2, 1, 0, 3))


def build(stop_after=None, dbg=()):
    nc = bass.Bass("TRN2", target_bir_lowering=False)
    p = Prog(nc)
    c = Ctx()
    W = {}

    def din(name, shape, dt=F32):
        W[name] = nc.dram_tensor(name, list(shape), dt, kind="ExternalInput").ap()
        return W[name]

    def dscr(name, shape, dt):
        kind = "ExternalOutput" if name in dbg else "Internal"
        return nc.dram_tensor(name, list(shape), dt, kind=kind).ap()

    din("xT_in", [8, 128, T])
    din("ident", [128, 128])
    din("g_mix", [2, 128, 8]); din("g_ffn", [2, 128, 8]); din("g_fin", [128, 8])
    din("w_q", [4, 128, 8, 128]); din("w_k", [4, 128, 8, 128]); din("w_iq", [8, 128, 8, 128])
    din("w_ik2", [1, 128, 8, 128]); din("w_v", [1, 128, 8, 512]); din("w_iw", [1, 128, 8, 16])
    din("w_u", [1, 128, 8, 512]); din("w_vg", [1, 128, 8, 512])
    din("gmlp_norm", [1, 512]); din("w_sT", [128, 8, 128]); din("b_s", [8, 128]); din("e8", [8, 512])
    din("gk2", [128, 1]); din("pow2", [128, NBIS]); din("oh1", [32, 383]); din("rel_bias", [32, 8])
    din("w_out0", [8, 128, 8, 128])
    for l in range(2):
        din("w_up%d" % l, [44, 128, 8, 128]); din("w_dn%d" % l, [8, 128, NJ, 128])
        din("ffn_cw%d" % l, [128, 44, 3]); din("ffn_cb%d" % l, [128, 44])
    din("w_cq", [8, 128, 8, 128]); din("w_cf", [8, 128, 8, 128]); din("w_cg", [8, 128, 8, 128]); din("w_ci", [2, 128, 8, 512])
    din("hgrn_lb", [128, 2, 8]); din("tri64", [64, 64]); din("c_out_norm", [128, 1]); din("w_out1", [8, 128, 8, 128])
    out = nc.dram_tensor("outT", [8, 128, T], F32, kind="ExternalOutput").ap()

    c.qT_d = dscr("qT_d", [4, 128, T], BF16)
    c.kT_d = dscr("kT_d", [4, 128, T], BF16)
    c.iqT_d = dscr("iqT_d", [8, 128, T], BF16)
    c.v_d = dscr("v_d", [T, 512], BF16)
    c.yT_d = dscr("yT_d", [8, 128, T], BF16)
    c.qh_d = dscr("qh_d", [8, 128, T], BF16)
    c.gate_d = dscr("gate_d", [8, 128, T], BF16)
    c.kk_d = dscr("kk_d", [8, 128, T], BF16)
    c.lg_d = dscr("lg_d", [8, 128, T], F32)
    c.i_d = dscr("i_d", [T, 1024], BF16)
    c.oT_d = dscr("oT_d", [8, 128, T], BF16)

    c.xT = p.sb("xT", [128, 8, T], F32, True)
    c.ident_f = p.sb("ident_f", [128, 128], F32, True)
    c.ident_b = p.sb("ident_b", [128, 128], BF16, True)
    c.ones_b = p.sb("ones_b", [128, 128], BF16, True)
    c.eps_ap = p.sb("eps_ap", [128, 1], F32, True)
    gm = p.sb("g_mix_sb", [128, 2, 8], F32, True)
    gf = p.sb("g_ffn_sb", [128, 2, 8], F32, True)
    gfin = p.sb("g_fin_sb", [128, 8], F32, True)
    c.psum = p.es.enter_context(nc.psum_tensor("psum", [128, 4096], F32))
    c.ps = [c.psum[:, i * 512:(i + 1) * 512] for i in range(8)]
    c.psb = [t.bitcast(BF16) for t in c.ps]
    c.psi = 0
    c.stop_after = stop_after
    c.dbg = dbg
    c.dscr = dscr

    p.dma("sp", c.ident_f[:, :], W["ident"])
    p.dma("pool", c.ident_b[:, :], W["ident"])
    p.memset("dve", c.ones_b[:, :], 1.0)
    p.memset("dve", c.eps_ap[:, :], EPS)
    for l in range(2):
        p.dma("sp", gm[:, l, :], W["g_mix"][l])
        p.dma("sp", gf[:, l, :], W["g_ffn"][l])
    p.dma("sp", gfin[:, :], W["g_fin"])
    for tb in range(4):
        p.dma("sp", c.xT[:, :, tb * 512:(tb + 1) * 512],
              W["xT_in"][:, :, tb * 512:(tb + 1) * 512].rearrange("k p t -> p k t"), sub={c.xT.name: tb})
    c.g_mix = [gm[:, l, :] for l in range(2)]
    c.g_ffn = [gf[:, l, :] for l in range(2)]
    c.g_fin = gfin

    layer0_mixer(p, c, W)
    stages = ["gmlp", "indexer", "attn", "mix0", "ffn0", "hproj", "hrec", "mix1", "ffn1", None]
    si_ = stages.index(stop_after)
    if si_ >= stages.index("ffn0"):
        conv_ffn(p, c, W, 0)
    if si_ >= stages.index("hproj"):
        hgrn_mixer(p, c, W)
    if si_ >= stages.index("ffn1"):
        conv_ffn(p, c, W, 1)
    if stop_after is None:
        final_norm(p, c, out)
        p.barrier()
        return nc, p, c

    for k in range(8):
        p.dma("sp", out[k], c.xT[:, k, :])
    p.barrier()
    return nc, p, c


def t5_onehot():
    rel = np.arange(-255, 128)
    half, max_exact = 16, 8
    ret = np.where(rel > 0, half, 0)
    n = np.abs(rel)
    nf = np.maximum(n, max_exact).astype(np.float32)
    large = max_exact + (np.log(nf / np.float32(max_exact)) / np.float32(np.log(128 / 8)) * np.float32(half - max_exact)).astype(np.int32)
    large = np.minimum(large, half - 1)
    bucket = ret + np.where(n < max_exact, n, large)
    oh = np.zeros((32, 383), np.float32)
    oh[bucket, np.arange(383)] = 1.0
    return oh


def host_inputs(inp, b):
    f = np.float32
    m = {}
    x = inp["x"][b]
    m["xT_in"] = np.ascontiguousarray(x.T.reshape(8, 128, T))
    m["ident"] = np.eye(128, dtype=f)
    m["g_mix"] = np.ascontiguousarray(inp["mix_norm"].reshape(2, 8, 128).transpose(0, 2, 1))
    m["g_ffn"] = np.ascontiguousarray(inp["ffn_norm"].reshape(2, 8, 128).transpose(0, 2, 1))
    m["g_fin"] = np.ascontiguousarray(inp["final_norm"].reshape(8, 128).T)
    Wi = inp["ab_w_in"][0]
    m["w_q"] = lay(Wi[:, 0:512], 128)
    m["w_k"] = lay(Wi[:, 512:1024], 128)
    m["w_v"] = lay(Wi[:, 1024:1536], 512)
    m["w_iq"] = lay(Wi[:, 1536:2560], 128)
    m["w_ik2"] = lay(np.concatenate([Wi[:, 2560:2624], Wi[:, 2560:2624]], axis=1), 128)
    m["w_iw"] = lay(Wi[:, 2624:2640], 16)
    m["w_u"] = lay(Wi[:, 2640:3152], 512)
    m["w_vg"] = lay(Wi[:, 3152:3664], 512)
    m["gmlp_norm"] = np.ascontiguousarray(inp["ab_gmlp_norm"][0].reshape(1, 512))
    m["w_sT"] = np.ascontiguousarray(inp["ab_w_s"][0].transpose(2, 0, 1))
    m["b_s"] = np.ascontiguousarray(inp["ab_b_s"][0])
    e8 = np.zeros((8, 512), f)
    for g in range(8):
        e8[g, g * 64:(g + 1) * 64] = 1.0
    m["e8"] = e8
    m["gk2"] = np.concatenate([inp["ab_idx_k_norm"][0], inp["ab_idx_k_norm"][0]]).reshape(128, 1)
    m["pow2"] = np.tile((0.5 ** np.arange(1, NBIS + 1)).astype(f)[None, :], (128, 1))
    m["oh1"] = t5_onehot()
    m["rel_bias"] = inp["rel_bias"]
    m["w_out0"] = lay(inp["ab_w_out"][0], 128)
    Wc = inp["c_w_in"][0]
    m["w_cq"] = lay(Wc[:, 0:1024], 128)
    m["w_cf"] = lay(Wc[:, 1024:2048], 128)
    m["w_ci"] = lay(Wc[:, 2048:3072], 512)
    m["w_cg"] = lay(Wc[:, 3072:4096], 128)
    m["hgrn_lb"] = inp["hgrn_lb"].reshape(2, 8, 128).transpose(2, 0, 1)
    m["tri64"] = np.triu(np.ones((64, 64), f))
    m["c_out_norm"] = inp["c_out_norm"][0].reshape(128, 1)
    m["w_out1"] = lay(inp["c_w_out"][0], 128)
    for l in range(2):
        m["w_up%d" % l] = lay(inp["ffn_w_up"][l], 128)
        m["w_dn%d" % l] = lay(inp["ffn_w_down"][l], 128)
        m["ffn_cw%d" % l] = inp["ffn_conv_w"][l].reshape(3, 44, 128).transpose(2, 1, 0)
        m["ffn_cb%d" % l] = inp["ffn_conv_b"][l].reshape(44, 128).T
    return {k: np.ascontiguousarray(v, dtype=f) for k, v in m.items()}


def kernel(**inputs):
    inp = {k: np.asarray(v) for k, v in inputs.items()}
    nc, p, c = build()
    in_maps = [host_inputs(inp, b) for b in range(8)]
    res = run_bass_kernel_spmd(nc, in_maps, core_ids=list(range(8)))
    outs = [np.asarray(r["outT"]).reshape(D, T).T for r in res.results]
    return np.stack(outs, axis=0).astype(np.float32)
```

```python
import numpy as np
from contextlib import ExitStack, contextmanager
import concourse.bass as bass
import concourse.mybir as mybir
from concourse.bass_utils import run_bass_kernel_spmd

F32 = mybir.dt.float32
BF16 = mybir.dt.bfloat16
ALU = mybir.AluOpType
AF = mybir.ActivationFunctionType
AX = mybir.AxisListType

T = 2048
D = 1024
NT = 16
EPS = 1e-6
D_FF = 2816
NJ = 22
TOPK = 256
NBIS = 14
NEG = -30000.0


class Tok:
    __slots__ = ("sem", "key", "val", "clock")

    def __init__(self, sem, key, val, clock):
        self.sem, self.key, self.val, self.clock = sem, key, val, clock


class Reg:
    __slots__ = ("w", "r")

    def __init__(self):
        self.w = None
        self.r = {}


class Prog:
    def __init__(self, nc):
        self.nc = nc
        self.es = ExitStack()
        self.E = {"pe": nc.tensor, "act": nc.scalar, "dve": nc.vector, "pool": nc.gpsimd, "sp": nc.sync}
        self.csem = {e: self.es.enter_context(nc.semaphore("cs_" + e)) for e in ("pe", "act", "dve", "pool")}
        self.ccnt = {e: 0 for e in self.csem}
        self.known = {e: {} for e in self.E}
        self.dq = {}
        for q, n in (("sp", 24), ("pool", 16)):
            self.dq[q] = dict(
                sems=[self.es.enter_context(nc.semaphore("d_%s%d" % (q, i))) for i in range(n)],
                use=[0] * n, last=[None] * n, nxt=0)
        self.regs = {}
        self.last = {e: None for e in self.csem}
        self.phase_stack = None
        self.ninst = 0
        self.nwait = 0
        self.nsb = 0

    @staticmethod
    def key(x, sub=None):
        if isinstance(x, (tuple, list)):
            return x
        if isinstance(x, str):
            return (x, None)
        name = x.tensor.name if hasattr(x, "tensor") else x.name
        if name == "psum":
            sz = mybir.dt.size(x.dtype)
            apl = list(x.ap)
            off = (x.offset % apl[0][0]) * sz
            ext = sum((cnt - 1) * abs(st) for st, cnt in apl[1:]) * sz + sz
            return [("psum", bk) for bk in range(off // 512, (off + ext - 1) // 512 + 1)]
        if sub and name in sub:
            return (name, sub[name])
        return (name, None)

    def _targets(self, k):
        name, s = k
        d = self.regs.setdefault(name, {})
        if s is None:
            if None not in d:
                d[None] = Reg()
            return list(d.values())
        if s not in d:
            d[s] = Reg()
        out = [d[s]]
        if None in d:
            out.append(d[None])
        return out

    def _wait(self, eng, tok):
        if tok is None:
            return
        if eng == "pe" and tok.key == "pe":
            return
        k = self.known[eng]
        if k.get(tok.key, 0) >= tok.val:
            return
        if tok.key in self.ccnt:
            assert tok.val <= self.ccnt[tok.key], ("wait on future inc", eng, tok.key, tok.val)
        self.E[eng].wait_ge(tok.sem, tok.val)
        self.nwait += 1
        for kk, vv in tok.clock.items():
            if k.get(kk, 0) < vv:
                k[kk] = vv

    def _sync(self, eng, reads, writes):
        for k in reads:
            for rg in self._targets(k):
                self._wait(eng, rg.w)
        for k in writes:
            for rg in self._targets(k):
                self._wait(eng, rg.w)
                for t in list(rg.r.values()):
                    self._wait(eng, t)

    def _record(self, tok, reads, writes):
        for k in reads:
            name, s = k
            rg = self.regs[name][s]
            old = rg.r.get(tok.key)
            if old is None or old.val < tok.val:
                rg.r[tok.key] = tok
        for k in writes:
            name, s = k
            rg = self.regs[name][s]
            rg.w = tok
            rg.r = {}

    def _flat(self, ks):
        out = []
        for k in ks:
            k = self.key(k)
            if isinstance(k, list):
                out += k
            else:
                out.append(k)
        return out

    def op(self, eng, fn, reads, writes, inc=True):
        reads = self._flat(reads)
        writes = self._flat(writes)
        self._sync(eng, reads, writes)
        inst = fn(self.E[eng])
        self.ninst += 1
        if inc:
            self.ccnt[eng] += 1
            inst.then_inc(self.csem[eng], 1)
            val = self.ccnt[eng]
        else:
            val = self.ccnt[eng] + 1
        clock = dict(self.known[eng])
        clock[eng] = max(clock.get(eng, 0), val)
        tok = Tok(self.csem[eng], eng, val, clock)
        self.last[eng] = tok
        self._record(tok, reads, writes)
        return tok

    def dma(self, q, out, in_, sub=None):
        reads = self._flat([self.key(in_, sub)])
        writes = self._flat([self.key(out, sub)])
        self._sync(q, reads, writes)
        d = self.dq[q]
        j = d["nxt"]
        d["nxt"] = (j + 1) % len(d["sems"])
        self._wait(q, d["last"][j])
        inst = self.E[q].dma_start(out=out, in_=in_)
        self.ninst += 1
        d["use"][j] += 1
        inst.then_inc(d["sems"][j], 16)
        key = ("d", q, j)
        val = 16 * d["use"][j]
        clock = dict(self.known[q])
        clock[key] = val
        tok = Tok(d["sems"][j], key, val, clock)
        d["last"][j] = tok
        self._record(tok, reads, writes)
        return tok

    def barrier(self):
        toks = [t for t in self.last.values() if t is not None]
        for d in self.dq.values():
            toks += [t for t in d["last"] if t is not None]
        for e in self.E:
            for t in toks:
                self._wait(e, t)

    def sb(self, name, shape, dtype, persistent=False):
        st = self.es if (persistent or self.phase_stack is None) else self.phase_stack
        self.nsb += 1
        return st.enter_context(self.nc.sbuf_tensor("s%d_%s" % (self.nsb, name), list(shape), dtype))

    @contextmanager
    def phase(self):
        prev = self.phase_stack
        self.phase_stack = ExitStack()
        try:
            yield
            self.barrier()
        finally:
            self.phase_stack.close()
            self.phase_stack = prev

    def _aps(self, sub, *xs):
        return [self.key(x, sub) for x in xs if x is not None and not isinstance(x, (int, float))]

    def mm(self, out, lhsT, rhs, start=True, stop=True, inc=None, sub=None):
        if inc is None:
            inc = stop
        return self.op("pe", lambda e: e.matmul(out, lhsT, rhs, start=start, stop=stop),
                       self._aps(sub, lhsT, rhs), self._aps(sub, out), inc=inc)

    def tr(self, out, in_, ident, inc=True, sub=None):
        return self.op("pe", lambda e: e.transpose(out, in_, ident),
                       self._aps(sub, in_, ident), self._aps(sub, out), inc=inc)

    def act(self, out, in_, func, bias=None, scale=None, accum_out=None, sub=None):
        kw = {}
        if bias is not None:
            kw["bias"] = bias
        if scale is not None:
            kw["scale"] = scale
        if accum_out is not None:
            kw["accum_out"] = accum_out
        return self.op("act", lambda e: e.activation(out=out, in_=in_, func=func, **kw),
                       self._aps(sub, in_, bias, scale), self._aps(sub, out, accum_out))

    def ts(self, eng, out, in0, s1, s2, op0, op1=None, accum_out=None, sub=None):
        kw = {}
        if op1 is not None:
            kw["op1"] = op1
        if accum_out is not None:
            kw["accum_out"] = accum_out
        return self.op(eng, lambda e: e.tensor_scalar(out=out, in0=in0, scalar1=s1, scalar2=s2, op0=op0, **kw),
                       self._aps(sub, in0, s1, s2), self._aps(sub, out, accum_out))

    def stt(self, out, in0, scalar, in1, op0, op1, sub=None):
        return self.op("dve", lambda e: e.scalar_tensor_tensor(out=out, in0=in0, scalar=scalar, in1=in1, op0=op0, op1=op1),
                       self._aps(sub, in0, scalar, in1), self._aps(sub, out))

    def tt(self, eng, out, in0, in1, op, sub=None):
        return self.op(eng, lambda e: e.tensor_tensor(out=out, in0=in0, in1=in1, op=op),
                       self._aps(sub, in0, in1), self._aps(sub, out))

    def copy(self, eng, out, in_, sub=None):
        if eng == "act":
            return self.act(out, in_, AF.Copy, sub=sub)
        return self.op(eng, lambda e: e.tensor_copy(out=out, in_=in_), self._aps(sub, in_), self._aps(sub, out))

    def memset(self, eng, ap, val, sub=None):
        return self.op(eng, lambda e: e.memset(ap, val), [], self._aps(sub, ap))

    def recip(self, out, in_, sub=None):
        return self.op("dve", lambda e: e.reciprocal(out=out, in_=in_), self._aps(sub, in_), self._aps(sub, out))

    def reduce(self, out, in_, op, sub=None):
        return self.op("dve", lambda e: e.tensor_reduce(out=out, in_=in_, axis=AX.X, op=op),
                       self._aps(sub, in_), self._aps(sub, out))

    def scan(self, out, d0, d1, initial, op0, op1, sub=None):
        return self.op("dve", lambda e: e.tensor_tensor_scan(out=out, data0=d0, data1=d1, initial=initial, op0=op0, op1=op1),
                       self._aps(sub, d0, d1), self._aps(sub, out))


class Ctx:
    pass


def rmsnorm_fm(p, c, g_ap, hT, t0, nt, nparts=128, kch=8, src=None, scale_div=None):
    src = c.xT if src is None else src
    div = float(scale_div if scale_div is not None else nparts * kch)
    for i, tb in enumerate(range(t0, t0 + nt, 512)):
        sq = c.sq[i % 2]
        sub = {hT.name: (tb - t0) // 512}
        if src is c.xT:
            sub[c.xT.name] = tb // 512
        p.act(sq[:nparts, :kch, :], src[:nparts, :kch, tb:tb + 512], AF.Square, sub=sub)
        ps = c.ps[c.psi % 8]
        c.psi += 1
        for k in range(kch):
            p.mm(ps[:nparts, :], c.ones_b[:nparts, :nparts], sq[:nparts, k, :], start=(k == 0), stop=(k == kch - 1))
        rs = c.rs[i % 2]
        p.act(rs[:nparts, :], ps[:nparts, :], AF.Ln, bias=c.eps_ap[:nparts, :], scale=1.0 / div)
        p.act(rs[:nparts, :], rs[:nparts, :], AF.Exp, scale=-0.5)
        for k in range(kch):
            p.stt(hT[:nparts, k, tb - t0:tb - t0 + 512], src[:nparts, k, tb:tb + 512], g_ap[:nparts, k:k + 1],
                  rs[:nparts, :], ALU.mult, ALU.mult, sub=sub)


def proj_multi(p, c, hT, jobs, kch=8, wname="wb"):
    ntok = hT.shape[2]
    wbs = c.wb[wname]
    flat = [(ji, b) for ji, (mode, w_l, consume) in enumerate(jobs) for b in range(w_l.shape[0])]

    def load(i):
        ji, b = flat[i]
        w_l = jobs[ji][1]
        cw = w_l.shape[3]
        p.dma("pool", wbs[i % 2][:, :kch, :cw], w_l[b])

    load(0)
    for i, (ji, b) in enumerate(flat):
        mode, w_l, consume = jobs[ji]
        cw = w_l.shape[3]
        if i + 1 < len(flat):
            load(i + 1)
        wb = wbs[i % 2]
        if mode == "fm":
            for tb in range(ntok // 512):
                ps = c.ps[c.psi % 8]
                c.psi += 1
                for k in range(kch):
                    p.mm(ps[:cw, :], wb[:, k, :cw], hT[:, k, tb * 512:(tb + 1) * 512], start=(k == 0), stop=(k == kch - 1),
                         sub={hT.name: tb})
                consume(b, tb, ps)
        else:
            for tt in range(ntok // 128):
                ps = c.ps[c.psi % 8]
                c.psi += 1
                for k in range(kch):
                    p.mm(ps[:, :cw], hT[:, k, tt * 128:(tt + 1) * 128], wb[:, k, :cw], start=(k == 0), stop=(k == kch - 1),
                         sub={hT.name: tt // 4})
                consume(b, tt, ps)


def proj_stream(p, c, mode, hT, w_l, consume, kch=8, tsl=None, wname="wb"):
    proj_multi(p, c, hT, [(mode, w_l, consume)], kch=kch, wname=wname)


def layer0_mixer(p, c, W):
    with p.phase():
        c.nmT = p.sb("nmT", [128, 136, 128], BF16)
        with p.phase():
            c.ik2 = p.sb("ik2", [128, 1, T], F32)
            c.iw_sb = p.sb("iw_sb", [128, NT, 16], F32)
            with p.phase():
                hT = p.sb("hT", [128, 8, T], BF16)
                c.wb = {"wb": [p.sb("wb%d" % i, [128, 8, 512], BF16) for i in range(2)]}
                with p.phase():
                    c.sq = [p.sb("sq%d" % i, [128, 8, 512], BF16) for i in range(2)]
                    c.rs = [p.sb("rs%d" % i, [128, 512], F32) for i in range(2)]
                    rmsnorm_fm(p, c, c.g_mix[0], hT, 0, T)
                    layer0_inproj_a(p, c, W, hT)
                layer0_gmlp(p, c, W, hT)
            if c.stop_after == "gmlp":
                return
            layer0_indexer(p, c, W)
        if c.stop_after == "indexer":
            return
        layer0_attention(p, c, W)
    if c.stop_after == "attn":
        return
    with p.phase():
        yT = p.sb("yT", [128, 8, T], BF16)
        for k in range(8):
            p.dma("sp", yT[:, k, :], c.yT_d[k])
        out_proj_residual(p, c, W["w_out0"], yT)


def layer0_inproj_a(p, c, W, hT):
    if True:
        stg = [p.sb("stg%d" % i, [128, T], BF16) for i in range(2)]
        cnt = [0]

        def cons_fm(dst, scale=None):
            def f(b, tb, ps):
                st = stg[b % 2]
                eng = "act" if (cnt[0] % 2 == 0) else "dve"
                cnt[0] += 1
                if scale is None:
                    p.copy(eng, st[:, tb * 512:(tb + 1) * 512], ps[:, :])
                elif eng == "act":
                    p.act(st[:, tb * 512:(tb + 1) * 512], ps[:, :], AF.Copy, scale=scale)
                else:
                    p.ts("dve", st[:, tb * 512:(tb + 1) * 512], ps[:, :], scale, None, ALU.mult)
                if tb == 3:
                    p.dma("sp", dst[b], st[:, :])
            return f

        jobs = [("fm", W["w_q"], cons_fm(c.qT_d, 0.125)), ("fm", W["w_k"], cons_fm(c.kT_d)),
                ("fm", W["w_iq"], cons_fm(c.iqT_d))]

        def cons_ik(b, tb, ps):
            p.copy("act", c.ik2[:, 0, tb * 512:(tb + 1) * 512], ps[:, :])
        jobs.append(("fm", W["w_ik2"], cons_ik))

        vst = [p.sb("vst%d" % i, [128, 512], BF16) for i in range(2)]

        def cons_v(b, tt, ps):
            st = vst[tt % 2]
            p.copy("act" if tt % 2 == 0 else "dve", st[:, :], ps[:, :])
            p.dma("sp", c.v_d[tt * 128:(tt + 1) * 128, :], st[:, :])
        jobs.append(("tm", W["w_v"], cons_v))

        def cons_iw(b, tt, ps):
            p.copy("dve", c.iw_sb[:, tt, :], ps[:, 0:16])
        jobs.append(("tm", W["w_iw"], cons_iw))
        proj_multi(p, c, hT, jobs)


def layer0_gmlp(p, c, W, hT):
    if True:
        u_all = p.sb("u_all", [128, NT, 512], BF16)
        vn_all = p.sb("vn_all", [128, NT, 512], BF16)
        vgf = [p.sb("vgf%d" % i, [128, 512], F32) for i in range(2)]
        junk = p.sb("junk", [128, 512], BF16)
        ssv = p.sb("ssv", [128, NT], F32)
        rsv = p.sb("rsv", [128, NT], F32)
        gn_bc = p.sb("gn_bc", [128, 512], F32)
        p.dma("sp", gn_bc[:, :], W["gmlp_norm"].partition_broadcast(128))

        def cons_u(b, tt, ps):
            p.act(u_all[:, tt, :], ps[:, :], AF.Gelu_apprx_tanh)
        jobs = [("tm", W["w_u"], cons_u)]

        def cons_vg(b, tt, ps):
            vg = vgf[tt % 2]
            p.act(vg[:, :], ps[:, :], AF.Gelu_apprx_tanh)
            p.act(junk[:, :], vg[:, :], AF.Square, accum_out=ssv[:, tt:tt + 1])
            p.act(rsv[:, tt:tt + 1], ssv[:, tt:tt + 1], AF.Sqrt, bias=c.eps_ap[:, :], scale=1.0 / 512)
            p.recip(rsv[:, tt:tt + 1], rsv[:, tt:tt + 1])
            p.stt(vn_all[:, tt, :], vg[:, :], rsv[:, tt:tt + 1], gn_bc[:, :], ALU.mult, ALU.mult)
        jobs.append(("tm", W["w_vg"], cons_vg))
        proj_multi(p, c, hT, jobs)

        wsT = p.sb("wsT", [128, 8, 128], BF16)
        p.dma("pool", wsT[:, :, :], W["w_sT"])
        p.memset("dve", wsT[64:128, :, 0:64], 0.0)
        bs8 = p.sb("bs8", [8, 128], BF16)
        p.dma("pool", bs8[:, :], W["b_s"])
        e8 = p.sb("e8", [8, 512], BF16)
        p.dma("pool", e8[:, :], W["e8"])
        ybs = [p.sb("ybs%d" % i, [128, 512], BF16) for i in range(2)]
        ybT = [p.sb("ybT%d" % i, [128, 512], BF16) for i in range(2)]
        for n in range(NT):
            ps = c.ps[c.psi % 8]
            c.psi += 1
            for g in range(8):
                p.mm(ps[:, g * 64:(g + 1) * 64], wsT[:, g, :], vn_all[:, n, g * 64:(g + 1) * 64], start=True, stop=False, inc=False)
                p.mm(ps[:, g * 64:(g + 1) * 64], bs8[:, :], e8[:, g * 64:(g + 1) * 64], start=False, stop=True, inc=(g == 7))
            yb = ybs[n % 2]
            p.tt("dve", yb[:, :], ps[:, :], u_all[:, n, :], ALU.mult)
            pst = c.psb[c.psi % 8]
            pstn = c.ps[c.psi % 8]
            c.psi += 1
            for cc in range(4):
                p.op("pe", lambda e, cc=cc: e.transpose(pst[:, cc * 128:(cc + 1) * 128], yb[:, cc * 128:(cc + 1) * 128], c.ident_b[:, :]),
                     [p.key(yb), p.key(c.ident_b)], [p.key(pstn)], inc=(cc == 3))
            yst = ybT[n % 2]
            p.copy("act", yst[:, :], pst[:, 0:512])
            p.dma("sp", c.yT_d[4:8, :, n * 128:(n + 1) * 128].rearrange("c p t -> p c t"),
                  yst[:, :].rearrange("p (a b) -> p a b", a=4))


def tri(b):
    return b * (b + 1) // 2


def layer0_indexer(p, c, W):
    with p.phase():
        ikn = p.sb("ikn", [128, 1, T], BF16)
        gk2 = p.sb("gk2", [128, 1], F32)
        p.dma("sp", gk2[:, :], W["gk2"])
        with p.phase():
            c.sq = [p.sb("isq%d" % i, [128, 1, 512], BF16) for i in range(2)]
            c.rs = [p.sb("irs%d" % i, [128, 512], F32) for i in range(2)]
            rmsnorm_fm(p, c, gk2, ikn, 0, T, nparts=128, kch=1, src=c.ik2, scale_div=128)
        wabs = p.sb("wabs", [128, NT, 16], F32)
        sgn = p.sb("sgn", [128, NT, 16], F32)
        p.ts("dve", wabs[:, :, :], c.iw_sb[:, :, :], -1.0, None, ALU.mult)
        p.tt("dve", wabs[:, :, :], wabs[:, :, :], c.iw_sb[:, :, :], ALU.max)
        p.ts("dve", sgn[:, :, :], c.iw_sb[:, :, :], 0.0, 2.0, ALU.is_ge, ALU.mult)
        p.ts("dve", sgn[:, :, :], sgn[:, :, :], -1.0, None, ALU.add)
        sgn_b = p.sb("sgn_b", [128, NT, 16], BF16)
        p.copy("dve", sgn_b[:, :, :], sgn[:, :, :])
        iqz = [p.sb("iqz%d" % i, [128, 8, 2, 128], BF16) for i in range(2)]
        for i in range(2):
            p.memset("pool", iqz[i][:, :, :, :], 0.0)
        Dsg = [p.sb("Dsg%d" % i, [128, 16, 128], BF16) for i in range(4)]
        acc = [p.sb("acc%d" % i, [128, T], F32) for i in range(4)]
        rr = [p.sb("rr%d" % i, [128, 1024], BF16) for i in range(3)]
        nm = [p.sb("nm%d" % i, [128, T], BF16) for i in range(4)]
        junk = [p.sb("junkc%d" % i, [128, T], BF16) for i in range(2)]
        pow2 = p.sb("pow2", [128, NBIS], F32)
        p.dma("sp", pow2[:, :], W["pow2"])
        st = [p.sb("bis%d" % b, [128, 8], F32) for b in range(NT)]
        wtab = [p.sb("wtab%d" % b, [128, NBIS], F32) for b in range(NT)]
        cnt_ = {"ri": 0, "ei": 0, "si": 0}

        def jobs_for(b):
            L = 128 * (b + 1)
            units = [(0, min(L, 1024))] + ([(1024, L)] if L > 1024 else [])
            return [(b, k0, k1, h) for (k0, k1) in units for h in range(16)]

        def load_iq(b):
            iz = iqz[b % 2]
            for par in range(2):
                p.dma("sp", iz[par * 64:(par + 1) * 64, :, par, :],
                      c.iqT_d[:, par * 64:(par + 1) * 64, b * 128:(b + 1) * 128].rearrange("m p t -> p m t"))

        def emit_score(job):
            b, k0, k1, h = job
            n = k1 - k0
            nb = (n + 511) // 512
            m, par = divmod(h, 2)
            slot = 1 + cnt_["si"] % 3
            cnt_["si"] += 1
            psc = c.psum[:, slot * 1024:slot * 1024 + n]
            for kb in range(nb):
                c0, c1 = kb * 512, min(n, (kb + 1) * 512)
                p.mm(psc[:, c0:c1], iqz[b % 2][:, m, par, :], ikn[:, 0, k0 + c0:k0 + c1])
            return psc

        def emit_rest(job, psc):
            b, k0, k1, h = job
            n = k1 - k0
            nb = (n + 511) // 512
            pacc = c.psum[:, 0:n]
            r = rr[cnt_["ri"] % 3]
            cnt_["ri"] += 1
            p.act(r[:, :n], psc, AF.Relu, scale=wabs[:, b, h:h + 1])
            for kb in range(nb):
                c0, c1 = kb * 512, min(n, (kb + 1) * 512)
                p.mm(pacc[:, c0:c1], Dsg[b % 4][:, h, :], r[:, c0:c1], start=(h == 0), stop=(h == 15))
            if h == 15:
                p.copy("act", acc[b % 4][:, k0:k1], pacc)

        def scores(bs):
            jobs = []
            for b in bs:
                load_iq(b)
                jobs += jobs_for(b)
            psc = emit_score(jobs[0])
            for i, job in enumerate(jobs):
                nxt = emit_score(jobs[i + 1]) if i + 1 < len(jobs) else None
                emit_rest(job, psc)
                psc = nxt

        def build_dsg(b):
            for h in range(16):
                p.tt("pool", Dsg[b % 4][:, h, :], c.ident_b[:, :], sgn_b[:, b, h:h + 1].to_broadcast([128, 128]), ALU.mult)

        def bisect(bs):
            S = {}
            for i_, b in enumerate(bs):
                L = 128 * (b + 1)
                ac = acc[b % 4]
                mx, mn, w0, cand, cnt, tq = [st[b][:, i:i + 1] for i in range(6)]
                S[b] = (L, ac, cand, cnt, tq, junk[i_ % 2])
                p.reduce(mx, ac[:, :L], ALU.max)
                p.reduce(mn, ac[:, :L], ALU.min)
                p.memset("dve", ac[0:64, L - 64:L], -1e30)
                p.tt("dve", w0, mx, mn, ALU.subtract)
                p.ts("dve", wtab[b][:, :], pow2[:, :], w0, None, ALU.mult)
                p.tt("dve", cand, mn, wtab[b][:, 0:1], ALU.add)
            for i in range(NBIS):
                last = (i == NBIS - 1)
                for b in bs:
                    L, ac, cand, cnt, tq, jk = S[b]
                    p.ts("dve", jk[:, :L], ac[:, :L], cand, 0.0, ALU.is_ge, ALU.add, accum_out=cnt)
                for b in bs:
                    L, ac, cand, cnt, tq, jk = S[b]
                    p.ts("dve", tq, cnt, float(TOPK), (1.0 if last else 0.5), ALU.is_ge, ALU.subtract)
                for b in bs:
                    L, ac, cand, cnt, tq, jk = S[b]
                    p.stt(cand, tq, wtab[b][:, i:i + 1], cand, ALU.mult, ALU.add)
            for b in bs:
                L, ac, cand, cnt, tq, jk = S[b]
                p.ts("dve", nm[b % 4][:, :L], ac[:, :L], cand, NEG, ALU.is_lt, ALU.mult)

        def transposes(b):
            nmb = nm[b % 4]
            for a0 in range(0, b + 1, 4):
                g = min(4, b + 1 - a0)
                slot = 1 + cnt_["si"] % 3
                cnt_["si"] += 1
                pstn = c.ps[2 * slot]
                pst = c.psb[2 * slot]
                for j in range(g):
                    a = a0 + j
                    p.op("pe", lambda e, j=j, a=a: e.transpose(pst[:, j * 128:(j + 1) * 128], nmb[:, a * 128:(a + 1) * 128], c.ident_b[:, :]),
                         [p.key(nmb), p.key(c.ident_b)], [p.key(pstn)], inc=(j == g - 1))
                o_ap = c.nmT[:, tri(b) + a0:tri(b) + a0 + g, :]
                i_ap = pst[:, 0:g * 128].rearrange("p (a b) -> p a b", a=g)
                p.op("act", lambda e: e.activation(out=o_ap, in_=i_ap, func=AF.Copy), [p.key(pstn)], [p.key(c.nmT, {c.nmT.name: b})])

        for b in range(2):
            L = 128 * (b + 1)
            p.memset("pool", nm[b][:, :L], 0.0)
            p.memset("pool", nm[b][0:64, L - 64:L], NEG)
        prev = [0, 1]
        order = list(range(NT - 2, 1, -2))
        build_dsg(order[0])
        build_dsg(order[0] + 1)
        for oi, b0 in enumerate(order):
            bs = [b0, b0 + 1]
            if oi + 1 < len(order):
                build_dsg(order[oi + 1])
                build_dsg(order[oi + 1] + 1)
            scores(bs)
            for b in prev:
                transposes(b)
            bisect(bs)
            prev = bs
        for b in prev:
            transposes(b)


def layer0_attention(p, c, W):
    with p.phase():
        kT = p.sb("kT", [128, 4, T], BF16)
        for m in range(4):
            p.dma("sp", kT[:, m, :], c.kT_d[m])
        qzs = [p.sb("qz%d" % i, [128, 4, 2, 128], BF16) for i in range(2)]
        for i in range(2):
            p.memset("pool", qzs[i][:, :, :, :], 0.0)
        va = p.sb("va", [128, NT, 8, 65], BF16)
        p.memset("pool", va[:, :, :, 64:65], 1.0)
        for tt in range(NT):
            p.dma("sp", va[:, tt, :, 0:64], c.v_d[tt * 128:(tt + 1) * 128, :].rearrange("p (h d) -> p h d", h=8))
        oh1 = p.sb("oh1", [32, 383], F32)
        p.dma("sp", oh1[:, :], W["oh1"])
        rb = p.sb("rb", [32, 8], F32)
        p.dma("sp", rb[:, :], W["rel_bias"])
        cb = p.sb("cb", [128, 8], F32)
        p.dma("sp", cb[:, :], W["rel_bias"][15:16, :].partition_broadcast(128))
        Tb = p.sb("Tb", [128, 2, 8, 128], BF16)
        for kind in range(2):
            for t0 in range(0, 128, 64):
                ps = c.ps[c.psi % 8]
                c.psi += 1
                for t in range(t0, t0 + 64):
                    base = (255 - t) if kind == 0 else (127 - t)
                    p.mm(ps[:, (t - t0) * 8:(t - t0) * 8 + 8], oh1[:, base:base + 128], rb[:, :], start=True, stop=True,
                         inc=(t == t0 + 63))
                p.tt("dve", Tb[:, kind, :, t0:t0 + 64], ps[:, 0:512].rearrange("p (t h) -> p h t", h=8),
                     cb[:, :].unsqueeze(2).to_broadcast([128, 8, 64]), ALU.subtract)
        ya = [p.sb("ya%d" % i, [128, 512], BF16) for i in range(2)]
        PT = [p.sb("PT%d" % i, [128, 1024], BF16) for i in range(3)]
        rec = p.sb("rec", [128, 16], F32)
        yaT = [p.sb("yaT%d" % i, [128, 512], BF16) for i in range(2)]
        si = 0
        pend = []

        def normalize(b, h, po):
            ri = (b * 8 + h) % 16
            p.recip(rec[:, ri:ri + 1], po[:, 64:65])
            p.act(ya[b % 2][:, h * 64:(h + 1) * 64], po[:, 0:64], AF.Copy, scale=rec[:, ri:ri + 1])
            if h == 7:
                pstn = c.ps[6 + b % 2]
                pst = c.psb[6 + b % 2]
                yab = ya[b % 2]
                for cc in range(4):
                    p.op("pe", lambda e, cc=cc: e.transpose(pst[:, cc * 128:(cc + 1) * 128], yab[:, cc * 128:(cc + 1) * 128], c.ident_b[:, :]),
                         [p.key(yab), p.key(c.ident_b)], [p.key(pstn)], inc=(cc == 3))
                yst = yaT[b % 2]
                p.copy("dve", yst[:, :], pst[:, 0:512])
                p.dma("sp", c.yT_d[0:4, :, b * 128:(b + 1) * 128].rearrange("c p t -> p c t"),
                      yst[:, :].rearrange("p (a b) -> p a b", a=4))

        jobs = [(b, h, a0) for b in range(NT) for h in range(8) for a0 in range(0, b + 1, 8)]

        def emit_score(job, idx):
            b, h, a0 = job
            qz = qzs[b % 2]
            if h == 0 and a0 == 0:
                for par in range(2):
                    p.dma("sp", qz[par * 64:(par + 1) * 64, :, par, :],
                          c.qT_d[:, par * 64:(par + 1) * 64, b * 128:(b + 1) * 128].rearrange("m p t -> p m t"))
            m = h // 2
            grp = list(range(a0, min(a0 + 8, b + 1)))
            n = len(grp) * 128
            slot = idx % 2
            ps = c.psum[:, slot * 1024:slot * 1024 + n]
            for j, a in enumerate(grp):
                near = (a >= b - 1)
                lastj = (j == len(grp) - 1)
                o = ps[:, j * 128:(j + 1) * 128]
                p.mm(o, kT[:, m, a * 128:(a + 1) * 128], qz[:, h // 2, h % 2, :], start=True, stop=False, inc=False)
                p.mm(o, c.ident_b[:, :], c.nmT[:, tri(b) + a, :], start=False, stop=(not near), inc=((not near) and lastj))
                if near:
                    kind = 0 if a == b else 1
                    p.mm(o, c.ident_b[:, :], Tb[:, kind, h, :], start=False, stop=True, inc=lastj)
            return ps

        def emit_rest(job, idx, ps):
            nonlocal pend
            b, h, a0 = job
            grp = list(range(a0, min(a0 + 8, b + 1)))
            n = len(grp) * 128
            po = c.ps[4 + (b * 8 + h) % 2]
            pt = PT[idx % 3]
            p.act(pt[:, :n], ps, AF.Exp, bias=cb[:, h:h + 1])
            for j, a in enumerate(grp):
                p.mm(po[:, 0:65], pt[:, j * 128:(j + 1) * 128], va[:, a, h, :], start=(a == 0), stop=(a == b))
            if grp[-1] == b:
                for f in pend:
                    f()
                pend = [lambda b=b, h=h, po=po: normalize(b, h, po)]

        ps = emit_score(jobs[0], 0)
        for i, job in enumerate(jobs):
            nxt = emit_score(jobs[i + 1], i + 1) if i + 1 < len(jobs) else None
            emit_rest(job, i, ps)
            ps = nxt
        for f in pend:
            f()


def out_proj_residual(p, c, w_l, src):
    kch = w_l.shape[2]
    with p.phase():
        c.wb = {"wb": [p.sb("wbo%d" % i, [128, kch, 128], BF16) for i in range(2)]}

        def cons(b, tb, ps):
            p.tt("dve", c.xT[:, b, tb * 512:(tb + 1) * 512], ps[:, :], c.xT[:, b, tb * 512:(tb + 1) * 512], ALU.add)
        proj_stream(p, c, "fm", src, w_l, cons, kch=kch)


def conv_ffn(p, c, W, l):
    w_up = W["w_up%d" % l]
    w_dn = W["w_dn%d" % l]
    with p.phase():
        cw = p.sb("cw", [128, 44, 3], F32)
        cbs = p.sb("cbs", [128, 44], F32)
        p.dma("sp", cw[:, :, :], W["ffn_cw%d" % l])
        p.dma("sp", cbs[:, :], W["ffn_cb%d" % l])
        halo = p.sb("halo", [128, 44, 2], F32)
        for half in range(2):
            t0 = half * 1024
            with p.phase():
                hT = p.sb("hTf", [128, 8, 1024], BF16)
                gT = p.sb("gT", [128, NJ, 1024], BF16)
                with p.phase():
                    c.sq = [p.sb("fsq%d" % i, [128, 8, 512], BF16) for i in range(2)]
                    c.rs = [p.sb("frs%d" % i, [128, 512], F32) for i in range(2)]
                    rmsnorm_fm(p, c, c.g_ffn[l], hT, t0, 1024)
                wu = [[p.sb("wu%d_%d" % (sd, i), [128, 8, 128], BF16) for i in range(2)] for sd in range(2)]
                wd = [p.sb("wd%d" % i, [128, NJ, 128], BF16) for i in range(2)]
                ya = [p.sb("fya%d" % i, [128, 512], F32) for i in range(3)]
                yb = [p.sb("fyb%d" % i, [128, 512], F32) for i in range(3)]
                sa = [p.sb("fsa%d" % i, [128, 512], F32) for i in range(2)]
                hl = [p.sb("fhl%d" % i, [128, 2], F32) for i in range(4)]
                hli = 0

                def load(j):
                    p.dma("pool", wu[0][j % 2][:, :, :], w_up[j])
                    p.dma("pool", wu[1][j % 2][:, :, :], w_up[NJ + j])

                load(0)
                it = 0
                for j in range(NJ):
                    if j + 1 < NJ:
                        load(j + 1)
                    if j == NJ - 5:
                        p.dma("pool", wd[0][:, :, :], w_dn[0])
                    if j == NJ - 3:
                        p.dma("pool", wd[1][:, :, :], w_dn[1])
                    for tb in range(2):
                        ys = []
                        for sd in range(2):
                            m = sd * NJ + j
                            ps = c.ps[c.psi % 8]
                            c.psi += 1
                            wt = wu[sd][j % 2]
                            for k in range(8):
                                p.mm(ps[:, :], wt[:, k, :], hT[:, k, tb * 512:(tb + 1) * 512], start=(k == 0), stop=(k == 7),
                                     sub={hT.name: tb})
                            y = (ya if sd == 0 else yb)[it % 3]
                            p.act(y[:, :], ps[:, :], AF.Identity, bias=cbs[:, m:m + 1], scale=cw[:, m, 2:3])
                            p.stt(y[:, 1:512], ps[:, 0:511], cw[:, m, 1:2], y[:, 1:512], ALU.mult, ALU.add)
                            p.stt(y[:, 2:512], ps[:, 0:510], cw[:, m, 0:1], y[:, 2:512], ALU.mult, ALU.add)
                            first = (half == 0 and tb == 0)
                            if not first:
                                if tb == 0:
                                    hsrc = halo[:, m, :]
                                else:
                                    hsrc = hprev[sd][:, :]
                                p.stt(y[:, 0:1], hsrc[:, 1:2], cw[:, m, 1:2], y[:, 0:1], ALU.mult, ALU.add)
                                p.stt(y[:, 0:2], hsrc[:, 0:2], cw[:, m, 0:1], y[:, 0:2], ALU.mult, ALU.add)
                            if tb == 0:
                                if sd == 0:
                                    hprev = [None, None]
                                hprev[sd] = hl[hli % 4]
                                hli += 1
                                p.copy("act", hprev[sd][:, :], ps[:, 510:512])
                            elif half == 0:
                                p.copy("act", halo[:, m, :], ps[:, 510:512])
                            ys.append(y)
                        sg = sa[it % 2]
                        p.act(sg[:, :], ys[0][:, :], AF.Silu)
                        p.tt("pool", gT[:, j, tb * 512:(tb + 1) * 512], sg[:, :], ys[1][:, :], ALU.mult)
                        it += 1
                for dc in range(8):
                    for tb in range(2):
                        ps = c.ps[c.psi % 8]
                        c.psi += 1
                        for j in range(NJ):
                            p.mm(ps[:, :], wd[dc % 2][:, j, :], gT[:, j, tb * 512:(tb + 1) * 512], start=(j == 0), stop=(j == NJ - 1))
                        sl = c.xT[:, dc, t0 + tb * 512:t0 + (tb + 1) * 512]
                        p.tt("dve", sl, ps[:, :], sl, ALU.add)
                    if dc + 2 < 8:
                        p.dma("pool", wd[dc % 2][:, :, :], w_dn[dc + 2])


def hgrn_mixer(p, c, W):
    with p.phase():
        hT = p.sb("hT1", [128, 8, T], BF16)
        c.wb = {"wb": [p.sb("wbh%d" % i, [128, 8, 512], BF16) for i in range(2)]}
        lbr = p.sb("lbr", [128, 2, 8], F32)
        p.dma("sp", lbr[:, :, :], W["hgrn_lb"])
        lb = p.sb("lb", [128, 8], F32)
        oml = p.sb("oml", [128, 8], F32)
        p.tt("dve", lb[:, :], lbr[:, 1, :], lbr[:, 0, :], ALU.subtract)
        p.act(lb[:, :], lb[:, :], AF.Sigmoid)
        p.ts("dve", oml[:, :], lb[:, :], -1.0, 1.0, ALU.mult, ALU.add)
        with p.phase():
            c.sq = [p.sb("hsq%d" % i, [128, 8, 512], BF16) for i in range(2)]
            c.rs = [p.sb("hrs%d" % i, [128, 512], F32) for i in range(2)]
            rmsnorm_fm(p, c, c.g_mix[1], hT, 0, T)
        stg = [p.sb("hstg%d" % i, [128, T], BF16) for i in range(2)]

        def cons_silu(dst):
            def f(b, tb, ps):
                st = stg[b % 2]
                p.act(st[:, tb * 512:(tb + 1) * 512], ps[:, :], AF.Silu)
                if tb == 3:
                    p.dma("sp", dst[b], st[:, :])
            return f
        jobs = [("fm", W["w_cq"], cons_silu(c.qh_d)), ("fm", W["w_cg"], cons_silu(c.gate_d))]

        gst = [p.sb("gst%d" % i, [128, T], F32) for i in range(2)]
        sgt = [p.sb("sgt%d" % i, [128, 512], F32) for i in range(2)]

        def cons_f(b, tb, ps):
            g = gst[b % 2]
            sg = sgt[tb % 2]
            p.act(sg[:, :], ps[:, :], AF.Sigmoid)
            p.ts("dve", g[:, tb * 512:(tb + 1) * 512], sg[:, :], oml[:, b:b + 1], lb[:, b:b + 1], ALU.mult, ALU.add)
            if tb == 3:
                st = stg[b % 2]
                p.ts("dve", st[:, :], g[:, :], -1.0, 1.0, ALU.mult, ALU.add)
                p.dma("sp", c.kk_d[b], st[:, :])
                p.act(g[:, :], g[:, :], AF.Ln)
                p.dma("sp", c.lg_d[b], g[:, :])
        jobs.append(("fm", W["w_cf"], cons_f))

        ist = [p.sb("ist%d" % i, [128, 512], BF16) for i in range(2)]

        def cons_i(b, tt, ps):
            st = ist[tt % 2]
            p.copy("act" if tt % 2 == 0 else "dve", st[:, :], ps[:, :])
            p.dma("sp", c.i_d[tt * 128:(tt + 1) * 128, b * 512:(b + 1) * 512], st[:, :])
        jobs.append(("tm", W["w_ci"], cons_i))
        proj_multi(p, c, hT, jobs)
    if c.stop_after == "hproj":
        return
    with p.phase():
        cm = p.sb("cm", [128, 32, 64], BF16)
        p.memset("pool", cm[:, :, :], 1.0)
        p.memset("pool", cm[:, :, 0:1], 0.0)
        tri64 = p.sb("tri64", [64, 64], F32)
        p.dma("sp", tri64[:, :], W["tri64"])
        cn = p.sb("cn", [128, 1], F32)
        p.dma("sp", cn[:, :], W["c_out_norm"])
        lg = p.sb("lg", [128, T], F32)
        bb = p.sb("bb", [128, T], F32)
        eb = p.sb("eb", [128, T], F32)
        t1 = p.sb("t1", [128, T], F32)
        qh = p.sb("qh", [128, T], BF16)
        kk = p.sb("kk", [128, T], BF16)
        KlT = p.sb("KlT", [128, T], BF16)
        Qb = [p.sb("Qb%d" % i, [128, T], BF16) for i in range(2)]
        Sb = [p.sb("Sb%d" % i, [128, 128], BF16) for i in range(2)]
        Kbb = [p.sb("Kbb%d" % i, [128, T], BF16) for i in range(2)]
        Klc = [p.sb("Klc%d" % i, [64, 32, 128], BF16) for i in range(2)]
        ic = [p.sb("ic%d" % i, [64, 32, 128], BF16) for i in range(2)]
        ebl = [p.sb("ebl%d" % i, [128, 32], F32) for i in range(2)]
        gt = [p.sb("gt%d" % i, [128, T], BF16) for i in range(2)]
        oTt = [p.sb("oTt%d" % i, [128, 512], F32) for i in range(2)]
        Sf = [p.sb("Sf%d" % i, [128, 128], F32) for i in range(2)]
        ATs = [p.sb("ATs%d" % i, [64, 64], BF16) for i in range(3)]
        osq = [p.sb("osq%d" % i, [128, 512], BF16) for i in range(2)]
        ors = [p.sb("ors%d" % i, [128, 512], F32) for i in range(2)]
        on = [p.sb("on%d" % i, [128, 512], F32) for i in range(2)]
        ost = [p.sb("ost%d" % i, [128, 512], BF16) for i in range(2)]

        def precompute_steps(h):
            u = h % 2
            ebv = eb[:, :].rearrange("p (a b) -> p a b", b=64)
            st = []

            def loads():
                p.dma("sp", lg[:, :], c.lg_d[h])
                p.dma("sp", qh[:, :], c.qh_d[h])
                p.dma("sp", kk[:, :], c.kk_d[h])
                p.dma("sp", gt[u][:, :], c.gate_d[h])
                p.dma("sp", ic[u][:, :, :], c.i_d[:, h * 128:(h + 1) * 128].rearrange("(n s) e -> s n e", s=64))
            st.append(loads)
            st.append(lambda: p.scan(bb[:, :], cm[:, :, :].rearrange("p a b -> p (a b)"), lg[:, :], 0.0, ALU.mult, ALU.add))
            st.append(lambda: p.act(eb[:, :], bb[:, :], AF.Exp))
            st.append(lambda: p.act(t1[:, :], bb[:, :], AF.Exp, scale=-1.0))
            st.append(lambda: p.tt("pool", Qb[u][:, :], qh[:, :], eb[:, :], ALU.mult))
            st.append(lambda: p.tt("pool", t1[:, :], kk[:, :], t1[:, :], ALU.mult))
            st.append(lambda: p.copy("act", ebl[u][:, :], ebv[:, :, 63]))
            st.append(lambda: p.copy("act", Kbb[u][:, :], t1[:, :]))
            st.append(lambda: p.tt("pool", KlT[:, :].rearrange("p (a b) -> p a b", b=64), t1[:, :].rearrange("p (a b) -> p a b", b=64),
                                   ebv[:, :, 63:64].to_broadcast([128, 32, 64]), ALU.mult))

            def trs(g8):
                pstn = c.ps[7]
                pst = c.psb[7]
                for j in range(8):
                    n = g8 * 8 + j
                    p.op("pe", lambda e, j=j, n=n: e.transpose(pst[0:64, j * 128:(j + 1) * 128], KlT[:, n * 64:(n + 1) * 64], c.ident_b[:, :]),
                         [p.key(KlT), p.key(c.ident_b)], [p.key(pstn)], inc=(j == 7))
                p.op("act", lambda e: e.activation(out=Klc[u][:, g8 * 8:(g8 + 1) * 8, :],
                                                   in_=pst[0:64, :].rearrange("p (a b) -> p a b", a=8), func=AF.Copy),
                     [p.key(pstn)], [p.key(Klc[u])])
            for g8 in range(4):
                st.append(lambda g8=g8: trs(g8))
            return st

        def chunkloop(h, bg):
            u = h % 2

            def front(n):
                cs = slice(n * 64, (n + 1) * 64)
                if n < 31:
                    psS = c.ps[4 + n % 3]
                    p.mm(psS[:, 0:128], Klc[u][:, n, :], ic[u][:, n, :])
                psA = c.ps[n % 2]
                p.mm(psA[0:64, 0:64], Kbb[u][:, cs], Qb[u][:, cs])
                p.tt("dve", ATs[n % 3][:, :], psA[0:64, 0:64], tri64[:, :], ALU.mult)

            def outnorm_steps(tb, po):
                ts_ = slice(tb * 512, (tb + 1) * 512)
                oT = oTt[tb % 2]
                sq = osq[tb % 2]
                rs = ors[tb % 2]
                o2 = on[tb % 2]
                o3 = ost[tb % 2]
                ps = c.ps[7]

                def s1():
                    p.copy("act", oT[:, :], po[:, :])
                    p.act(sq[:, :], oT[:, :], AF.Square)

                def s2():
                    p.mm(ps[:, :], c.ones_b[:, :], sq[:, :])

                def s3():
                    p.act(rs[:, :], ps[:, :], AF.Ln, bias=c.eps_ap[:, :], scale=1.0 / 128)
                    p.act(rs[:, :], rs[:, :], AF.Exp, scale=-0.5)
                    p.stt(o2[:, :], oT[:, :], cn[:, 0:1], rs[:, :], ALU.mult, ALU.mult)
                    p.tt("pool", o3[:, :], o2[:, :], gt[u][:, ts_], ALU.mult)
                    p.dma("pool", c.oT_d[h][:, ts_], o3[:, :])
                return [s1, s2, s3]

            front(0)
            po = None
            for n in range(32):
                cs = slice(n * 64, (n + 1) * 64)
                if n + 1 < 32:
                    front(n + 1)
                if n % 8 == 0:
                    po = c.ps[2 + (n // 8) % 2]
                oc = po[:, (n % 8) * 64:(n % 8 + 1) * 64]
                p.mm(oc, ic[u][:, n, :], ATs[n % 3][:, :], start=True, stop=(n == 0), inc=(n == 0))
                if n > 0:
                    p.mm(oc, Sb[(n - 1) % 2][:, :], Qb[u][:, cs], start=False, stop=True, inc=True)
                if n < 31:
                    psS = c.ps[4 + n % 3]
                    if n == 0:
                        p.copy("dve", Sf[0][:, :], psS[:, 0:128])
                    else:
                        p.stt(Sf[n % 2][:, :], Sf[(n - 1) % 2][:, :], ebl[u][:, n:n + 1], psS[:, 0:128], ALU.mult, ALU.add)
                    p.copy("act", Sb[n % 2][:, :], Sf[n % 2][:, :])
                if n % 8 == 7:
                    for f in reversed(outnorm_steps(n // 8, po)):
                        bg.insert(0, f)
                if bg:
                    bg.pop(0)()
            while bg:
                bg.pop(0)()

        for f in precompute_steps(0):
            f()
        for h in range(8):
            bg = precompute_steps(h + 1) if h + 1 < 8 else []
            chunkloop(h, bg)
    if c.stop_after == "hrec":
        return
    with p.phase():
        oTa = p.sb("oTa", [128, 8, T], BF16)
        for k in range(8):
            p.dma("sp", oTa[:, k, :], c.oT_d[k])
        out_proj_residual(p, c, W["w_out1"], oTa)


def final_norm(p, c, out):
    with p.phase():
        c.sq = [p.sb("nsq%d" % i, [128, 8, 512], BF16) for i in range(2)]
        c.rs = [p.sb("nrs%d" % i, [128, 512], F32) for i in range(2)]
        ot = [p.sb("fot%d" % i, [128, 8, 512], F32) for i in range(2)]
        for i, tb in enumerate(range(0, T, 512)):
            sq = c.sq[i % 2]
            p.act(sq[:, :, :], c.xT[:, :, tb:tb + 512], AF.Square)
            ps = c.ps[c.psi % 8]
            c.psi += 1
            for k in range(8):
                p.mm(ps[:, :], c.ones_b[:, :], sq[:, k, :], start=(k == 0), stop=(k == 7))
            rs = c.rs[i % 2]
            p.act(rs[:, :], ps[:, :], AF.Ln, bias=c.eps_ap[:, :], scale=1.0 / D)
            p.act(rs[:, :], rs[:, :], AF.Exp, scale=-0.5)
            o = ot[i % 2]
            for k in range(8):
                p.stt(o[:, k, :], c.xT[:, k, tb:tb + 512], c.g_fin[:, k:k + 1], rs[:, :], ALU.mult, ALU.mult)
            p.dma("sp", out[:, :, tb:tb + 512].rearrange("c p t -> p c t"), o[:, :, :])

def lay(Wm, cw):
    K, N = Wm.shape
    return np.ascontiguousarray(Wm.reshape(K // 128, 128, N // cw, cw).transpose(2, 1, 0, 3))


def build(stop_after=None, dbg=()):
    nc = bass.Bass("TRN2", target_bir_lowering=False)
    p = Prog(nc)
    c = Ctx()
    W = {}

    def din(name, shape, dt=F32):
        W[name] = nc.dram_tensor(name, list(shape), dt, kind="ExternalInput").ap()
        return W[name]

    def dscr(name, shape, dt):
        kind = "ExternalOutput" if name in dbg else "Internal"
        return nc.dram_tensor(name, list(shape), dt, kind=kind).ap()

    din("xT_in", [8, 128, T])
    din("ident", [128, 128])
    din("g_mix", [2, 128, 8]); din("g_ffn", [2, 128, 8]); din("g_fin", [128, 8])
    din("w_q", [4, 128, 8, 128]); din("w_k", [4, 128, 8, 128]); din("w_iq", [8, 128, 8, 128])
    din("w_ik2", [1, 128, 8, 128]); din("w_v", [1, 128, 8, 512]); din("w_iw", [1, 128, 8, 16])
    din("w_u", [1, 128, 8, 512]); din("w_vg", [1, 128, 8, 512])
    din("gmlp_norm", [1, 512]); din("w_sT", [128, 8, 128]); din("b_s", [8, 128]); din("e8", [8, 512])
    din("gk2", [128, 1]); din("pow2", [128, NBIS]); din("oh1", [32, 383]); din("rel_bias", [32, 8])
    din("w_out0", [8, 128, 8, 128])
    for l in range(2):
        din("w_up%d" % l, [44, 128, 8, 128]); din("w_dn%d" % l, [8, 128, NJ, 128])
        din("ffn_cw%d" % l, [128, 44, 3]); din("ffn_cb%d" % l, [128, 44])
    din("w_cq", [8, 128, 8, 128]); din("w_cf", [8, 128, 8, 128]); din("w_cg", [8, 128, 8, 128]); din("w_ci", [2, 128, 8, 512])
    din("hgrn_lb", [128, 2, 8]); din("tri64", [64, 64]); din("c_out_norm", [128, 1]); din("w_out1", [8, 128, 8, 128])
    out = nc.dram_tensor("outT", [8, 128, T], F32, kind="ExternalOutput").ap()

    c.qT_d = dscr("qT_d", [4, 128, T], BF16)
    c.kT_d = dscr("kT_d", [4, 128, T], BF16)
    c.iqT_d = dscr("iqT_d", [8, 128, T], BF16)
    c.v_d = dscr("v_d", [T, 512], BF16)
    c.yT_d = dscr("yT_d", [8, 128, T], BF16)
    c.qh_d = dscr("qh_d", [8, 128, T], BF16)
    c.gate_d = dscr("gate_d", [8, 128, T], BF16)
    c.kk_d = dscr("kk_d", [8, 128, T], BF16)
    c.lg_d = dscr("lg_d", [8, 128, T], F32)
    c.i_d = dscr("i_d", [T, 1024], BF16)
    c.oT_d = dscr("oT_d", [8, 128, T], BF16)

    c.xT = p.sb("xT", [128, 8, T], F32, True)
    c.ident_f = p.sb("ident_f", [128, 128], F32, True)
    c.ident_b = p.sb("ident_b", [128, 128], BF16, True)
    c.ones_b = p.sb("ones_b", [128, 128], BF16, True)
    c.eps_ap = p.sb("eps_ap", [128, 1], F32, True)
    gm = p.sb("g_mix_sb", [128, 2, 8], F32, True)
    gf = p.sb("g_ffn_sb", [128, 2, 8], F32, True)
    gfin = p.sb("g_fin_sb", [128, 8], F32, True)
    c.psum = p.es.enter_context(nc.psum_tensor("psum", [128, 4096], F32))
    c.ps = [c.psum[:, i * 512:(i + 1) * 512] for i in range(8)]
    c.psb = [t.bitcast(BF16) for t in c.ps]
    c.psi = 0
    c.stop_after = stop_after
    c.dbg = dbg
    c.dscr = dscr

    p.dma("sp", c.ident_f[:, :], W["ident"])
    p.dma("pool", c.ident_b[:, :], W["ident"])
    p.memset("dve", c.ones_b[:, :], 1.0)
    p.memset("dve", c.eps_ap[:, :], EPS)
    for l in range(2):
        p.dma("sp", gm[:, l, :], W["g_mix"][l])
        p.dma("sp", gf[:, l, :], W["g_ffn"][l])
    p.dma("sp", gfin[:, :], W["g_fin"])
    for tb in range(4):
        p.dma("sp", c.xT[:, :, tb * 512:(tb + 1) * 512],
              W["xT_in"][:, :, tb * 512:(tb + 1) * 512].rearrange("k p t -> p k t"), sub={c.xT.name: tb})
    c.g_mix = [gm[:, l, :] for l in range(2)]
    c.g_ffn = [gf[:, l, :] for l in range(2)]
    c.g_fin = gfin

    layer0_mixer(p, c, W)
    stages = ["gmlp", "indexer", "attn", "mix0", "ffn0", "hproj", "hrec", "mix1", "ffn1", None]
    si_ = stages.index(stop_after)
    if si_ >= stages.index("ffn0"):
        conv_ffn(p, c, W, 0)
    if si_ >= stages.index("hproj"):
        hgrn_mixer(p, c, W)
    if si_ >= stages.index("ffn1"):
        conv_ffn(p, c, W, 1)
    if stop_after is None:
        final_norm(p, c, out)
        p.barrier()
        return nc, p, c

    for k in range(8):
        p.dma("sp", out[k], c.xT[:, k, :])
    p.barrier()
    return nc, p, c


def t5_onehot():
    rel = np.arange(-255, 128)
    half, max_exact = 16, 8
    ret = np.where(rel > 0, half, 0)
    n = np.abs(rel)
    nf = np.maximum(n, max_exact).astype(np.float32)
    large = max_exact + (np.log(nf / np.float32(max_exact)) / np.float32(np.log(128 / 8)) * np.float32(half - max_exact)).astype(np.int32)
    large = np.minimum(large, half - 1)
    bucket = ret + np.where(n < max_exact, n, large)
    oh = np.zeros((32, 383), np.float32)
    oh[bucket, np.arange(383)] = 1.0
    return oh


def host_inputs(inp, b):
    f = np.float32
    m = {}
    x = inp["x"][b]
    m["xT_in"] = np.ascontiguousarray(x.T.reshape(8, 128, T))
    m["ident"] = np.eye(128, dtype=f)
    m["g_mix"] = np.ascontiguousarray(inp["mix_norm"].reshape(2, 8, 128).transpose(0, 2, 1))
    m["g_ffn"] = np.ascontiguousarray(inp["ffn_norm"].reshape(2, 8, 128).transpose(0, 2, 1))
    m["g_fin"] = np.ascontiguousarray(inp["final_norm"].reshape(8, 128).T)
    Wi = inp["ab_w_in"][0]
    m["w_q"] = lay(Wi[:, 0:512], 128)
    m["w_k"] = lay(Wi[:, 512:1024], 128)
    m["w_v"] = lay(Wi[:, 1024:1536], 512)
    m["w_iq"] = lay(Wi[:, 1536:2560], 128)
    m["w_ik2"] = lay(np.concatenate([Wi[:, 2560:2624], Wi[:, 2560:2624]], axis=1), 128)
    m["w_iw"] = lay(Wi[:, 2624:2640], 16)
    m["w_u"] = lay(Wi[:, 2640:3152], 512)
    m["w_vg"] = lay(Wi[:, 3152:3664], 512)
    m["gmlp_norm"] = np.ascontiguousarray(inp["ab_gmlp_norm"][0].reshape(1, 512))
    m["w_sT"] = np.ascontiguousarray(inp["ab_w_s"][0].transpose(2, 0, 1))
    m["b_s"] = np.ascontiguousarray(inp["ab_b_s"][0])
    e8 = np.zeros((8, 512), f)
    for g in range(8):
        e8[g, g * 64:(g + 1) * 64] = 1.0
    m["e8"] = e8
    m["gk2"] = np.concatenate([inp["ab_idx_k_norm"][0], inp["ab_idx_k_norm"][0]]).reshape(128, 1)
    m["pow2"] = np.tile((0.5 ** np.arange(1, NBIS + 1)).astype(f)[None, :], (128, 1))
    m["oh1"] = t5_onehot()
    m["rel_bias"] = inp["rel_bias"]
    m["w_out0"] = lay(inp["ab_w_out"][0], 128)
    Wc = inp["c_w_in"][0]
    m["w_cq"] = lay(Wc[:, 0:1024], 128)
    m["w_cf"] = lay(Wc[:, 1024:2048], 128)
    m["w_ci"] = lay(Wc[:, 2048:3072], 512)
    m["w_cg"] = lay(Wc[:, 3072:4096], 128)
    m["hgrn_lb"] = inp["hgrn_lb"].reshape(2, 8, 128).transpose(2, 0, 1)
    m["tri64"] = np.triu(np.ones((64, 64), f))
    m["c_out_norm"] = inp["c_out_norm"][0].reshape(128, 1)
    m["w_out1"] = lay(inp["c_w_out"][0], 128)
    for l in range(2):
        m["w_up%d" % l] = lay(inp["ffn_w_up"][l], 128)
        m["w_dn%d" % l] = lay(inp["ffn_w_down"][l], 128)
        m["ffn_cw%d" % l] = inp["ffn_conv_w"][l].reshape(3, 44, 128).transpose(2, 1, 0)
        m["ffn_cb%d" % l] = inp["ffn_conv_b"][l].reshape(44, 128).T
    return {k: np.ascontiguousarray(v, dtype=f) for k, v in m.items()}


def kernel(**inputs):
    inp = {k: np.asarray(v) for k, v in inputs.items()}
    nc, p, c = build()
    in_maps = [host_inputs(inp, b) for b in range(8)]
    res = run_bass_kernel_spmd(nc, in_maps, core_ids=list(range(8)))
    outs = [np.asarray(r["outT"]).reshape(D, T).T for r in res.results]
    return np.stack(outs, axis=0).astype(np.float32)
```

```python
import numpy as np
from contextlib import ExitStack, contextmanager
import concourse.bass as bass
import concourse.mybir as mybir
from concourse.bass_utils import run_bass_kernel_spmd

F32 = mybir.dt.float32
BF16 = mybir.dt.bfloat16
ALU = mybir.AluOpType
AF = mybir.ActivationFunctionType
AX = mybir.AxisListType

T = 2048
D = 1024
NT = 16
EPS = 1e-6
D_FF = 2816
NJ = 22
TOPK = 256
NBIS = 14
NEG = -30000.0


class Tok:
    __slots__ = ("sem", "key", "val", "clock")

    def __init__(self, sem, key, val, clock):
        self.sem, self.key, self.val, self.clock = sem, key, val, clock


class Reg:
    __slots__ = ("w", "r")

    def __init__(self):
        self.w = None
        self.r = {}


class Prog:
    def __init__(self, nc):
        self.nc = nc
        self.es = ExitStack()
        self.E = {"pe": nc.tensor, "act": nc.scalar, "dve": nc.vector, "pool": nc.gpsimd, "sp": nc.sync}
        self.csem = {e: self.es.enter_context(nc.semaphore("cs_" + e)) for e in ("pe", "act", "dve", "pool")}
        self.ccnt = {e: 0 for e in self.csem}
        self.known = {e: {} for e in self.E}
        self.dq = {}
        for q, n in (("sp", 24), ("pool", 16)):
            self.dq[q] = dict(
                sems=[self.es.enter_context(nc.semaphore("d_%s%d" % (q, i))) for i in range(n)],
                use=[0] * n, last=[None] * n, nxt=0)
        self.regs = {}
        self.last = {e: None for e in self.csem}
        self.phase_stack = None
        self.ninst = 0
        self.nwait = 0
        self.nsb = 0

    @staticmethod
    def key(x, sub=None):
        if isinstance(x, (tuple, list)):
            return x
        if isinstance(x, str):
            return (x, None)
        name = x.tensor.name if hasattr(x, "tensor") else x.name
        if name == "psum":
            sz = mybir.dt.size(x.dtype)
            apl = list(x.ap)
            off = (x.offset % apl[0][0]) * sz
            ext = sum((cnt - 1) * abs(st) for st, cnt in apl[1:]) * sz + sz
            return [("psum", bk) for bk in range(off // 512, (off + ext - 1) // 512 + 1)]
        if sub and name in sub:
            return (name, sub[name])
        return (name, None)

    def _targets(self, k):
        name, s = k
        d = self.regs.setdefault(name, {})
        if s is None:
            if None not in d:
                d[None] = Reg()
            return list(d.values())
        if s not in d:
            d[s] = Reg()
        out = [d[s]]
        if None in d:
            out.append(d[None])
        return out

    def _wait(self, eng, tok):
        if tok is None:
            return
        if eng == "pe" and tok.key == "pe":
            return
        k = self.known[eng]
        if k.get(tok.key, 0) >= tok.val:
            return
        if tok.key in self.ccnt:
            assert tok.val <= self.ccnt[tok.key], ("wait on future inc", eng, tok.key, tok.val)
        self.E[eng].wait_ge(tok.sem, tok.val)
        self.nwait += 1
        for kk, vv in tok.clock.items():
            if k.get(kk, 0) < vv:
                k[kk] = vv

    def _sync(self, eng, reads, writes):
        for k in reads:
            for rg in self._targets(k):
                self._wait(eng, rg.w)
        for k in writes:
            for rg in self._targets(k):
                self._wait(eng, rg.w)
                for t in list(rg.r.values()):
                    self._wait(eng, t)

    def _record(self, tok, reads, writes):
        for k in reads:
            name, s = k
            rg = self.regs[name][s]
            old = rg.r.get(tok.key)
            if old is None or old.val < tok.val:
                rg.r[tok.key] = tok
        for k in writes:
            name, s = k
            rg = self.regs[name][s]
            rg.w = tok
            rg.r = {}

    def _flat(self, ks):
        out = []
        for k in ks:
            k = self.key(k)
            if isinstance(k, list):
                out += k
            else:
                out.append(k)
        return out

    def op(self, eng, fn, reads, writes, inc=True):
        reads = self._flat(reads)
        writes = self._flat(writes)
        self._sync(eng, reads, writes)
        inst = fn(self.E[eng])
        self.ninst += 1
        if inc:
            self.ccnt[eng] += 1
            inst.then_inc(self.csem[eng], 1)
            val = self.ccnt[eng]
        else:
            val = self.ccnt[eng] + 1
        clock = dict(self.known[eng])
        clock[eng] = max(clock.get(eng, 0), val)
        tok = Tok(self.csem[eng], eng, val, clock)
        self.last[eng] = tok
        self._record(tok, reads, writes)
        return tok

    def dma(self, q, out, in_, sub=None):
        reads = self._flat([self.key(in_, sub)])
        writes = self._flat([self.key(out, sub)])
        self._sync(q, reads, writes)
        d = self.dq[q]
        j = d["nxt"]
        d["nxt"] = (j + 1) % len(d["sems"])
        self._wait(q, d["last"][j])
        inst = self.E[q].dma_start(out=out, in_=in_)
        self.ninst += 1
        d["use"][j] += 1
        inst.then_inc(d["sems"][j], 16)
        key = ("d", q, j)
        val = 16 * d["use"][j]
        clock = dict(self.known[q])
        clock[key] = val
        tok = Tok(d["sems"][j], key, val, clock)
        d["last"][j] = tok
        self._record(tok, reads, writes)
        return tok

    def barrier(self):
        toks = [t for t in self.last.values() if t is not None]
        for d in self.dq.values():
            toks += [t for t in d["last"] if t is not None]
        for e in self.E:
            for t in toks:
                self._wait(e, t)

    def sb(self, name, shape, dtype, persistent=False):
        st = self.es if (persistent or self.phase_stack is None) else self.phase_stack
        self.nsb += 1
        return st.enter_context(self.nc.sbuf_tensor("s%d_%s" % (self.nsb, name), list(shape), dtype))

    @contextmanager
    def phase(self):
        prev = self.phase_stack
        self.phase_stack = ExitStack()
        try:
            yield
            self.barrier()
        finally:
            self.phase_stack.close()
            self.phase_stack = prev

    def _aps(self, sub, *xs):
        return [self.key(x, sub) for x in xs if x is not None and not isinstance(x, (int, float))]

    def mm(self, out, lhsT, rhs, start=True, stop=True, inc=None, sub=None):
        if inc is None:
            inc = stop
        return self.op("pe", lambda e: e.matmul(out, lhsT, rhs, start=start, stop=stop),
                       self._aps(sub, lhsT, rhs), self._aps(sub, out), inc=inc)

    def tr(self, out, in_, ident, inc=True, sub=None):
        return self.op("pe", lambda e: e.transpose(out, in_, ident),
                       self._aps(sub, in_, ident), self._aps(sub, out), inc=inc)

    def act(self, out, in_, func, bias=None, scale=None, accum_out=None, sub=None):
        kw = {}
        if bias is not None:
            kw["bias"] = bias
        if scale is not None:
            kw["scale"] = scale
        if accum_out is not None:
            kw["accum_out"] = accum_out
        return self.op("act", lambda e: e.activation(out=out, in_=in_, func=func, **kw),
                       self._aps(sub, in_, bias, scale), self._aps(sub, out, accum_out))

    def ts(self, eng, out, in0, s1, s2, op0, op1=None, accum_out=None, sub=None):
        kw = {}
        if op1 is not None:
            kw["op1"] = op1
        if accum_out is not None:
            kw["accum_out"] = accum_out
        return self.op(eng, lambda e: e.tensor_scalar(out=out, in0=in0, scalar1=s1, scalar2=s2, op0=op0, **kw),
                       self._aps(sub, in0, s1, s2), self._aps(sub, out, accum_out))

    def stt(self, out, in0, scalar, in1, op0, op1, sub=None):
        return self.op("dve", lambda e: e.scalar_tensor_tensor(out=out, in0=in0, scalar=scalar, in1=in1, op0=op0, op1=op1),
                       self._aps(sub, in0, scalar, in1), self._aps(sub, out))

    def tt(self, eng, out, in0, in1, op, sub=None):
        return self.op(eng, lambda e: e.tensor_tensor(out=out, in0=in0, in1=in1, op=op),
                       self._aps(sub, in0, in1), self._aps(sub, out))

    def copy(self, eng, out, in_, sub=None):
        if eng == "act":
            return self.act(out, in_, AF.Copy, sub=sub)
        return self.op(eng, lambda e: e.tensor_copy(out=out, in_=in_), self._aps(sub, in_), self._aps(sub, out))

    def memset(self, eng, ap, val, sub=None):
        return self.op(eng, lambda e: e.memset(ap, val), [], self._aps(sub, ap))

    def recip(self, out, in_, sub=None):
        return self.op("dve", lambda e: e.reciprocal(out=out, in_=in_), self._aps(sub, in_), self._aps(sub, out))

    def reduce(self, out, in_, op, sub=None):
        return self.op("dve", lambda e: e.tensor_reduce(out=out, in_=in_, axis=AX.X, op=op),
                       self._aps(sub, in_), self._aps(sub, out))

    def scan(self, out, d0, d1, initial, op0, op1, sub=None):
        return self.op("dve", lambda e: e.tensor_tensor_scan(out=out, data0=d0, data1=d1, initial=initial, op0=op0, op1=op1),
                       self._aps(sub, d0, d1), self._aps(sub, out))


class Ctx:
    pass


def rmsnorm_fm(p, c, g_ap, hT, t0, nt, nparts=128, kch=8, src=None, scale_div=None):
    src = c.xT if src is None else src
    div = float(scale_div if scale_div is not None else nparts * kch)
    for i, tb in enumerate(range(t0, t0 + nt, 512)):
        sq = c.sq[i % 2]
        sub = {hT.name: (tb - t0) // 512}
        if src is c.xT:
            sub[c.xT.name] = tb // 512
        p.act(sq[:nparts, :kch, :], src[:nparts, :kch, tb:tb + 512], AF.Square, sub=sub)
        ps = c.ps[c.psi % 8]
        c.psi += 1
        for k in range(kch):
            p.mm(ps[:nparts, :], c.ones_b[:nparts, :nparts], sq[:nparts, k, :], start=(k == 0), stop=(k == kch - 1))
        rs = c.rs[i % 2]
        p.act(rs[:nparts, :], ps[:nparts, :], AF.Ln, bias=c.eps_ap[:nparts, :], scale=1.0 / div)
        p.act(rs[:nparts, :], rs[:nparts, :], AF.Exp, scale=-0.5)
        for k in range(kch):
            p.stt(hT[:nparts, k, tb - t0:tb - t0 + 512], src[:nparts, k, tb:tb + 512], g_ap[:nparts, k:k + 1],
                  rs[:nparts, :], ALU.mult, ALU.mult, sub=sub)


def proj_multi(p, c, hT, jobs, kch=8, wname="wb"):
    ntok = hT.shape[2]
    wbs = c.wb[wname]
    flat = [(ji, b) for ji, (mode, w_l, consume) in enumerate(jobs) for b in range(w_l.shape[0])]

    def load(i):
        ji, b = flat[i]
        w_l = jobs[ji][1]
        cw = w_l.shape[3]
        p.dma("pool", wbs[i % 2][:, :kch, :cw], w_l[b])

    load(0)
    for i, (ji, b) in enumerate(flat):
        mode, w_l, consume = jobs[ji]
        cw = w_l.shape[3]
        if i + 1 < len(flat):
            load(i + 1)
        wb = wbs[i % 2]
        if mode == "fm":
            for tb in range(ntok // 512):
                ps = c.ps[c.psi % 8]
                c.psi += 1
                for k in range(kch):
                    p.mm(ps[:cw, :], wb[:, k, :cw], hT[:, k, tb * 512:(tb + 1) * 512], start=(k == 0), stop=(k == kch - 1),
                         sub={hT.name: tb})
                consume(b, tb, ps)
        else:
            for tt in range(ntok // 128):
                ps = c.ps[c.psi % 8]
                c.psi += 1
                for k in range(kch):
                    p.mm(ps[:, :cw], hT[:, k, tt * 128:(tt + 1) * 128], wb[:, k, :cw], start=(k == 0), stop=(k == kch - 1),
                         sub={hT.name: tt // 4})
                consume(b, tt, ps)


def proj_stream(p, c, mode, hT, w_l, consume, kch=8, tsl=None, wname="wb"):
    proj_multi(p, c, hT, [(mode, w_l, consume)], kch=kch, wname=wname)


def layer0_mixer(p, c, W):
    with p.phase():
        c.nmT = p.sb("nmT", [128, 136, 128], BF16)
        with p.phase():
            c.ik2 = p.sb("ik2", [128, 1, T], F32)
            c.iw_sb = p.sb("iw_sb", [128, NT, 16], F32)
            with p.phase():
                hT = p.sb("hT", [128, 8, T], BF16)
                c.wb = {"wb": [p.sb("wb%d" % i, [128, 8, 512], BF16) for i in range(2)]}
                with p.phase():
                    c.sq = [p.sb("sq%d" % i, [128, 8, 512], BF16) for i in range(2)]
                    c.rs = [p.sb("rs%d" % i, [128, 512], F32) for i in range(2)]
                    rmsnorm_fm(p, c, c.g_mix[0], hT, 0, T)
                    layer0_inproj_a(p, c, W, hT)
                layer0_gmlp(p, c, W, hT)
            if c.stop_after == "gmlp":
                return
            layer0_indexer(p, c, W)
        if c.stop_after == "indexer":
            return
        layer0_attention(p, c, W)
    if c.stop_after == "attn":
        return
    with p.phase():
        yT = p.sb("yT", [128, 8, T], BF16)
        for tb in range(4):
            p.dma("sp", yT[:, :, tb * 512:(tb + 1) * 512],
                  c.yT_d[:, :, tb * 512:(tb + 1) * 512].rearrange("k p t -> p k t"), sub={yT.name: tb})
        out_proj_residual(p, c, W["w_out0"], yT)


def layer0_inproj_a(p, c, W, hT):
    if True:
        stg = [p.sb("stg%d" % i, [128, T], BF16) for i in range(2)]
        cnt = [0]

        def cons_fm(dst, scale=None):
            def f(b, tb, ps):
                st = stg[b % 2]
                eng = "act" if (cnt[0] % 2 == 0) else "dve"
                cnt[0] += 1
                if scale is None:
                    p.copy(eng, st[:, tb * 512:(tb + 1) * 512], ps[:, :])
                elif eng == "act":
                    p.act(st[:, tb * 512:(tb + 1) * 512], ps[:, :], AF.Copy, scale=scale)
                else:
                    p.ts("dve", st[:, tb * 512:(tb + 1) * 512], ps[:, :], scale, None, ALU.mult)
                if tb == 3:
                    p.dma("sp", dst[b], st[:, :])
            return f

        jobs = [("fm", W["w_q"], cons_fm(c.qT_d, 0.125)), ("fm", W["w_k"], cons_fm(c.kT_d)),
                ("fm", W["w_iq"], cons_fm(c.iqT_d))]

        def cons_ik(b, tb, ps):
            p.copy("act", c.ik2[:, 0, tb * 512:(tb + 1) * 512], ps[:, :])
        jobs.append(("fm", W["w_ik2"], cons_ik))

        vst = [p.sb("vst%d" % i, [128, 512], BF16) for i in range(2)]

        def cons_v(b, tt, ps):
            st = vst[tt % 2]
            p.copy("act" if tt % 2 == 0 else "dve", st[:, :], ps[:, :])
            p.dma("sp", c.v_d[tt * 128:(tt + 1) * 128, :], st[:, :])
        jobs.append(("tm", W["w_v"], cons_v))

        def cons_iw(b, tt, ps):
            p.copy("dve", c.iw_sb[:, tt, :], ps[:, 0:16])
        jobs.append(("tm", W["w_iw"], cons_iw))
        proj_multi(p, c, hT, jobs)


def layer0_gmlp(p, c, W, hT):
    if True:
        u_all = p.sb("u_all", [128, NT, 512], BF16)
        vn_all = p.sb("vn_all", [128, NT, 512], BF16)
        vgf = [p.sb("vgf%d" % i, [128, 512], F32) for i in range(2)]
        junk = p.sb("junk", [128, 512], BF16)
        ssv = p.sb("ssv", [128, NT], F32)
        rsv = p.sb("rsv", [128, NT], F32)
        gn_bc = p.sb("gn_bc", [128, 512], F32)
        p.dma("sp", gn_bc[:, :], W["gmlp_norm"].partition_broadcast(128))

        def cons_u(b, tt, ps):
            p.act(u_all[:, tt, :], ps[:, :], AF.Gelu_apprx_tanh)
        jobs = [("tm", W["w_u"], cons_u)]

        def cons_vg(b, tt, ps):
            vg = vgf[tt % 2]
            p.act(vg[:, :], ps[:, :], AF.Gelu_apprx_tanh)
            p.op("dve", lambda e, vg=vg, tt=tt: e.tensor_tensor_scan(out=junk[:, :], data0=vg[:, :], data1=vg[:, :], initial=0.0,
                                                                      op0=ALU.bypass, op1=ALU.add) if False else
                 e.scalar_tensor_tensor(out=junk[:, :], in0=vg[:, :], scalar=1.0, in1=vg[:, :], op0=ALU.mult, op1=ALU.mult,
                                        accum_out=ssv[:, tt:tt + 1]),
                 [p.key(vg)], [p.key(junk), p.key(ssv)])
            p.act(rsv[:, tt:tt + 1], ssv[:, tt:tt + 1], AF.Sqrt, bias=c.eps_ap[:, :], scale=1.0 / 512)
            p.recip(rsv[:, tt:tt + 1], rsv[:, tt:tt + 1])
            p.stt(vn_all[:, tt, :], vg[:, :], rsv[:, tt:tt + 1], gn_bc[:, :], ALU.mult, ALU.mult)
        jobs.append(("tm", W["w_vg"], cons_vg))
        proj_multi(p, c, hT, jobs)

        wsT = p.sb("wsT", [128, 8, 128], BF16)
        p.dma("pool", wsT[:, :, :], W["w_sT"])
        p.memset("dve", wsT[64:128, :, 0:64], 0.0)
        bs8 = p.sb("bs8", [8, 128], BF16)
        p.dma("pool", bs8[:, :], W["b_s"])
        e8 = p.sb("e8", [8, 512], BF16)
        p.dma("pool", e8[:, :], W["e8"])
        ybs = [p.sb("ybs%d" % i, [128, 512], BF16) for i in range(2)]
        ybT = [p.sb("ybT%d" % i, [128, 512], BF16) for i in range(2)]
        for n in range(NT):
            ps = c.ps[c.psi % 8]
            c.psi += 1
            for g in range(8):
                p.mm(ps[:, g * 64:(g + 1) * 64], wsT[:, g, :], vn_all[:, n, g * 64:(g + 1) * 64], start=True, stop=False, inc=False)
                p.mm(ps[:, g * 64:(g + 1) * 64], bs8[:, :], e8[:, g * 64:(g + 1) * 64], start=False, stop=True, inc=(g == 7))
            yb = ybs[n % 2]
            p.tt("dve", yb[:, :], ps[:, :], u_all[:, n, :], ALU.mult)
            pst = c.psb[c.psi % 8]
            pstn = c.ps[c.psi % 8]
            c.psi += 1
            for cc in range(4):
                p.op("pe", lambda e, cc=cc: e.transpose(pst[:, cc * 128:(cc + 1) * 128], yb[:, cc * 128:(cc + 1) * 128], c.ident_b[:, :]),
                     [p.key(yb), p.key(c.ident_b)], [p.key(pstn)], inc=(cc == 3))
            yst = ybT[n % 2]
            p.copy("act", yst[:, :], pst[:, 0:512])
            p.dma("sp", c.yT_d[4:8, :, n * 128:(n + 1) * 128].rearrange("c p t -> p c t"),
                  yst[:, :].rearrange("p (a b) -> p a b", a=4))


def tri(b):
    return b * (b + 1) // 2


def layer0_indexer(p, c, W):
    with p.phase():
        ikn = p.sb("ikn", [128, 1, T], BF16)
        gk2 = p.sb("gk2", [128, 1], F32)
        p.dma("sp", gk2[:, :], W["gk2"])
        with p.phase():
            c.sq = [p.sb("isq%d" % i, [128, 1, 512], BF16) for i in range(2)]
            c.rs = [p.sb("irs%d" % i, [128, 512], F32) for i in range(2)]
            rmsnorm_fm(p, c, gk2, ikn, 0, T, nparts=128, kch=1, src=c.ik2, scale_div=128)
        wabs = p.sb("wabs", [128, NT, 16], F32)
        sgn = p.sb("sgn", [128, NT, 16], F32)
        p.ts("dve", wabs[:, :, :], c.iw_sb[:, :, :], -1.0, None, ALU.mult)
        p.tt("dve", wabs[:, :, :], wabs[:, :, :], c.iw_sb[:, :, :], ALU.max)
        p.ts("dve", sgn[:, :, :], c.iw_sb[:, :, :], 0.0, 2.0, ALU.is_ge, ALU.mult)
        p.ts("dve", sgn[:, :, :], sgn[:, :, :], -1.0, None, ALU.add)
        sgn_b = p.sb("sgn_b", [128, NT, 16], BF16)
        p.copy("dve", sgn_b[:, :, :], sgn[:, :, :])
        iqz = [p.sb("iqz%d" % i, [128, 8, 2, 128], BF16) for i in range(2)]
        for i in range(2):
            p.memset("pool", iqz[i][:, :, :, :], 0.0)
        Dsg = [p.sb("Dsg%d" % i, [128, 16, 128], BF16) for i in range(4)]
        acc = [p.sb("acc%d" % i, [128, T], F32) for i in range(4)]
        rr = [p.sb("rr%d" % i, [128, 1024], BF16) for i in range(3)]
        nm = [p.sb("nm%d" % i, [128, T], BF16) for i in range(4)]
        junk = [p.sb("junkc%d" % i, [128, T], BF16) for i in range(2)]
        pow2 = p.sb("pow2", [128, NBIS], F32)
        p.dma("sp", pow2[:, :], W["pow2"])
        st = [p.sb("bis%d" % b, [128, 8], F32) for b in range(NT)]
        wtab = [p.sb("wtab%d" % b, [128, NBIS], F32) for b in range(NT)]
        cnt_ = {"ri": 0, "ei": 0, "si": 0}

        def jobs_for(b):
            L = 128 * (b + 1)
            units = [(0, min(L, 1024))] + ([(1024, L)] if L > 1024 else [])
            return [(b, k0, k1, h) for (k0, k1) in units for h in range(16)]

        def load_iq(b):
            iz = iqz[b % 2]
            for par in range(2):
                p.dma("sp", iz[par * 64:(par + 1) * 64, :, par, :],
                      c.iqT_d[:, par * 64:(par + 1) * 64, b * 128:(b + 1) * 128].rearrange("m p t -> p m t"))

        def emit_score(job):
            b, k0, k1, h = job
            n = k1 - k0
            nb = (n + 511) // 512
            m, par = divmod(h, 2)
            slot = 1 + cnt_["si"] % 3
            cnt_["si"] += 1
            psc = c.psum[:, slot * 1024:slot * 1024 + n]
            for kb in range(nb):
                c0, c1 = kb * 512, min(n, (kb + 1) * 512)
                p.mm(psc[:, c0:c1], iqz[b % 2][:, m, par, :], ikn[:, 0, k0 + c0:k0 + c1])
            return psc

        def emit_rest(job, psc):
            b, k0, k1, h = job
            n = k1 - k0
            nb = (n + 511) // 512
            pacc = c.psum[:, 0:n]
            r = rr[cnt_["ri"] % 3]
            cnt_["ri"] += 1
            p.act(r[:, :n], psc, AF.Relu, scale=wabs[:, b, h:h + 1])
            for kb in range(nb):
                c0, c1 = kb * 512, min(n, (kb + 1) * 512)
                p.mm(pacc[:, c0:c1], Dsg[b % 4][:, h, :], r[:, c0:c1], start=(h == 0), stop=(h == 15))
            if h == 15:
                p.copy("act", acc[b % 4][:, k0:k1], pacc)

        def scores(bs):
            jobs = []
            for b in bs:
                load_iq(b)
                jobs += jobs_for(b)
            psc = emit_score(jobs[0])
            for i, job in enumerate(jobs):
                nxt = emit_score(jobs[i + 1]) if i + 1 < len(jobs) else None
                emit_rest(job, psc)
                psc = nxt

        def build_dsg(b):
            for h in range(16):
                p.tt("pool", Dsg[b % 4][:, h, :], c.ident_b[:, :], sgn_b[:, b, h:h + 1].to_broadcast([128, 128]), ALU.mult)

        def bisect(bs):
            S = {}
            for i_, b in enumerate(bs):
                L = 128 * (b + 1)
                ac = acc[b % 4]
                mx, mn, w0, cand, cnt, tq = [st[b][:, i:i + 1] for i in range(6)]
                S[b] = (L, ac, cand, cnt, tq, junk[i_ % 2])
                p.reduce(mx, ac[:, :L], ALU.max)
                p.reduce(mn, ac[:, :L], ALU.min)
                p.memset("dve", ac[0:64, L - 64:L], -1e30)
                p.tt("dve", w0, mx, mn, ALU.subtract)
                p.ts("dve", wtab[b][:, :], pow2[:, :], w0, None, ALU.mult)
                p.tt("dve", cand, mn, wtab[b][:, 0:1], ALU.add)
            for i in range(NBIS):
                last = (i == NBIS - 1)
                for b in bs:
                    L, ac, cand, cnt, tq, jk = S[b]
                    p.ts("dve", jk[:, :L], ac[:, :L], cand, 0.0, ALU.is_ge, ALU.add, accum_out=cnt)
                for b in bs:
                    L, ac, cand, cnt, tq, jk = S[b]
                    p.ts("dve", tq, cnt, float(TOPK), (1.0 if last else 0.5), ALU.is_ge, ALU.subtract)
                for b in bs:
                    L, ac, cand, cnt, tq, jk = S[b]
                    p.stt(cand, tq, wtab[b][:, i:i + 1], cand, ALU.mult, ALU.add)
            for b in bs:
                L, ac, cand, cnt, tq, jk = S[b]
                p.ts("dve", nm[b % 4][:, :L], ac[:, :L], cand, NEG, ALU.is_lt, ALU.mult)

        def transposes(b):
            nmb = nm[b % 4]
            for a0 in range(0, b + 1, 4):
                g = min(4, b + 1 - a0)
                slot = 1 + cnt_["si"] % 3
                cnt_["si"] += 1
                pstn = c.ps[2 * slot]
                pst = c.psb[2 * slot]
                for j in range(g):
                    a = a0 + j
                    p.op("pe", lambda e, j=j, a=a: e.transpose(pst[:, j * 128:(j + 1) * 128], nmb[:, a * 128:(a + 1) * 128], c.ident_b[:, :]),
                         [p.key(nmb), p.key(c.ident_b)], [p.key(pstn)], inc=(j == g - 1))
                o_ap = c.nmT[:, tri(b) + a0:tri(b) + a0 + g, :]
                i_ap = pst[:, 0:g * 128].rearrange("p (a b) -> p a b", a=g)
                p.op("act", lambda e: e.activation(out=o_ap, in_=i_ap, func=AF.Copy), [p.key(pstn)], [p.key(c.nmT, {c.nmT.name: b})])

        for b in range(2):
            L = 128 * (b + 1)
            p.memset("pool", nm[b][:, :L], 0.0)
            p.memset("pool", nm[b][0:64, L - 64:L], NEG)
        prev = [0, 1]
        order = list(range(NT - 2, 1, -2))
        build_dsg(order[0])
        build_dsg(order[0] + 1)
        for oi, b0 in enumerate(order):
            bs = [b0, b0 + 1]
            if oi + 1 < len(order):
                build_dsg(order[oi + 1])
                build_dsg(order[oi + 1] + 1)
            scores(bs)
            for b in prev:
                transposes(b)
            bisect(bs)
            prev = bs
        for b in prev:
            transposes(b)


def layer0_attention(p, c, W):
    with p.phase():
        kT = p.sb("kT", [128, 4, T], BF16)
        for m in range(4):
            p.dma("sp", kT[:, m, :], c.kT_d[m])
        qzs = [p.sb("qz%d" % i, [128, 4, 2, 128], BF16) for i in range(2)]
        for i in range(2):
            p.memset("pool", qzs[i][:, :, :, :], 0.0)
        va = p.sb("va", [128, NT, 8, 65], BF16)
        p.memset("pool", va[:, :, :, 64:65], 1.0)
        for tt in range(NT):
            p.dma("sp", va[:, tt, :, 0:64], c.v_d[tt * 128:(tt + 1) * 128, :].rearrange("p (h d) -> p h d", h=8))
        oh1 = p.sb("oh1", [32, 383], F32)
        p.dma("sp", oh1[:, :], W["oh1"])
        rb = p.sb("rb", [32, 8], F32)
        p.dma("sp", rb[:, :], W["rel_bias"])
        cb = p.sb("cb", [128, 8], F32)
        p.dma("sp", cb[:, :], W["rel_bias"][15:16, :].partition_broadcast(128))
        Tb = p.sb("Tb", [128, 2, 8, 128], BF16)
        for kind in range(2):
            for t0 in range(0, 128, 64):
                ps = c.ps[c.psi % 8]
                c.psi += 1
                for t in range(t0, t0 + 64):
                    base = (255 - t) if kind == 0 else (127 - t)
                    p.mm(ps[:, (t - t0) * 8:(t - t0) * 8 + 8], oh1[:, base:base + 128], rb[:, :], start=True, stop=True,
                         inc=(t == t0 + 63))
                p.tt("dve", Tb[:, kind, :, t0:t0 + 64], ps[:, 0:512].rearrange("p (t h) -> p h t", h=8),
                     cb[:, :].unsqueeze(2).to_broadcast([128, 8, 64]), ALU.subtract)
        ya = [p.sb("ya%d" % i, [128, 512], BF16) for i in range(2)]
        PT = [p.sb("PT%d" % i, [128, 1024], BF16) for i in range(3)]
        rec = p.sb("rec", [128, 16], F32)
        yaT = [p.sb("yaT%d" % i, [128, 512], BF16) for i in range(2)]
        si = 0
        pend = []

        def normalize(b, h, po):
            ri = (b * 8 + h) % 16
            p.recip(rec[:, ri:ri + 1], po[:, 64:65])
            p.act(ya[b % 2][:, h * 64:(h + 1) * 64], po[:, 0:64], AF.Copy, scale=rec[:, ri:ri + 1])
            if h == 7:
                pstn = c.ps[6 + b % 2]
                pst = c.psb[6 + b % 2]
                yab = ya[b % 2]
                for cc in range(4):
                    p.op("pe", lambda e, cc=cc: e.transpose(pst[:, cc * 128:(cc + 1) * 128], yab[:, cc * 128:(cc + 1) * 128], c.ident_b[:, :]),
                         [p.key(yab), p.key(c.ident_b)], [p.key(pstn)], inc=(cc == 3))
                yst = yaT[b % 2]
                p.copy("dve", yst[:, :], pst[:, 0:512])
                p.dma("sp", c.yT_d[0:4, :, b * 128:(b + 1) * 128].rearrange("c p t -> p c t"),
                      yst[:, :].rearrange("p (a b) -> p a b", a=4))

        jobs = [(b, h, a0) for b in range(NT) for h in range(8) for a0 in range(0, b + 1, 8)]

        def emit_score(job, idx):
            b, h, a0 = job
            qz = qzs[b % 2]
            if h == 0 and a0 == 0:
                for par in range(2):
                    p.dma("sp", qz[par * 64:(par + 1) * 64, :, par, :],
                          c.qT_d[:, par * 64:(par + 1) * 64, b * 128:(b + 1) * 128].rearrange("m p t -> p m t"))
            m = h // 2
            grp = list(range(a0, min(a0 + 8, b + 1)))
            n = len(grp) * 128
            slot = idx % 2
            ps = c.psum[:, slot * 1024:slot * 1024 + n]
            for j, a in enumerate(grp):
                near = (a >= b - 1)
                lastj = (j == len(grp) - 1)
                o = ps[:, j * 128:(j + 1) * 128]
                p.mm(o, kT[:, m, a * 128:(a + 1) * 128], qz[:, h // 2, h % 2, :], start=True, stop=False, inc=False)
                p.mm(o, c.ident_b[:, :], c.nmT[:, tri(b) + a, :], start=False, stop=(not near), inc=((not near) and lastj))
                if near:
                    kind = 0 if a == b else 1
                    p.mm(o, c.ident_b[:, :], Tb[:, kind, h, :], start=False, stop=True, inc=lastj)
            return ps

        def emit_rest(job, idx, ps):
            nonlocal pend
            b, h, a0 = job
            grp = list(range(a0, min(a0 + 8, b + 1)))
            n = len(grp) * 128
            po = c.ps[4 + (b * 8 + h) % 2]
            pt = PT[idx % 3]
            p.act(pt[:, :n], ps, AF.Exp, bias=cb[:, h:h + 1])
            for j, a in enumerate(grp):
                p.mm(po[:, 0:65], pt[:, j * 128:(j + 1) * 128], va[:, a, h, :], start=(a == 0), stop=(a == b))
            if grp[-1] == b:
                for f in pend:
                    f()
                pend = [lambda b=b, h=h, po=po: normalize(b, h, po)]

        ps = emit_score(jobs[0], 0)
        for i, job in enumerate(jobs):
            nxt = emit_score(jobs[i + 1], i + 1) if i + 1 < len(jobs) else None
            emit_rest(job, i, ps)
            ps = nxt
        for f in pend:
            f()


def out_proj_residual(p, c, w_l, src):
    kch = w_l.shape[2]
    with p.phase():
        c.wb = {"wb": [p.sb("wbo%d" % i, [128, kch, 128], BF16) for i in range(2)]}

        def cons(b, tb, ps):
            p.tt("dve", c.xT[:, b, tb * 512:(tb + 1) * 512], ps[:, :], c.xT[:, b, tb * 512:(tb + 1) * 512], ALU.add)
        proj_stream(p, c, "fm", src, w_l, cons, kch=kch)


def conv_ffn(p, c, W, l):
    w_up = W["w_up%d" % l]
    w_dn = W["w_dn%d" % l]
    with p.phase():
        cw = p.sb("cw", [128, 44, 3], F32)
        cbs = p.sb("cbs", [128, 44], F32)
        p.dma("sp", cw[:, :, :], W["ffn_cw%d" % l])
        p.dma("sp", cbs[:, :], W["ffn_cb%d" % l])
        halo = p.sb("halo", [128, 44, 2], F32)
        for half in range(2):
            t0 = half * 1024
            with p.phase():
                hT = p.sb("hTf", [128, 8, 1024], BF16)
                gT = p.sb("gT", [128, NJ, 1024], BF16)
                with p.phase():
                    c.sq = [p.sb("fsq%d" % i, [128, 8, 512], BF16) for i in range(2)]
                    c.rs = [p.sb("frs%d" % i, [128, 512], F32) for i in range(2)]
                    rmsnorm_fm(p, c, c.g_ffn[l], hT, t0, 1024)
                wu = [[p.sb("wu%d_%d" % (sd, i), [128, 8, 128], BF16) for i in range(2)] for sd in range(2)]
                wd = [p.sb("wd%d" % i, [128, NJ, 128], BF16) for i in range(2)]
                ya = [p.sb("fya%d" % i, [128, 512], F32) for i in range(3)]
                yb = [p.sb("fyb%d" % i, [128, 512], F32) for i in range(3)]
                sa = [p.sb("fsa%d" % i, [128, 512], F32) for i in range(2)]
                hl = [p.sb("fhl%d" % i, [128, 2], F32) for i in range(4)]
                hli = 0

                def load(j):
                    p.dma("pool", wu[0][j % 2][:, :, :], w_up[j])
                    p.dma("pool", wu[1][j % 2][:, :, :], w_up[NJ + j])

                load(0)
                it = 0
                for j in range(NJ):
                    if j + 1 < NJ:
                        load(j + 1)
                    if j == NJ - 5:
                        p.dma("pool", wd[0][:, :, :], w_dn[0])
                    if j == NJ - 3:
                        p.dma("pool", wd[1][:, :, :], w_dn[1])
                    for tb in range(2):
                        ys = []
                        for sd in range(2):
                            m = sd * NJ + j
                            ps = c.ps[c.psi % 8]
                            c.psi += 1
                            wt = wu[sd][j % 2]
                            for k in range(8):
                                p.mm(ps[:, :], wt[:, k, :], hT[:, k, tb * 512:(tb + 1) * 512], start=(k == 0), stop=(k == 7),
                                     sub={hT.name: tb})
                            y = (ya if sd == 0 else yb)[it % 3]
                            p.act(y[:, :], ps[:, :], AF.Identity, bias=cbs[:, m:m + 1], scale=cw[:, m, 2:3])
                            p.stt(y[:, 1:512], ps[:, 0:511], cw[:, m, 1:2], y[:, 1:512], ALU.mult, ALU.add)
                            p.stt(y[:, 2:512], ps[:, 0:510], cw[:, m, 0:1], y[:, 2:512], ALU.mult, ALU.add)
                            first = (half == 0 and tb == 0)
                            if not first:
                                if tb == 0:
                                    hsrc = halo[:, m, :]
                                else:
                                    hsrc = hprev[sd][:, :]
                                p.stt(y[:, 0:1], hsrc[:, 1:2], cw[:, m, 1:2], y[:, 0:1], ALU.mult, ALU.add)
                                p.stt(y[:, 0:2], hsrc[:, 0:2], cw[:, m, 0:1], y[:, 0:2], ALU.mult, ALU.add)
                            if tb == 0:
                                if sd == 0:
                                    hprev = [None, None]
                                hprev[sd] = hl[hli % 4]
                                hli += 1
                                p.copy("act", hprev[sd][:, :], ps[:, 510:512])
                            elif half == 0:
                                p.copy("act", halo[:, m, :], ps[:, 510:512])
                            ys.append(y)
                        sg = sa[it % 2]
                        p.act(sg[:, :], ys[0][:, :], AF.Silu)
                        p.tt("pool", gT[:, j, tb * 512:(tb + 1) * 512], sg[:, :], ys[1][:, :], ALU.mult)
                        it += 1
                for dc in range(8):
                    for tb in range(2):
                        ps = c.ps[c.psi % 8]
                        c.psi += 1
                        for j in range(NJ):
                            p.mm(ps[:, :], wd[dc % 2][:, j, :], gT[:, j, tb * 512:(tb + 1) * 512], start=(j == 0), stop=(j == NJ - 1))
                        sl = c.xT[:, dc, t0 + tb * 512:t0 + (tb + 1) * 512]
                        p.tt("dve", sl, ps[:, :], sl, ALU.add)
                    if dc + 2 < 8:
                        p.dma("pool", wd[dc % 2][:, :, :], w_dn[dc + 2])


def hgrn_mixer(p, c, W):
    with p.phase():
        hT = p.sb("hT1", [128, 8, T], BF16)
        c.wb = {"wb": [p.sb("wbh%d" % i, [128, 8, 512], BF16) for i in range(2)]}
        lbr = p.sb("lbr", [128, 2, 8], F32)
        p.dma("sp", lbr[:, :, :], W["hgrn_lb"])
        lb = p.sb("lb", [128, 8], F32)
        oml = p.sb("oml", [128, 8], F32)
        p.tt("dve", lb[:, :], lbr[:, 1, :], lbr[:, 0, :], ALU.subtract)
        p.act(lb[:, :], lb[:, :], AF.Sigmoid)
        p.ts("dve", oml[:, :], lb[:, :], -1.0, 1.0, ALU.mult, ALU.add)
        with p.phase():
            c.sq = [p.sb("hsq%d" % i, [128, 8, 512], BF16) for i in range(2)]
            c.rs = [p.sb("hrs%d" % i, [128, 512], F32) for i in range(2)]
            rmsnorm_fm(p, c, c.g_mix[1], hT, 0, T)
        stg = [p.sb("hstg%d" % i, [128, T], BF16) for i in range(2)]

        def cons_silu(dst):
            def f(b, tb, ps):
                st = stg[b % 2]
                p.act(st[:, tb * 512:(tb + 1) * 512], ps[:, :], AF.Silu)
                if tb == 3:
                    p.dma("sp", dst[b], st[:, :])
            return f
        jobs = [("fm", W["w_cq"], cons_silu(c.qh_d)), ("fm", W["w_cg"], cons_silu(c.gate_d))]

        gst = [p.sb("gst%d" % i, [128, T], F32) for i in range(2)]
        sgt = [p.sb("sgt%d" % i, [128, 512], F32) for i in range(2)]

        def cons_f(b, tb, ps):
            g = gst[b % 2]
            sg = sgt[tb % 2]
            p.act(sg[:, :], ps[:, :], AF.Sigmoid)
            p.ts("dve", g[:, tb * 512:(tb + 1) * 512], sg[:, :], oml[:, b:b + 1], lb[:, b:b + 1], ALU.mult, ALU.add)
            if tb == 3:
                st = stg[b % 2]
                p.ts("dve", st[:, :], g[:, :], -1.0, 1.0, ALU.mult, ALU.add)
                p.dma("sp", c.kk_d[b], st[:, :])
                p.act(g[:, :], g[:, :], AF.Ln)
                p.dma("sp", c.lg_d[b], g[:, :])
        jobs.append(("fm", W["w_cf"], cons_f))

        ist = [p.sb("ist%d" % i, [128, 512], BF16) for i in range(2)]

        def cons_i(b, tt, ps):
            st = ist[tt % 2]
            p.copy("act" if tt % 2 == 0 else "dve", st[:, :], ps[:, :])
            p.dma("sp", c.i_d[tt * 128:(tt + 1) * 128, b * 512:(b + 1) * 512], st[:, :])
        jobs.append(("tm", W["w_ci"], cons_i))
        proj_multi(p, c, hT, jobs)
    if c.stop_after == "hproj":
        return
    with p.phase():
        cm = p.sb("cm", [128, 32, 64], BF16)
        p.memset("pool", cm[:, :, :], 1.0)
        p.memset("pool", cm[:, :, 0:1], 0.0)
        tri64 = p.sb("tri64", [64, 64], F32)
        p.dma("sp", tri64[:, :], W["tri64"])
        cn = p.sb("cn", [128, 1], F32)
        p.dma("sp", cn[:, :], W["c_out_norm"])
        lg = p.sb("lg", [128, T], F32)
        bb = p.sb("bb", [128, T], F32)
        eb = p.sb("eb", [128, T], F32)
        t1 = p.sb("t1", [128, T], F32)
        qh = p.sb("qh", [128, T], BF16)
        kk = p.sb("kk", [128, T], BF16)
        KlT = p.sb("KlT", [128, T], BF16)
        Qb = [p.sb("Qb%d" % i, [128, T], BF16) for i in range(2)]
        Sb = [p.sb("Sb%d" % i, [128, 128], BF16) for i in range(2)]
        Kbb = [p.sb("Kbb%d" % i, [128, T], BF16) for i in range(2)]
        Klc = [p.sb("Klc%d" % i, [64, 32, 128], BF16) for i in range(2)]
        ic = [p.sb("ic%d" % i, [64, 32, 128], BF16) for i in range(2)]
        ebl = [p.sb("ebl%d" % i, [128, 32], F32) for i in range(2)]
        gt = [p.sb("gt%d" % i, [128, T], BF16) for i in range(2)]
        oTt = [p.sb("oTt%d" % i, [128, 512], F32) for i in range(2)]
        Sf = [p.sb("Sf%d" % i, [128, 128], F32) for i in range(2)]
        ATs = [p.sb("ATs%d" % i, [64, 64], BF16) for i in range(3)]
        osq = [p.sb("osq%d" % i, [128, 512], BF16) for i in range(2)]
        ors = [p.sb("ors%d" % i, [128, 512], F32) for i in range(2)]
        on = [p.sb("on%d" % i, [128, 512], F32) for i in range(2)]
        ost = [p.sb("ost%d" % i, [128, 512], BF16) for i in range(2)]

        def precompute_steps(h):
            u = h % 2
            ebv = eb[:, :].rearrange("p (a b) -> p a b", b=64)
            st = []

            def loads():
                p.dma("sp", lg[:, :], c.lg_d[h])
                p.dma("sp", qh[:, :], c.qh_d[h])
                p.dma("sp", kk[:, :], c.kk_d[h])
                p.dma("sp", gt[u][:, :], c.gate_d[h])
                p.dma("sp", ic[u][:, :, :], c.i_d[:, h * 128:(h + 1) * 128].rearrange("(n s) e -> s n e", s=64))
            st.append(loads)
            st.append(lambda: p.scan(bb[:, :], cm[:, :, :].rearrange("p a b -> p (a b)"), lg[:, :], 0.0, ALU.mult, ALU.add))
            st.append(lambda: p.act(eb[:, :], bb[:, :], AF.Exp))
            st.append(lambda: p.act(t1[:, :], bb[:, :], AF.Exp, scale=-1.0))
            st.append(lambda: p.tt("pool", Qb[u][:, :], qh[:, :], eb[:, :], ALU.mult))
            st.append(lambda: p.tt("pool", t1[:, :], kk[:, :], t1[:, :], ALU.mult))
            st.append(lambda: p.copy("act", ebl[u][:, :], ebv[:, :, 63]))
            st.append(lambda: p.copy("act", Kbb[u][:, :], t1[:, :]))
            st.append(lambda: p.tt("pool", KlT[:, :].rearrange("p (a b) -> p a b", b=64), t1[:, :].rearrange("p (a b) -> p a b", b=64),
                                   ebv[:, :, 63:64].to_broadcast([128, 32, 64]), ALU.mult))

            def trs(g8):
                pstn = c.ps[7]
                pst = c.psb[7]
                for j in range(8):
                    n = g8 * 8 + j
                    p.op("pe", lambda e, j=j, n=n: e.transpose(pst[0:64, j * 128:(j + 1) * 128], KlT[:, n * 64:(n + 1) * 64], c.ident_b[:, :]),
                         [p.key(KlT), p.key(c.ident_b)], [p.key(pstn)], inc=(j == 7))
                p.op("act", lambda e: e.activation(out=Klc[u][:, g8 * 8:(g8 + 1) * 8, :],
                                                   in_=pst[0:64, :].rearrange("p (a b) -> p a b", a=8), func=AF.Copy),
                     [p.key(pstn)], [p.key(Klc[u])])
            for g8 in range(4):
                st.append(lambda g8=g8: trs(g8))
            return st

        def chunkloop(h, bg):
            u = h % 2

            def front(n):
                cs = slice(n * 64, (n + 1) * 64)
                if n < 31:
                    psS = c.ps[4 + n % 3]
                    p.mm(psS[:, 0:128], Klc[u][:, n, :], ic[u][:, n, :])
                psA = c.ps[n % 2]
                p.mm(psA[0:64, 0:64], Kbb[u][:, cs], Qb[u][:, cs])
                p.tt("dve", ATs[n % 3][:, :], psA[0:64, 0:64], tri64[:, :], ALU.mult)

            def outnorm_steps(tb, po):
                ts_ = slice(tb * 512, (tb + 1) * 512)
                oT = oTt[tb % 2]
                sq = osq[tb % 2]
                rs = ors[tb % 2]
                o2 = on[tb % 2]
                o3 = ost[tb % 2]
                ps = c.ps[7]

                def s1():
                    p.copy("act", oT[:, :], po[:, :])
                    p.act(sq[:, :], oT[:, :], AF.Square)

                def s2():
                    p.mm(ps[:, :], c.ones_b[:, :], sq[:, :])

                def s3():
                    p.act(rs[:, :], ps[:, :], AF.Ln, bias=c.eps_ap[:, :], scale=1.0 / 128)
                    p.act(rs[:, :], rs[:, :], AF.Exp, scale=-0.5)
                    p.stt(o2[:, :], oT[:, :], cn[:, 0:1], rs[:, :], ALU.mult, ALU.mult)
                    p.tt("pool", o3[:, :], o2[:, :], gt[u][:, ts_], ALU.mult)
                    p.dma("pool", c.oT_d[h][:, ts_], o3[:, :])
                return [s1, s2, s3]

            front(0)
            po = None
            for n in range(32):
                cs = slice(n * 64, (n + 1) * 64)
                if n + 1 < 32:
                    front(n + 1)
                if n % 8 == 0:
                    po = c.ps[2 + (n // 8) % 2]
                oc = po[:, (n % 8) * 64:(n % 8 + 1) * 64]
                p.mm(oc, ic[u][:, n, :], ATs[n % 3][:, :], start=True, stop=(n == 0), inc=(n == 0))
                if n > 0:
                    p.mm(oc, Sb[(n - 1) % 2][:, :], Qb[u][:, cs], start=False, stop=True, inc=True)
                if n < 31:
                    psS = c.ps[4 + n % 3]
                    if n == 0:
                        p.copy("dve", Sf[0][:, :], psS[:, 0:128])
                    else:
                        p.stt(Sf[n % 2][:, :], Sf[(n - 1) % 2][:, :], ebl[u][:, n:n + 1], psS[:, 0:128], ALU.mult, ALU.add)
                    p.copy("act", Sb[n % 2][:, :], Sf[n % 2][:, :])
                if n % 8 == 7:
                    for f in reversed(outnorm_steps(n // 8, po)):
                        bg.insert(0, f)
                if bg:
                    bg.pop(0)()
            while bg:
                bg.pop(0)()

        for f in precompute_steps(0):
            f()
        for h in range(8):
            bg = precompute_steps(h + 1) if h + 1 < 8 else []
            chunkloop(h, bg)
    if c.stop_after == "hrec":
        return
    with p.phase():
        oTa = p.sb("oTa", [128, 8, T], BF16)
        for tb in range(4):
            p.dma("sp", oTa[:, :, tb * 512:(tb + 1) * 512],
                  c.oT_d[:, :, tb * 512:(tb + 1) * 512].rearrange("k p t -> p k t"), sub={oTa.name: tb})
        out_proj_residual(p, c, W["w_out1"], oTa)


def final_norm(p, c, out):
    with p.phase():
        c.sq = [p.sb("nsq%d" % i, [128, 8, 512], BF16) for i in range(2)]
        c.rs = [p.sb("nrs%d" % i, [128, 512], F32) for i in range(2)]
        ot = [p.sb("fot%d" % i, [128, 8, 512], F32) for i in range(2)]
        for i, tb in enumerate(range(0, T, 512)):
            sq = c.sq[i % 2]
            p.act(sq[:, :, :], c.xT[:, :, tb:tb + 512], AF.Square)
            ps = c.ps[c.psi % 8]
            c.psi += 1
            for k in range(8):
                p.mm(ps[:, :], c.ones_b[:, :], sq[:, k, :], start=(k == 0), stop=(k == 7))
            rs = c.rs[i % 2]
            p.act(rs[:, :], ps[:, :], AF.Ln, bias=c.eps_ap[:, :], scale=1.0 / D)
            p.act(rs[:, :], rs[:, :], AF.Exp, scale=-0.5)
            o = ot[i % 2]
            for k in range(8):
                p.stt(o[:, k, :], c.xT[:, k, tb:tb + 512], c.g_fin[:, k:k + 1], rs[:, :], ALU.mult, ALU.mult)
            p.dma("sp", out[:, :, tb:tb + 512].rearrange("c p t -> p c t"), o[:, :, :])

def lay(Wm, cw):
    K, N = Wm.shape
    return np.ascontiguousarray(Wm.reshape(K // 128, 128, N // cw, cw).transpose(2, 1, 0, 3))


def build(stop_after=None, dbg=()):
    nc = bass.Bass("TRN2", target_bir_lowering=False)
    p = Prog(nc)
    c = Ctx()
    W = {}

    def din(name, shape, dt=F32):
        W[name] = nc.dram_tensor(name, list(shape), dt, kind="ExternalInput").ap()
        return W[name]

    def dscr(name, shape, dt):
        kind = "ExternalOutput" if name in dbg else "Internal"
        return nc.dram_tensor(name, list(shape), dt, kind=kind).ap()

    din("xT_in", [8, 128, T])
    din("ident", [128, 128])
    din("g_mix", [2, 128, 8]); din("g_ffn", [2, 128, 8]); din("g_fin", [128, 8])
    din("w_q", [4, 128, 8, 128]); din("w_k", [4, 128, 8, 128]); din("w_iq", [8, 128, 8, 128])
    din("w_ik2", [1, 128, 8, 128]); din("w_v", [1, 128, 8, 512]); din("w_iw", [1, 128, 8, 16])
    din("w_u", [1, 128, 8, 512]); din("w_vg", [1, 128, 8, 512])
    din("gmlp_norm", [1, 512]); din("w_sT", [128, 8, 128]); din("b_s", [8, 128]); din("e8", [8, 512])
    din("gk2", [128, 1]); din("pow2", [128, NBIS]); din("oh1", [32, 383]); din("rel_bias", [32, 8])
    din("w_out0", [8, 128, 8, 128])
    for l in range(2):
        din("w_up%d" % l, [44, 128, 8, 128]); din("w_dn%d" % l, [8, 128, NJ, 128])
        din("ffn_cw%d" % l, [128, 44, 3]); din("ffn_cb%d" % l, [128, 44])
    din("w_cq", [8, 128, 8, 128]); din("w_cf", [8, 128, 8, 128]); din("w_cg", [8, 128, 8, 128]); din("w_ci", [2, 128, 8, 512])
    din("hgrn_lb", [128, 2, 8]); din("tri64", [64, 64]); din("c_out_norm", [128, 1]); din("w_out1", [8, 128, 8, 128])
    out = nc.dram_tensor("outT", [8, 128, T], F32, kind="ExternalOutput").ap()

    c.qT_d = dscr("qT_d", [4, 128, T], BF16)
    c.kT_d = dscr("kT_d", [4, 128, T], BF16)
    c.iqT_d = dscr("iqT_d", [8, 128, T], BF16)
    c.v_d = dscr("v_d", [T, 512], BF16)
    c.yT_d = dscr("yT_d", [8, 128, T], BF16)
    c.qh_d = dscr("qh_d", [8, 128, T], BF16)
    c.gate_d = dscr("gate_d", [8, 128, T], BF16)
    c.kk_d = dscr("kk_d", [8, 128, T], BF16)
    c.lg_d = dscr("lg_d", [8, 128, T], F32)
    c.i_d = dscr("i_d", [T, 1024], BF16)
    c.oT_d = dscr("oT_d", [8, 128, T], BF16)

    c.xT = p.sb("xT", [128, 8, T], F32, True)
    c.ident_f = p.sb("ident_f", [128, 128], F32, True)
    c.ident_b = p.sb("ident_b", [128, 128], BF16, True)
    c.ones_b = p.sb("ones_b", [128, 128], BF16, True)
    c.eps_ap = p.sb("eps_ap", [128, 1], F32, True)
    gm = p.sb("g_mix_sb", [128, 2, 8], F32, True)
    gf = p.sb("g_ffn_sb", [128, 2, 8], F32, True)
    gfin = p.sb("g_fin_sb", [128, 8], F32, True)
    c.psum = p.es.enter_context(nc.psum_tensor("psum", [128, 4096], F32))
    c.ps = [c.psum[:, i * 512:(i + 1) * 512] for i in range(8)]
    c.psb = [t.bitcast(BF16) for t in c.ps]
    c.psi = 0
    c.stop_after = stop_after
    c.dbg = dbg
    c.dscr = dscr

    p.dma("sp", c.ident_f[:, :], W["ident"])
    p.dma("pool", c.ident_b[:, :], W["ident"])
    p.memset("dve", c.ones_b[:, :], 1.0)
    p.memset("dve", c.eps_ap[:, :], EPS)
    for l in range(2):
        p.dma("sp", gm[:, l, :], W["g_mix"][l])
        p.dma("sp", gf[:, l, :], W["g_ffn"][l])
    p.dma("sp", gfin[:, :], W["g_fin"])
    for tb in range(4):
        p.dma("sp", c.xT[:, :, tb * 512:(tb + 1) * 512],
              W["xT_in"][:, :, tb * 512:(tb + 1) * 512].rearrange("k p t -> p k t"), sub={c.xT.name: tb})
    c.g_mix = [gm[:, l, :] for l in range(2)]
    c.g_ffn = [gf[:, l, :] for l in range(2)]
    c.g_fin = gfin

    layer0_mixer(p, c, W)
    stages = ["gmlp", "indexer", "attn", "mix0", "ffn0", "hproj", "hrec", "mix1", "ffn1", None]
    si_ = stages.index(stop_after)
    if si_ >= stages.index("ffn0"):
        conv_ffn(p, c, W, 0)
    if si_ >= stages.index("hproj"):
        hgrn_mixer(p, c, W)
    if si_ >= stages.index("ffn1"):
        conv_ffn(p, c, W, 1)
    if stop_after is None:
        final_norm(p, c, out)
        p.barrier()
        return nc, p, c

    for k in range(8):
        p.dma("sp", out[k], c.xT[:, k, :])
    p.barrier()
    return nc, p, c


def t5_onehot():
    rel = np.arange(-255, 128)
    half, max_exact = 16, 8
    ret = np.where(rel > 0, half, 0)
    n = np.abs(rel)
    nf = np.maximum(n, max_exact).astype(np.float32)
    large = max_exact + (np.log(nf / np.float32(max_exact)) / np.float32(np.log(128 / 8)) * np.float32(half - max_exact)).astype(np.int32)
    large = np.minimum(large, half - 1)
    bucket = ret + np.where(n < max_exact, n, large)
    oh = np.zeros((32, 383), np.float32)
    oh[bucket, np.arange(383)] = 1.0
    return oh


def host_inputs(inp, b):
    f = np.float32
    m = {}
    x = inp["x"][b]
    m["xT_in"] = np.ascontiguousarray(x.T.reshape(8, 128, T))
    m["ident"] = np.eye(128, dtype=f)
    m["g_mix"] = np.ascontiguousarray(inp["mix_norm"].reshape(2, 8, 128).transpose(0, 2, 1))
    m["g_ffn"] = np.ascontiguousarray(inp["ffn_norm"].reshape(2, 8, 128).transpose(0, 2, 1))
    m["g_fin"] = np.ascontiguousarray(inp["final_norm"].reshape(8, 128).T)
    Wi = inp["ab_w_in"][0]
    m["w_q"] = lay(Wi[:, 0:512], 128)
    m["w_k"] = lay(Wi[:, 512:1024], 128)
    m["w_v"] = lay(Wi[:, 1024:1536], 512)
    m["w_iq"] = lay(Wi[:, 1536:2560], 128)
    m["w_ik2"] = lay(np.concatenate([Wi[:, 2560:2624], Wi[:, 2560:2624]], axis=1), 128)
    m["w_iw"] = lay(Wi[:, 2624:2640], 16)
    m["w_u"] = lay(Wi[:, 2640:3152], 512)
    m["w_vg"] = lay(Wi[:, 3152:3664], 512)
    m["gmlp_norm"] = np.ascontiguousarray(inp["ab_gmlp_norm"][0].reshape(1, 512))
    m["w_sT"] = np.ascontiguousarray(inp["ab_w_s"][0].transpose(2, 0, 1))
    m["b_s"] = np.ascontiguousarray(inp["ab_b_s"][0])
    e8 = np.zeros((8, 512), f)
    for g in range(8):
        e8[g, g * 64:(g + 1) * 64] = 1.0
    m["e8"] = e8
    m["gk2"] = np.concatenate([inp["ab_idx_k_norm"][0], inp["ab_idx_k_norm"][0]]).reshape(128, 1)
    m["pow2"] = np.tile((0.5 ** np.arange(1, NBIS + 1)).astype(f)[None, :], (128, 1))
    m["oh1"] = t5_onehot()
    m["rel_bias"] = inp["rel_bias"]
    m["w_out0"] = lay(inp["ab_w_out"][0], 128)
    Wc = inp["c_w_in"][0]
    m["w_cq"] = lay(Wc[:, 0:1024], 128)
    m["w_cf"] = lay(Wc[:, 1024:2048], 128)
    m["w_ci"] = lay(Wc[:, 2048:3072], 512)
    m["w_cg"] = lay(Wc[:, 3072:4096], 128)
    m["hgrn_lb"] = inp["hgrn_lb"].reshape(2, 8, 128).transpose(2, 0, 1)
    m["tri64"] = np.triu(np.ones((64, 64), f))
    m["c_out_norm"] = inp["c_out_norm"][0].reshape(128, 1)
    m["w_out1"] = lay(inp["c_w_out"][0], 128)
    for l in range(2):
        m["w_up%d" % l] = lay(inp["ffn_w_up"][l], 128)
        m["w_dn%d" % l] = lay(inp["ffn_w_down"][l], 128)
        m["ffn_cw%d" % l] = inp["ffn_conv_w"][l].reshape(3, 44, 128).transpose(2, 1, 0)
        m["ffn_cb%d" % l] = inp["ffn_conv_b"][l].reshape(44, 128).T
    return {k: np.ascontiguousarray(v, dtype=f) for k, v in m.items()}


def kernel(**inputs):
    inp = {k: np.asarray(v) for k, v in inputs.items()}
    nc, p, c = build()
    in_maps = [host_inputs(inp, b) for b in range(8)]
    res = run_bass_kernel_spmd(nc, in_maps, core_ids=list(range(8)))
    outs = [np.asarray(r["outT"]).reshape(D, T).T for r in res.results]
    return np.stack(outs, axis=0).astype(np.float32)
```

```python
import numpy as np
from contextlib import ExitStack, contextmanager
import concourse.bass as bass
import concourse.mybir as mybir
from concourse.bass_utils import run_bass_kernel_spmd

F32 = mybir.dt.float32
BF16 = mybir.dt.bfloat16
ALU = mybir.AluOpType
AF = mybir.ActivationFunctionType
AX = mybir.AxisListType

T = 2048
D = 1024
NT = 16
EPS = 1e-6
D_FF = 2816
NJ = 22
TOPK = 256
NBIS = 14
NEG = -30000.0


class Tok:
    __slots__ = ("sem", "key", "val", "clock")

    def __init__(self, sem, key, val, clock):
        self.sem, self.key, self.val, self.clock = sem, key, val, clock


class Reg:
    __slots__ = ("w", "r")

    def __init__(self):
        self.w = None
        self.r = {}


class Prog:
    def __init__(self, nc):
        self.nc = nc
        self.es = ExitStack()
        self.E = {"pe": nc.tensor, "act": nc.scalar, "dve": nc.vector, "pool": nc.gpsimd, "sp": nc.sync}
        self.csem = {e: self.es.enter_context(nc.semaphore("cs_" + e)) for e in ("pe", "act", "dve", "pool")}
        self.ccnt = {e: 0 for e in self.csem}
        self.known = {e: {} for e in self.E}
        self.dq = {}
        for q, n in (("sp", 24), ("pool", 16)):
            self.dq[q] = dict(
                sems=[self.es.enter_context(nc.semaphore("d_%s%d" % (q, i))) for i in range(n)],
                use=[0] * n, last=[None] * n, nxt=0)
        self.regs = {}
        self.last = {e: None for e in self.csem}
        self.phase_stack = None
        self.ninst = 0
        self.nwait = 0
        self.nsb = 0

    @staticmethod
    def key(x, sub=None):
        if isinstance(x, (tuple, list)):
            return x
        if isinstance(x, str):
            return (x, None)
        name = x.tensor.name if hasattr(x, "tensor") else x.name
        if name == "psum":
            sz = mybir.dt.size(x.dtype)
            apl = list(x.ap)
            off = (x.offset % apl[0][0]) * sz
            ext = sum((cnt - 1) * abs(st) for st, cnt in apl[1:]) * sz + sz
            return [("psum", bk) for bk in range(off // 512, (off + ext - 1) // 512 + 1)]
        if sub and name in sub:
            return (name, sub[name])
        return (name, None)

    def _targets(self, k):
        name, s = k
        d = self.regs.setdefault(name, {})
        if s is None:
            if None not in d:
                d[None] = Reg()
            return list(d.values())
        if s not in d:
            d[s] = Reg()
        out = [d[s]]
        if None in d:
            out.append(d[None])
        return out

    def _wait(self, eng, tok):
        if tok is None:
            return
        if eng == "pe" and tok.key == "pe":
            return
        k = self.known[eng]
        if k.get(tok.key, 0) >= tok.val:
            return
        if tok.key in self.ccnt:
            assert tok.val <= self.ccnt[tok.key], ("wait on future inc", eng, tok.key, tok.val)
        self.E[eng].wait_ge(tok.sem, tok.val)
        self.nwait += 1
        for kk, vv in tok.clock.items():
            if k.get(kk, 0) < vv:
                k[kk] = vv

    def _sync(self, eng, reads, writes):
        for k in reads:
            for rg in self._targets(k):
                self._wait(eng, rg.w)
        for k in writes:
            for rg in self._targets(k):
                self._wait(eng, rg.w)
                for t in list(rg.r.values()):
                    self._wait(eng, t)

    def _record(self, tok, reads, writes):
        for k in reads:
            name, s = k
            rg = self.regs[name][s]
            old = rg.r.get(tok.key)
            if old is None or old.val < tok.val:
                rg.r[tok.key] = tok
        for k in writes:
            name, s = k
            rg = self.regs[name][s]
            rg.w = tok
            rg.r = {}

    def _flat(self, ks):
        out = []
        for k in ks:
            k = self.key(k)
            if isinstance(k, list):
                out += k
            else:
                out.append(k)
        return out

    def op(self, eng, fn, reads, writes, inc=True):
        reads = self._flat(reads)
        writes = self._flat(writes)
        self._sync(eng, reads, writes)
        inst = fn(self.E[eng])
        self.ninst += 1
        if inc:
            self.ccnt[eng] += 1
            inst.then_inc(self.csem[eng], 1)
            val = self.ccnt[eng]
        else:
            val = self.ccnt[eng] + 1
        clock = dict(self.known[eng])
        clock[eng] = max(clock.get(eng, 0), val)
        tok = Tok(self.csem[eng], eng, val, clock)
        self.last[eng] = tok
        self._record(tok, reads, writes)
        return tok

    def dma(self, q, out, in_, sub=None):
        reads = self._flat([self.key(in_, sub)])
        writes = self._flat([self.key(out, sub)])
        self._sync(q, reads, writes)
        d = self.dq[q]
        j = d["nxt"]
        d["nxt"] = (j + 1) % len(d["sems"])
        self._wait(q, d["last"][j])
        inst = self.E[q].dma_start(out=out, in_=in_)
        self.ninst += 1
        d["use"][j] += 1
        inst.then_inc(d["sems"][j], 16)
        key = ("d", q, j)
        val = 16 * d["use"][j]
        clock = dict(self.known[q])
        clock[key] = val
        tok = Tok(d["sems"][j], key, val, clock)
        d["last"][j] = tok
        self._record(tok, reads, writes)
        return tok

    def barrier(self):
        toks = [t for t in self.last.values() if t is not None]
        for d in self.dq.values():
            toks += [t for t in d["last"] if t is not None]
        for e in self.E:
            for t in toks:
                self._wait(e, t)

    def sb(self, name, shape, dtype, persistent=False):
        st = self.es if (persistent or self.phase_stack is None) else self.phase_stack
        self.nsb += 1
        return st.enter_context(self.nc.sbuf_tensor("s%d_%s" % (self.nsb, name), list(shape), dtype))

    @contextmanager
    def phase(self):
        prev = self.phase_stack
        self.phase_stack = ExitStack()
        try:
            yield
            self.barrier()
        finally:
            self.phase_stack.close()
            self.phase_stack = prev

    def _aps(self, sub, *xs):
        return [self.key(x, sub) for x in xs if x is not None and not isinstance(x, (int, float))]

    def mm(self, out, lhsT, rhs, start=True, stop=True, inc=None, sub=None):
        if inc is None:
            inc = stop
        return self.op("pe", lambda e: e.matmul(out, lhsT, rhs, start=start, stop=stop),
                       self._aps(sub, lhsT, rhs), self._aps(sub, out), inc=inc)

    def tr(self, out, in_, ident, inc=True, sub=None):
        return self.op("pe", lambda e: e.transpose(out, in_, ident),
                       self._aps(sub, in_, ident), self._aps(sub, out), inc=inc)

    def act(self, out, in_, func, bias=None, scale=None, accum_out=None, sub=None):
        kw = {}
        if bias is not None:
            kw["bias"] = bias
        if scale is not None:
            kw["scale"] = scale
        if accum_out is not None:
            kw["accum_out"] = accum_out
        return self.op("act", lambda e: e.activation(out=out, in_=in_, func=func, **kw),
                       self._aps(sub, in_, bias, scale), self._aps(sub, out, accum_out))

    def ts(self, eng, out, in0, s1, s2, op0, op1=None, accum_out=None, sub=None):
        kw = {}
        if op1 is not None:
            kw["op1"] = op1
        if accum_out is not None:
            kw["accum_out"] = accum_out
        return self.op(eng, lambda e: e.tensor_scalar(out=out, in0=in0, scalar1=s1, scalar2=s2, op0=op0, **kw),
                       self._aps(sub, in0, s1, s2), self._aps(sub, out, accum_out))

    def stt(self, out, in0, scalar, in1, op0, op1, sub=None):
        return self.op("dve", lambda e: e.scalar_tensor_tensor(out=out, in0=in0, scalar=scalar, in1=in1, op0=op0, op1=op1),
                       self._aps(sub, in0, scalar, in1), self._aps(sub, out))

    def tt(self, eng, out, in0, in1, op, sub=None):
        return self.op(eng, lambda e: e.tensor_tensor(out=out, in0=in0, in1=in1, op=op),
                       self._aps(sub, in0, in1), self._aps(sub, out))

    def copy(self, eng, out, in_, sub=None):
        if eng == "act":
            return self.act(out, in_, AF.Copy, sub=sub)
        return self.op(eng, lambda e: e.tensor_copy(out=out, in_=in_), self._aps(sub, in_), self._aps(sub, out))

    def memset(self, eng, ap, val, sub=None):
        return self.op(eng, lambda e: e.memset(ap, val), [], self._aps(sub, ap))

    def recip(self, out, in_, sub=None):
        return self.op("dve", lambda e: e.reciprocal(out=out, in_=in_), self._aps(sub, in_), self._aps(sub, out))

    def reduce(self, out, in_, op, sub=None):
        return self.op("dve", lambda e: e.tensor_reduce(out=out, in_=in_, axis=AX.X, op=op),
                       self._aps(sub, in_), self._aps(sub, out))

    def scan(self, out, d0, d1, initial, op0, op1, sub=None):
        return self.op("dve", lambda e: e.tensor_tensor_scan(out=out, data0=d0, data1=d1, initial=initial, op0=op0, op1=op1),
                       self._aps(sub, d0, d1), self._aps(sub, out))


class Ctx:
    pass


def rmsnorm_fm(p, c, g_ap, hT, t0, nt, nparts=128, kch=8, src=None, scale_div=None):
    src = c.xT if src is None else src
    div = float(scale_div if scale_div is not None else nparts * kch)
    for i, tb in enumerate(range(t0, t0 + nt, 512)):
        sq = c.sq[i % 2]
        sub = {hT.name: (tb - t0) // 512}
        if src is c.xT:
            sub[c.xT.name] = tb // 512
        p.act(sq[:nparts, :kch, :], src[:nparts, :kch, tb:tb + 512], AF.Square, sub=sub)
        ps = c.ps[c.psi % 8]
        c.psi += 1
        for k in range(kch):
            p.mm(ps[:nparts, :], c.ones_b[:nparts, :nparts], sq[:nparts, k, :], start=(k == 0), stop=(k == kch - 1))
        rs = c.rs[i % 2]
        p.act(rs[:nparts, :], ps[:nparts, :], AF.Ln, bias=c.eps_ap[:nparts, :], scale=1.0 / div)
        p.act(rs[:nparts, :], rs[:nparts, :], AF.Exp, scale=-0.5)
        for k in range(kch):
            p.stt(hT[:nparts, k, tb - t0:tb - t0 + 512], src[:nparts, k, tb:tb + 512], g_ap[:nparts, k:k + 1],
                  rs[:nparts, :], ALU.mult, ALU.mult, sub=sub)


def proj_multi(p, c, hT, jobs, kch=8, wname="wb"):
    ntok = hT.shape[2]
    wbs = c.wb[wname]
    flat = [(ji, b) for ji, (mode, w_l, consume) in enumerate(jobs) for b in range(w_l.shape[0])]

    def load(i):
        ji, b = flat[i]
        w_l = jobs[ji][1]
        cw = w_l.shape[3]
        p.dma("pool", wbs[i % 2][:, :kch, :cw], w_l[b])

    load(0)
    for i, (ji, b) in enumerate(flat):
        mode, w_l, consume = jobs[ji]
        cw = w_l.shape[3]
        if i + 1 < len(flat):
            load(i + 1)
        wb = wbs[i % 2]
        if mode == "fm":
            for tb in range(ntok // 512):
                ps = c.ps[c.psi % 8]
                c.psi += 1
                for k in range(kch):
                    p.mm(ps[:cw, :], wb[:, k, :cw], hT[:, k, tb * 512:(tb + 1) * 512], start=(k == 0), stop=(k == kch - 1),
                         sub={hT.name: tb})
                consume(b, tb, ps)
        else:
            for tt in range(ntok // 128):
                ps = c.ps[c.psi % 8]
                c.psi += 1
                for k in range(kch):
                    p.mm(ps[:, :cw], hT[:, k, tt * 128:(tt + 1) * 128], wb[:, k, :cw], start=(k == 0), stop=(k == kch - 1),
                         sub={hT.name: tt // 4})
                consume(b, tt, ps)


def proj_stream(p, c, mode, hT, w_l, consume, kch=8, tsl=None, wname="wb"):
    proj_multi(p, c, hT, [(mode, w_l, consume)], kch=kch, wname=wname)


def layer0_mixer(p, c, W):
    with p.phase():
        c.nmT = p.sb("nmT", [128, 136, 128], BF16)
        with p.phase():
            c.ik2 = p.sb("ik2", [128, 1, T], F32)
            c.iw_sb = p.sb("iw_sb", [128, NT, 16], F32)
            with p.phase():
                hT = p.sb("hT", [128, 8, T], BF16)
                c.wb = {"wb": [p.sb("wb%d" % i, [128, 8, 512], BF16) for i in range(2)]}
                with p.phase():
                    c.sq = [p.sb("sq%d" % i, [128, 8, 512], BF16) for i in range(2)]
                    c.rs = [p.sb("rs%d" % i, [128, 512], F32) for i in range(2)]
                    rmsnorm_fm(p, c, c.g_mix[0], hT, 0, T)
                    layer0_inproj_a(p, c, W, hT)
                layer0_gmlp(p, c, W, hT)
            if c.stop_after == "gmlp":
                return
            layer0_indexer(p, c, W)
        if c.stop_after == "indexer":
            return
        layer0_attention(p, c, W)
    if c.stop_after == "attn":
        return
    with p.phase():
        yT = p.sb("yT", [128, 8, T], BF16)
        for tb in range(4):
            p.dma("sp", yT[:, :, tb * 512:(tb + 1) * 512],
                  c.yT_d[:, :, tb * 512:(tb + 1) * 512].rearrange("k p t -> p k t"), sub={yT.name: tb})
        out_proj_residual(p, c, W["w_out0"], yT)


def layer0_inproj_a(p, c, W, hT):
    if True:
        stg = [p.sb("stg%d" % i, [128, T], BF16) for i in range(2)]
        cnt = [0]

        def cons_fm(dst, scale=None):
            def f(b, tb, ps):
                st = stg[b % 2]
                eng = "act" if (cnt[0] % 2 == 0) else "dve"
                cnt[0] += 1
                if scale is None:
                    p.copy(eng, st[:, tb * 512:(tb + 1) * 512], ps[:, :])
                elif eng == "act":
                    p.act(st[:, tb * 512:(tb + 1) * 512], ps[:, :], AF.Copy, scale=scale)
                else:
                    p.ts("dve", st[:, tb * 512:(tb + 1) * 512], ps[:, :], scale, None, ALU.mult)
                if tb == 3:
                    p.dma("sp", dst[b], st[:, :])
            return f

        jobs = [("fm", W["w_q"], cons_fm(c.qT_d, 0.125)), ("fm", W["w_k"], cons_fm(c.kT_d)),
                ("fm", W["w_iq"], cons_fm(c.iqT_d))]

        def cons_ik(b, tb, ps):
            p.copy("act", c.ik2[:, 0, tb * 512:(tb + 1) * 512], ps[:, :])
        jobs.append(("fm", W["w_ik2"], cons_ik))

        vst = [p.sb("vst%d" % i, [128, 512], BF16) for i in range(2)]

        def cons_v(b, tt, ps):
            st = vst[tt % 2]
            p.copy("act" if tt % 2 == 0 else "dve", st[:, :], ps[:, :])
            p.dma("sp", c.v_d[tt * 128:(tt + 1) * 128, :], st[:, :])
        jobs.append(("tm", W["w_v"], cons_v))

        def cons_iw(b, tt, ps):
            p.copy("dve", c.iw_sb[:, tt, :], ps[:, 0:16])
        jobs.append(("tm", W["w_iw"], cons_iw))
        proj_multi(p, c, hT, jobs)


def layer0_gmlp(p, c, W, hT):
    if True:
        u_all = p.sb("u_all", [128, NT, 512], BF16)
        vn_all = p.sb("vn_all", [128, NT, 512], BF16)
        vgf = [p.sb("vgf%d" % i, [128, 512], F32) for i in range(2)]
        junk = p.sb("junk", [128, 512], BF16)
        ssv = p.sb("ssv", [128, NT], F32)
        rsv = p.sb("rsv", [128, NT], F32)
        gn_bc = p.sb("gn_bc", [128, 512], F32)
        p.dma("sp", gn_bc[:, :], W["gmlp_norm"].partition_broadcast(128))

        def cons_u(b, tt, ps):
            p.act(u_all[:, tt, :], ps[:, :], AF.Gelu_apprx_tanh)
        jobs = [("tm", W["w_u"], cons_u)]

        def cons_vg(b, tt, ps):
            vg = vgf[tt % 2]
            p.act(vg[:, :], ps[:, :], AF.Gelu_apprx_tanh)
            p.act(junk[:, :], vg[:, :], AF.Square, accum_out=ssv[:, tt:tt + 1])
            p.act(rsv[:, tt:tt + 1], ssv[:, tt:tt + 1], AF.Sqrt, bias=c.eps_ap[:, :], scale=1.0 / 512)
            p.recip(rsv[:, tt:tt + 1], rsv[:, tt:tt + 1])
            p.stt(vn_all[:, tt, :], vg[:, :], rsv[:, tt:tt + 1], gn_bc[:, :], ALU.mult, ALU.mult)
        jobs.append(("tm", W["w_vg"], cons_vg))
        proj_multi(p, c, hT, jobs)

        wsT = p.sb("wsT", [128, 8, 128], BF16)
        p.dma("pool", wsT[:, :, :], W["w_sT"])
        p.memset("dve", wsT[64:128, :, 0:64], 0.0)
        bs8 = p.sb("bs8", [8, 128], BF16)
        p.dma("pool", bs8[:, :], W["b_s"])
        e8 = p.sb("e8", [8, 512], BF16)
        p.dma("pool", e8[:, :], W["e8"])
        ybs = [p.sb("ybs%d" % i, [128, 512], BF16) for i in range(2)]
        ybT = [p.sb("ybT%d" % i, [128, 512], BF16) for i in range(2)]
        for n in range(NT):
            ps = c.ps[c.psi % 8]
            c.psi += 1
            for g in range(8):
                p.mm(ps[:, g * 64:(g + 1) * 64], wsT[:, g, :], vn_all[:, n, g * 64:(g + 1) * 64], start=True, stop=False, inc=False)
                p.mm(ps[:, g * 64:(g + 1) * 64], bs8[:, :], e8[:, g * 64:(g + 1) * 64], start=False, stop=True, inc=(g == 7))
            yb = ybs[n % 2]
            p.tt("dve", yb[:, :], ps[:, :], u_all[:, n, :], ALU.mult)
            pst = c.psb[c.psi % 8]
            pstn = c.ps[c.psi % 8]
            c.psi += 1
            for cc in range(4):
                p.op("pe", lambda e, cc=cc: e.transpose(pst[:, cc * 128:(cc + 1) * 128], yb[:, cc * 128:(cc + 1) * 128], c.ident_b[:, :]),
                     [p.key(yb), p.key(c.ident_b)], [p.key(pstn)], inc=(cc == 3))
            yst = ybT[n % 2]
            p.copy("act", yst[:, :], pst[:, 0:512])
            p.dma("sp", c.yT_d[4:8, :, n * 128:(n + 1) * 128].rearrange("c p t -> p c t"),
                  yst[:, :].rearrange("p (a b) -> p a b", a=4))


def tri(b):
    return b * (b + 1) // 2


def layer0_indexer(p, c, W):
    with p.phase():
        ikn = p.sb("ikn", [128, 1, T], BF16)
        gk2 = p.sb("gk2", [128, 1], F32)
        p.dma("sp", gk2[:, :], W["gk2"])
        with p.phase():
            c.sq = [p.sb("isq%d" % i, [128, 1, 512], BF16) for i in range(2)]
            c.rs = [p.sb("irs%d" % i, [128, 512], F32) for i in range(2)]
            rmsnorm_fm(p, c, gk2, ikn, 0, T, nparts=128, kch=1, src=c.ik2, scale_div=128)
        wabs = p.sb("wabs", [128, NT, 16], F32)
        sgn = p.sb("sgn", [128, NT, 16], F32)
        p.ts("dve", wabs[:, :, :], c.iw_sb[:, :, :], -1.0, None, ALU.mult)
        p.tt("dve", wabs[:, :, :], wabs[:, :, :], c.iw_sb[:, :, :], ALU.max)
        p.ts("dve", sgn[:, :, :], c.iw_sb[:, :, :], 0.0, 2.0, ALU.is_ge, ALU.mult)
        p.ts("dve", sgn[:, :, :], sgn[:, :, :], -1.0, None, ALU.add)
        sgn_b = p.sb("sgn_b", [128, NT, 16], BF16)
        p.copy("dve", sgn_b[:, :, :], sgn[:, :, :])
        iqz = [p.sb("iqz%d" % i, [128, 8, 2, 128], BF16) for i in range(2)]
        for i in range(2):
            p.memset("pool", iqz[i][:, :, :, :], 0.0)
        Dsg = [p.sb("Dsg%d" % i, [128, 16, 128], BF16) for i in range(4)]
        acc = [p.sb("acc%d" % i, [128, T], F32) for i in range(4)]
        rr = [p.sb("rr%d" % i, [128, 1024], BF16) for i in range(3)]
        nm = [p.sb("nm%d" % i, [128, T], BF16) for i in range(4)]
        junk = [p.sb("junkc%d" % i, [128, T], BF16) for i in range(2)]
        pow2 = p.sb("pow2", [128, NBIS], F32)
        p.dma("sp", pow2[:, :], W["pow2"])
        st = [p.sb("bis%d" % b, [128, 8], F32) for b in range(NT)]
        wtab = [p.sb("wtab%d" % b, [128, NBIS], F32) for b in range(NT)]
        cnt_ = {"ri": 0, "ei": 0, "si": 0}

        def jobs_for(b):
            L = 128 * (b + 1)
            units = [(0, min(L, 1024))] + ([(1024, L)] if L > 1024 else [])
            return [(b, k0, k1, h) for (k0, k1) in units for h in range(16)]

        def load_iq(b):
            iz = iqz[b % 2]
            for par in range(2):
                p.dma("sp", iz[par * 64:(par + 1) * 64, :, par, :],
                      c.iqT_d[:, par * 64:(par + 1) * 64, b * 128:(b + 1) * 128].rearrange("m p t -> p m t"))

        def emit_score(job):
            b, k0, k1, h = job
            n = k1 - k0
            nb = (n + 511) // 512
            m, par = divmod(h, 2)
            slot = 1 + cnt_["si"] % 3
            cnt_["si"] += 1
            psc = c.psum[:, slot * 1024:slot * 1024 + n]
            for kb in range(nb):
                c0, c1 = kb * 512, min(n, (kb + 1) * 512)
                p.mm(psc[:, c0:c1], iqz[b % 2][:, m, par, :], ikn[:, 0, k0 + c0:k0 + c1])
            return psc

        def emit_rest(job, psc):
            b, k0, k1, h = job
            n = k1 - k0
            nb = (n + 511) // 512
            pacc = c.psum[:, 0:n]
            r = rr[cnt_["ri"] % 3]
            cnt_["ri"] += 1
            p.act(r[:, :n], psc, AF.Relu, scale=wabs[:, b, h:h + 1])
            for kb in range(nb):
                c0, c1 = kb * 512, min(n, (kb + 1) * 512)
                p.mm(pacc[:, c0:c1], Dsg[b % 4][:, h, :], r[:, c0:c1], start=(h == 0), stop=(h == 15))
            if h == 15:
                p.copy("act", acc[b % 4][:, k0:k1], pacc)

        def scores(bs):
            jobs = []
            for b in bs:
                load_iq(b)
                jobs += jobs_for(b)
            psc = emit_score(jobs[0])
            for i, job in enumerate(jobs):
                nxt = emit_score(jobs[i + 1]) if i + 1 < len(jobs) else None
                emit_rest(job, psc)
                psc = nxt

        def build_dsg(b):
            for h in range(16):
                p.tt("pool", Dsg[b % 4][:, h, :], c.ident_b[:, :], sgn_b[:, b, h:h + 1].to_broadcast([128, 128]), ALU.mult)

        def bisect(bs):
            S = {}
            for i_, b in enumerate(bs):
                L = 128 * (b + 1)
                ac = acc[b % 4]
                mx, mn, w0, cand, cnt, tq = [st[b][:, i:i + 1] for i in range(6)]
                S[b] = (L, ac, cand, cnt, tq, junk[i_ % 2])
                p.reduce(mx, ac[:, :L], ALU.max)
                p.reduce(mn, ac[:, :L], ALU.min)
                p.memset("dve", ac[0:64, L - 64:L], -1e30)
                p.tt("dve", w0, mx, mn, ALU.subtract)
                p.ts("dve", wtab[b][:, :], pow2[:, :], w0, None, ALU.mult)
                p.tt("dve", cand, mn, wtab[b][:, 0:1], ALU.add)
            for i in range(NBIS):
                last = (i == NBIS - 1)
                for b in bs:
                    L, ac, cand, cnt, tq, jk = S[b]
                    p.ts("dve", jk[:, :L], ac[:, :L], cand, 0.0, ALU.is_ge, ALU.add, accum_out=cnt)
                for b in bs:
                    L, ac, cand, cnt, tq, jk = S[b]
                    p.ts("dve", tq, cnt, float(TOPK), (1.0 if last else 0.5), ALU.is_ge, ALU.subtract)
                for b in bs:
                    L, ac, cand, cnt, tq, jk = S[b]
                    p.stt(cand, tq, wtab[b][:, i:i + 1], cand, ALU.mult, ALU.add)
            for b in bs:
                L, ac, cand, cnt, tq, jk = S[b]
                p.ts("dve", nm[b % 4][:, :L], ac[:, :L], cand, NEG, ALU.is_lt, ALU.mult)

        def transposes(b):
            nmb = nm[b % 4]
            for a0 in range(0, b + 1, 4):
                g = min(4, b + 1 - a0)
                slot = 1 + cnt_["si"] % 3
                cnt_["si"] += 1
                pstn = c.ps[2 * slot]
                pst = c.psb[2 * slot]
                for j in range(g):
                    a = a0 + j
                    p.op("pe", lambda e, j=j, a=a: e.transpose(pst[:, j * 128:(j + 1) * 128], nmb[:, a * 128:(a + 1) * 128], c.ident_b[:, :]),
                         [p.key(nmb), p.key(c.ident_b)], [p.key(pstn)], inc=(j == g - 1))
                o_ap = c.nmT[:, tri(b) + a0:tri(b) + a0 + g, :]
                i_ap = pst[:, 0:g * 128].rearrange("p (a b) -> p a b", a=g)
                p.op("act", lambda e: e.activation(out=o_ap, in_=i_ap, func=AF.Copy), [p.key(pstn)], [p.key(c.nmT, {c.nmT.name: b})])

        for b in range(2):
            L = 128 * (b + 1)
            p.memset("pool", nm[b][:, :L], 0.0)
            p.memset("pool", nm[b][0:64, L - 64:L], NEG)
        prev = [0, 1]
        order = list(range(NT - 2, 1, -2))
        build_dsg(order[0])
        build_dsg(order[0] + 1)
        for oi, b0 in enumerate(order):
            bs = [b0, b0 + 1]
            if oi + 1 < len(order):
                build_dsg(order[oi + 1])
                build_dsg(order[oi + 1] + 1)
            scores(bs)
            for b in prev:
                transposes(b)
            bisect(bs)
            prev = bs
        for b in prev:
            transposes(b)


def layer0_attention(p, c, W):
    with p.phase():
        kT = p.sb("kT", [128, 4, T], BF16)
        for m in range(4):
            p.dma("sp", kT[:, m, :], c.kT_d[m])
        qzs = [p.sb("qz%d" % i, [128, 4, 2, 128], BF16) for i in range(2)]
        for i in range(2):
            p.memset("pool", qzs[i][:, :, :, :], 0.0)
        va = p.sb("va", [128, NT, 8, 65], BF16)
        p.memset("pool", va[:, :, :, 64:65], 1.0)
        for tt in range(NT):
            p.dma("sp", va[:, tt, :, 0:64], c.v_d[tt * 128:(tt + 1) * 128, :].rearrange("p (h d) -> p h d", h=8))
        oh1 = p.sb("oh1", [32, 383], F32)
        p.dma("sp", oh1[:, :], W["oh1"])
        rb = p.sb("rb", [32, 8], F32)
        p.dma("sp", rb[:, :], W["rel_bias"])
        cb = p.sb("cb", [128, 8], F32)
        p.dma("sp", cb[:, :], W["rel_bias"][15:16, :].partition_broadcast(128))
        Tb = p.sb("Tb", [128, 2, 8, 128], BF16)
        for kind in range(2):
            for t0 in range(0, 128, 64):
                ps = c.ps[c.psi % 8]
                c.psi += 1
                for t in range(t0, t0 + 64):
                    base = (255 - t) if kind == 0 else (127 - t)
                    p.mm(ps[:, (t - t0) * 8:(t - t0) * 8 + 8], oh1[:, base:base + 128], rb[:, :], start=True, stop=True,
                         inc=(t == t0 + 63))
                p.tt("dve", Tb[:, kind, :, t0:t0 + 64], ps[:, 0:512].rearrange("p (t h) -> p h t", h=8),
                     cb[:, :].unsqueeze(2).to_broadcast([128, 8, 64]), ALU.subtract)
        ya = [p.sb("ya%d" % i, [128, 512], BF16) for i in range(2)]
        PT = [p.sb("PT%d" % i, [128, 1024], BF16) for i in range(3)]
        rec = p.sb("rec", [128, 16], F32)
        yaT = [p.sb("yaT%d" % i, [128, 512], BF16) for i in range(2)]
        si = 0
        pend = []

        def normalize(b, h, po):
            ri = (b * 8 + h) % 16
            p.recip(rec[:, ri:ri + 1], po[:, 64:65])
            p.act(ya[b % 2][:, h * 64:(h + 1) * 64], po[:, 0:64], AF.Copy, scale=rec[:, ri:ri + 1])
            if h == 7:
                pstn = c.ps[6 + b % 2]
                pst = c.psb[6 + b % 2]
                yab = ya[b % 2]
                for cc in range(4):
                    p.op("pe", lambda e, cc=cc: e.transpose(pst[:, cc * 128:(cc + 1) * 128], yab[:, cc * 128:(cc + 1) * 128], c.ident_b[:, :]),
                         [p.key(yab), p.key(c.ident_b)], [p.key(pstn)], inc=(cc == 3))
                yst = yaT[b % 2]
                p.copy("dve", yst[:, :], pst[:, 0:512])
                p.dma("sp", c.yT_d[0:4, :, b * 128:(b + 1) * 128].rearrange("c p t -> p c t"),
                      yst[:, :].rearrange("p (a b) -> p a b", a=4))

        jobs = [(b, h, a0) for b in range(NT) for h in range(8) for a0 in range(0, b + 1, 8)]

        def emit_score(job, idx):
            b, h, a0 = job
            qz = qzs[b % 2]
            if h == 0 and a0 == 0:
                for par in range(2):
                    p.dma("sp", qz[par * 64:(par + 1) * 64, :, par, :],
                          c.qT_d[:, par * 64:(par + 1) * 64, b * 128:(b + 1) * 128].rearrange("m p t -> p m t"))
            m = h // 2
            grp = list(range(a0, min(a0 + 8, b + 1)))
            n = len(grp) * 128
            slot = idx % 2
            ps = c.psum[:, slot * 1024:slot * 1024 + n]
            for j, a in enumerate(grp):
                near = (a >= b - 1)
                lastj = (j == len(grp) - 1)
                o = ps[:, j * 128:(j + 1) * 128]
                p.mm(o, kT[:, m, a * 128:(a + 1) * 128], qz[:, h // 2, h % 2, :], start=True, stop=False, inc=False)
                p.mm(o, c.ident_b[:, :], c.nmT[:, tri(b) + a, :], start=False, stop=(not near), inc=((not near) and lastj))
                if near:
                    kind = 0 if a == b else 1
                    p.mm(o, c.ident_b[:, :], Tb[:, kind, h, :], start=False, stop=True, inc=lastj)
            return ps

        def emit_rest(job, idx, ps):
            nonlocal pend
            b, h, a0 = job
            grp = list(range(a0, min(a0 + 8, b + 1)))
            n = len(grp) * 128
            po = c.ps[4 + (b * 8 + h) % 2]
            pt = PT[idx % 3]
            p.act(pt[:, :n], ps, AF.Exp, bias=cb[:, h:h + 1])
            for j, a in enumerate(grp):
                p.mm(po[:, 0:65], pt[:, j * 128:(j + 1) * 128], va[:, a, h, :], start=(a == 0), stop=(a == b))
            if grp[-1] == b:
                for f in pend:
                    f()
                pend = [lambda b=b, h=h, po=po: normalize(b, h, po)]

        ps = emit_score(jobs[0], 0)
        for i, job in enumerate(jobs):
            nxt = emit_score(jobs[i + 1], i + 1) if i + 1 < len(jobs) else None
            emit_rest(job, i, ps)
            ps = nxt
        for f in pend:
            f()


def out_proj_residual(p, c, w_l, src):
    kch = w_l.shape[2]
    with p.phase():
        c.wb = {"wb": [p.sb("wbo%d" % i, [128, kch, 128], BF16) for i in range(2)]}

        def cons(b, tb, ps):
            p.tt("dve", c.xT[:, b, tb * 512:(tb + 1) * 512], ps[:, :], c.xT[:, b, tb * 512:(tb + 1) * 512], ALU.add)
        proj_stream(p, c, "fm", src, w_l, cons, kch=kch)


def conv_ffn(p, c, W, l):
    w_up = W["w_up%d" % l]
    w_dn = W["w_dn%d" % l]
    with p.phase():
        cw = p.sb("cw", [128, 44, 3], F32)
        cbs = p.sb("cbs", [128, 44], F32)
        p.dma("sp", cw[:, :, :], W["ffn_cw%d" % l])
        p.dma("sp", cbs[:, :], W["ffn_cb%d" % l])
        halo = p.sb("halo", [128, 44, 2], F32)
        for half in range(2):
            t0 = half * 1024
            with p.phase():
                hT = p.sb("hTf", [128, 8, 1024], BF16)
                gT = p.sb("gT", [128, NJ, 1024], BF16)
                if True:
                    c.sq = [p.sb("fsq%d" % i, [128, 8, 512], BF16) for i in range(2)]
                    c.rs = [p.sb("frs%d" % i, [128, 512], F32) for i in range(2)]
                    rmsnorm_fm(p, c, c.g_ffn[l], hT, t0, 1024)
                wu = [[p.sb("wu%d_%d" % (sd, i), [128, 8, 128], BF16) for i in range(2)] for sd in range(2)]
                wd = [p.sb("wd%d" % i, [128, NJ, 128], BF16) for i in range(2)]
                ya = [p.sb("fya%d" % i, [128, 512], F32) for i in range(3)]
                yb = [p.sb("fyb%d" % i, [128, 512], F32) for i in range(3)]
                sa = [p.sb("fsa%d" % i, [128, 512], F32) for i in range(2)]
                hl = [p.sb("fhl%d" % i, [128, 2], F32) for i in range(4)]
                hli = 0

                def load(j):
                    p.dma("pool", wu[0][j % 2][:, :, :], w_up[j])
                    p.dma("pool", wu[1][j % 2][:, :, :], w_up[NJ + j])

                load(0)
                it = 0
                for j in range(NJ):
                    if j + 1 < NJ:
                        load(j + 1)
                    if j == NJ - 5:
                        p.dma("pool", wd[0][:, :, :], w_dn[0])
                    if j == NJ - 3:
                        p.dma("pool", wd[1][:, :, :], w_dn[1])
                    for tb in range(2):
                        ys = []
                        for sd in range(2):
                            m = sd * NJ + j
                            ps = c.ps[c.psi % 8]
                            c.psi += 1
                            wt = wu[sd][j % 2]
                            for k in range(8):
                                p.mm(ps[:, :], wt[:, k, :], hT[:, k, tb * 512:(tb + 1) * 512], start=(k == 0), stop=(k == 7),
                                     sub={hT.name: tb})
                            y = (ya if sd == 0 else yb)[it % 3]
                            p.act(y[:, :], ps[:, :], AF.Identity, bias=cbs[:, m:m + 1], scale=cw[:, m, 2:3])
                            p.stt(y[:, 1:512], ps[:, 0:511], cw[:, m, 1:2], y[:, 1:512], ALU.mult, ALU.add)
                            p.stt(y[:, 2:512], ps[:, 0:510], cw[:, m, 0:1], y[:, 2:512], ALU.mult, ALU.add)
                            first = (half == 0 and tb == 0)
                            if not first:
                                if tb == 0:
                                    hsrc = halo[:, m, :]
                                else:
                                    hsrc = hprev[sd][:, :]
                                p.stt(y[:, 0:1], hsrc[:, 1:2], cw[:, m, 1:2], y[:, 0:1], ALU.mult, ALU.add)
                                p.stt(y[:, 0:2], hsrc[:, 0:2], cw[:, m, 0:1], y[:, 0:2], ALU.mult, ALU.add)
                            if tb == 0:
                                if sd == 0:
                                    hprev = [None, None]
                                hprev[sd] = hl[hli % 4]
                                hli += 1
                                p.copy("act", hprev[sd][:, :], ps[:, 510:512])
                            elif half == 0:
                                p.copy("act", halo[:, m, :], ps[:, 510:512])
                            ys.append(y)
                        sg = sa[it % 2]
                        p.act(sg[:, :], ys[0][:, :], AF.Silu)
                        p.tt("pool", gT[:, j, tb * 512:(tb + 1) * 512], sg[:, :], ys[1][:, :], ALU.mult)
                        it += 1
                for dc in range(8):
                    for tb in range(2):
                        ps = c.ps[c.psi % 8]
                        c.psi += 1
                        for j in range(NJ):
                            p.mm(ps[:, :], wd[dc % 2][:, j, :], gT[:, j, tb * 512:(tb + 1) * 512], start=(j == 0), stop=(j == NJ - 1))
                        sl = c.xT[:, dc, t0 + tb * 512:t0 + (tb + 1) * 512]
                        p.tt("dve", sl, ps[:, :], sl, ALU.add)
                    if dc + 2 < 8:
                        p.dma("pool", wd[dc % 2][:, :, :], w_dn[dc + 2])


def hgrn_mixer(p, c, W):
    with p.phase():
        hT = p.sb("hT1", [128, 8, T], BF16)
        c.wb = {"wb": [p.sb("wbh%d" % i, [128, 8, 512], BF16) for i in range(2)]}
        lbr = p.sb("lbr", [128, 2, 8], F32)
        p.dma("sp", lbr[:, :, :], W["hgrn_lb"])
        lb = p.sb("lb", [128, 8], F32)
        oml = p.sb("oml", [128, 8], F32)
        p.tt("dve", lb[:, :], lbr[:, 1, :], lbr[:, 0, :], ALU.subtract)
        p.act(lb[:, :], lb[:, :], AF.Sigmoid)
        p.ts("dve", oml[:, :], lb[:, :], -1.0, 1.0, ALU.mult, ALU.add)
        if True:
            c.sq = [p.sb("hsq%d" % i, [128, 8, 512], BF16) for i in range(2)]
            c.rs = [p.sb("hrs%d" % i, [128, 512], F32) for i in range(2)]
            rmsnorm_fm(p, c, c.g_mix[1], hT, 0, T)
        stg = [p.sb("hstg%d" % i, [128, T], BF16) for i in range(2)]

        def cons_silu(dst):
            def f(b, tb, ps):
                st = stg[b % 2]
                p.act(st[:, tb * 512:(tb + 1) * 512], ps[:, :], AF.Silu)
                if tb == 3:
                    p.dma("sp", dst[b], st[:, :])
            return f
        jobs = [("fm", W["w_cq"], cons_silu(c.qh_d)), ("fm", W["w_cg"], cons_silu(c.gate_d))]

        gst = [p.sb("gst%d" % i, [128, T], F32) for i in range(2)]
        sgt = [p.sb("sgt%d" % i, [128, 512], F32) for i in range(2)]

        def cons_f(b, tb, ps):
            g = gst[b % 2]
            sg = sgt[tb % 2]
            p.act(sg[:, :], ps[:, :], AF.Sigmoid)
            p.ts("dve", g[:, tb * 512:(tb + 1) * 512], sg[:, :], oml[:, b:b + 1], lb[:, b:b + 1], ALU.mult, ALU.add)
            if tb == 3:
                st = stg[b % 2]
                p.ts("dve", st[:, :], g[:, :], -1.0, 1.0, ALU.mult, ALU.add)
                p.dma("sp", c.kk_d[b], st[:, :])
                p.act(g[:, :], g[:, :], AF.Ln)
                p.dma("sp", c.lg_d[b], g[:, :])
        jobs.append(("fm", W["w_cf"], cons_f))

        ist = [p.sb("ist%d" % i, [128, 512], BF16) for i in range(2)]

        def cons_i(b, tt, ps):
            st = ist[tt % 2]
            p.copy("act" if tt % 2 == 0 else "dve", st[:, :], ps[:, :])
            p.dma("sp", c.i_d[tt * 128:(tt + 1) * 128, b * 512:(b + 1) * 512], st[:, :])
        jobs.append(("tm", W["w_ci"], cons_i))
        proj_multi(p, c, hT, jobs)
    if c.stop_after == "hproj":
        return
    with p.phase():
        cm = p.sb("cm", [128, 32, 64], BF16)
        p.memset("pool", cm[:, :, :], 1.0)
        p.memset("pool", cm[:, :, 0:1], 0.0)
        tri64 = p.sb("tri64", [64, 64], F32)
        p.dma("sp", tri64[:, :], W["tri64"])
        cn = p.sb("cn", [128, 1], F32)
        p.dma("sp", cn[:, :], W["c_out_norm"])
        lg = p.sb("lg", [128, T], F32)
        bb = p.sb("bb", [128, T], F32)
        eb = p.sb("eb", [128, T], F32)
        t1 = p.sb("t1", [128, T], F32)
        qh = p.sb("qh", [128, T], BF16)
        kk = p.sb("kk", [128, T], BF16)
        KlT = p.sb("KlT", [128, T], BF16)
        Qb = [p.sb("Qb%d" % i, [128, T], BF16) for i in range(2)]
        Sb = [p.sb("Sb%d" % i, [128, 128], BF16) for i in range(2)]
        Kbb = [p.sb("Kbb%d" % i, [128, T], BF16) for i in range(2)]
        Klc = [p.sb("Klc%d" % i, [64, 32, 128], BF16) for i in range(2)]
        ic = [p.sb("ic%d" % i, [64, 32, 128], BF16) for i in range(2)]
        ebl = [p.sb("ebl%d" % i, [128, 32], F32) for i in range(2)]
        gt = [p.sb("gt%d" % i, [128, T], BF16) for i in range(2)]
        oTt = [p.sb("oTt%d" % i, [128, 512], F32) for i in range(2)]
        Sf = [p.sb("Sf%d" % i, [128, 128], F32) for i in range(2)]
        ATs = [p.sb("ATs%d" % i, [64, 64], BF16) for i in range(3)]
        osq = [p.sb("osq%d" % i, [128, 512], BF16) for i in range(2)]
        ors = [p.sb("ors%d" % i, [128, 512], F32) for i in range(2)]
        on = [p.sb("on%d" % i, [128, 512], F32) for i in range(2)]
        ost = [p.sb("ost%d" % i, [128, 512], BF16) for i in range(2)]

        def precompute_steps(h):
            u = h % 2
            ebv = eb[:, :].rearrange("p (a b) -> p a b", b=64)
            st = []

            def loads():
                p.dma("sp", lg[:, :], c.lg_d[h])
                p.dma("sp", qh[:, :], c.qh_d[h])
                p.dma("sp", kk[:, :], c.kk_d[h])
                p.dma("sp", gt[u][:, :], c.gate_d[h])
                p.dma("sp", ic[u][:, :, :], c.i_d[:, h * 128:(h + 1) * 128].rearrange("(n s) e -> s n e", s=64))
            st.append(loads)
            st.append(lambda: p.scan(bb[:, :], cm[:, :, :].rearrange("p a b -> p (a b)"), lg[:, :], 0.0, ALU.mult, ALU.add))
            st.append(lambda: p.act(eb[:, :], bb[:, :], AF.Exp))
            st.append(lambda: p.act(t1[:, :], bb[:, :], AF.Exp, scale=-1.0))
            st.append(lambda: p.tt("pool", Qb[u][:, :], qh[:, :], eb[:, :], ALU.mult))
            st.append(lambda: p.tt("pool", t1[:, :], kk[:, :], t1[:, :], ALU.mult))
            st.append(lambda: p.copy("act", ebl[u][:, :], ebv[:, :, 63]))
            st.append(lambda: p.copy("act", Kbb[u][:, :], t1[:, :]))
            st.append(lambda: p.tt("pool", KlT[:, :].rearrange("p (a b) -> p a b", b=64), t1[:, :].rearrange("p (a b) -> p a b", b=64),
                                   ebv[:, :, 63:64].to_broadcast([128, 32, 64]), ALU.mult))

            def trs(g8):
                pstn = c.ps[7]
                pst = c.psb[7]
                for j in range(8):
                    n = g8 * 8 + j
                    p.op("pe", lambda e, j=j, n=n: e.transpose(pst[0:64, j * 128:(j + 1) * 128], KlT[:, n * 64:(n + 1) * 64], c.ident_b[:, :]),
                         [p.key(KlT), p.key(c.ident_b)], [p.key(pstn)], inc=(j == 7))
                p.op("act", lambda e: e.activation(out=Klc[u][:, g8 * 8:(g8 + 1) * 8, :],
                                                   in_=pst[0:64, :].rearrange("p (a b) -> p a b", a=8), func=AF.Copy),
                     [p.key(pstn)], [p.key(Klc[u])])
            for g8 in range(4):
                st.append(lambda g8=g8: trs(g8))
            return st

        def chunkloop(h, bg):
            u = h % 2

            def front(n):
                cs = slice(n * 64, (n + 1) * 64)
                if n < 31:
                    psS = c.ps[4 + n % 3]
                    p.mm(psS[:, 0:128], Klc[u][:, n, :], ic[u][:, n, :])
                psA = c.ps[n % 2]
                p.mm(psA[0:64, 0:64], Kbb[u][:, cs], Qb[u][:, cs])
                p.tt("dve", ATs[n % 3][:, :], psA[0:64, 0:64], tri64[:, :], ALU.mult)

            def outnorm_steps(tb, po):
                ts_ = slice(tb * 512, (tb + 1) * 512)
                oT = oTt[tb % 2]
                sq = osq[tb % 2]
                rs = ors[tb % 2]
                o2 = on[tb % 2]
                o3 = ost[tb % 2]
                ps = c.ps[7]

                def s1():
                    p.copy("act", oT[:, :], po[:, :])
                    p.act(sq[:, :], oT[:, :], AF.Square)

                def s2():
                    p.mm(ps[:, :], c.ones_b[:, :], sq[:, :])

                def s3():
                    p.act(rs[:, :], ps[:, :], AF.Ln, bias=c.eps_ap[:, :], scale=1.0 / 128)
                    p.act(rs[:, :], rs[:, :], AF.Exp, scale=-0.5)
                    p.stt(o2[:, :], oT[:, :], cn[:, 0:1], rs[:, :], ALU.mult, ALU.mult)
                    p.tt("pool", o3[:, :], o2[:, :], gt[u][:, ts_], ALU.mult)
                    p.dma("pool", c.oT_d[h][:, ts_], o3[:, :])
                return [s1, s2, s3]

            front(0)
            po = None
            for n in range(32):
                cs = slice(n * 64, (n + 1) * 64)
                if n + 1 < 32:
                    front(n + 1)
                if n % 8 == 0:
                    po = c.ps[2 + (n // 8) % 2]
                oc = po[:, (n % 8) * 64:(n % 8 + 1) * 64]
                p.mm(oc, ic[u][:, n, :], ATs[n % 3][:, :], start=True, stop=(n == 0), inc=(n == 0))
                if n > 0:
                    p.mm(oc, Sb[(n - 1) % 2][:, :], Qb[u][:, cs], start=False, stop=True, inc=True)
                if n < 31:
                    psS = c.ps[4 + n % 3]
                    if n == 0:
                        p.copy("dve", Sf[0][:, :], psS[:, 0:128])
                    else:
                        p.stt(Sf[n % 2][:, :], Sf[(n - 1) % 2][:, :], ebl[u][:, n:n + 1], psS[:, 0:128], ALU.mult, ALU.add)
                    p.copy("act", Sb[n % 2][:, :], Sf[n % 2][:, :])
                if n % 8 == 7:
                    for f in reversed(outnorm_steps(n // 8, po)):
                        bg.insert(0, f)
                if bg:
                    bg.pop(0)()
            while bg:
                bg.pop(0)()

        for f in precompute_steps(0):
            f()
        for h in range(8):
            bg = precompute_steps(h + 1) if h + 1 < 8 else []
            chunkloop(h, bg)
    if c.stop_after == "hrec":
        return
    with p.phase():
        oTa = p.sb("oTa", [128, 8, T], BF16)
        for tb in range(4):
            p.dma("sp", oTa[:, :, tb * 512:(tb + 1) * 512],
                  c.oT_d[:, :, tb * 512:(tb + 1) * 512].rearrange("k p t -> p k t"), sub={oTa.name: tb})
        out_proj_residual(p, c, W["w_out1"], oTa)


def final_norm(p, c, out):
    with p.phase():
        c.sq = [p.sb("nsq%d" % i, [128, 8, 512], BF16) for i in range(2)]
        c.rs = [p.sb("nrs%d" % i, [128, 512], F32) for i in range(2)]
        ot = [p.sb("fot%d" % i, [128, 8, 512], F32) for i in range(2)]
        for i, tb in enumerate(range(0, T, 512)):
            sq = c.sq[i % 2]
            p.act(sq[:, :, :], c.xT[:, :, tb:tb + 512], AF.Square)
            ps = c.ps[c.psi % 8]
            c.psi += 1
            for k in range(8):
                p.mm(ps[:, :], c.ones_b[:, :], sq[:, k, :], start=(k == 0), stop=(k == 7))
            rs = c.rs[i % 2]
            p.act(rs[:, :], ps[:, :], AF.Ln, bias=c.eps_ap[:, :], scale=1.0 / D)
            p.act(rs[:, :], rs[:, :], AF.Exp, scale=-0.5)
            o = ot[i % 2]
            for k in range(8):
                p.stt(o[:, k, :], c.xT[:, k, tb:tb + 512], c.g_fin[:, k:k + 1], rs[:, :], ALU.mult, ALU.mult)
            p.dma("sp", out[:, :, tb:tb + 512].rearrange("c p t -> p c t"), o[:, :, :])

def lay(Wm, cw):
    K, N = Wm.shape
    return np.ascontiguousarray(Wm.reshape(K // 128, 128, N // cw, cw).transpose(2, 1, 0, 3))


def build(stop_after=None, dbg=()):
    nc = bass.Bass("TRN2", target_bir_lowering=False)
    p = Prog(nc)
    c = Ctx()
    W = {}

    def din(name, shape, dt=F32):
        W[name] = nc.dram_tensor(name, list(shape), dt, kind="ExternalInput").ap()
        return W[name]

    def dscr(name, shape, dt):
        kind = "ExternalOutput" if name in dbg else "Internal"
        return nc.dram_tensor(name, list(shape), dt, kind=kind).ap()

    din("xT_in", [8, 128, T])
    din("ident", [128, 128])
    din("g_mix", [2, 128, 8]); din("g_ffn", [2, 128, 8]); din("g_fin", [128, 8])
    din("w_q", [4, 128, 8, 128]); din("w_k", [4, 128, 8, 128]); din("w_iq", [8, 128, 8, 128])
    din("w_ik2", [1, 128, 8, 128]); din("w_v", [1, 128, 8, 512]); din("w_iw", [1, 128, 8, 16])
    din("w_u", [1, 128, 8, 512]); din("w_vg", [1, 128, 8, 512])
    din("gmlp_norm", [1, 512]); din("w_sT", [128, 8, 128]); din("b_s", [8, 128]); din("e8", [8, 512])
    din("gk2", [128, 1]); din("pow2", [128, NBIS]); din("oh1", [32, 383]); din("rel_bias", [32, 8])
    din("w_out0", [8, 128, 8, 128])
    for l in range(2):
        din("w_up%d" % l, [44, 128, 8, 128]); din("w_dn%d" % l, [8, 128, NJ, 128])
        din("ffn_cw%d" % l, [128, 44, 3]); din("ffn_cb%d" % l, [128, 44])
    din("w_cq", [8, 128, 8, 128]); din("w_cf", [8, 128, 8, 128]); din("w_cg", [8, 128, 8, 128]); din("w_ci", [2, 128, 8, 512])
    din("hgrn_lb", [128, 2, 8]); din("tri64", [64, 64]); din("c_out_norm", [128, 1]); din("w_out1", [8, 128, 8, 128])
    out = nc.dram_tensor("outT", [8, 128, T], F32, kind="ExternalOutput").ap()

    c.qT_d = dscr("qT_d", [4, 128, T], BF16)
    c.kT_d = dscr("kT_d", [4, 128, T], BF16)
    c.iqT_d = dscr("iqT_d", [8, 128, T], BF16)
    c.v_d = dscr("v_d", [T, 512], BF16)
    c.yT_d = dscr("yT_d", [8, 128, T], BF16)
    c.qh_d = dscr("qh_d", [8, 128, T], BF16)
    c.gate_d = dscr("gate_d", [8, 128, T], BF16)
    c.kk_d = dscr("kk_d", [8, 128, T], BF16)
    c.lg_d = dscr("lg_d", [8, 128, T], F32)
    c.i_d = dscr("i_d", [T, 1024], BF16)
    c.oT_d = dscr("oT_d", [8, 128, T], BF16)

    c.xT = p.sb("xT", [128, 8, T], F32, True)
    c.ident_f = p.sb("ident_f", [128, 128], F32, True)
    c.ident_b = p.sb("ident_b", [128, 128], BF16, True)
    c.ones_b = p.sb("ones_b", [128, 128], BF16, True)
    c.eps_ap = p.sb("eps_ap", [128, 1], F32, True)
    gm = p.sb("g_mix_sb", [128, 2, 8], F32, True)
    gf = p.sb("g_ffn_sb", [128, 2, 8], F32, True)
    gfin = p.sb("g_fin_sb", [128, 8], F32, True)
    c.psum = p.es.enter_context(nc.psum_tensor("psum", [128, 4096], F32))
    c.ps = [c.psum[:, i * 512:(i + 1) * 512] for i in range(8)]
    c.psb = [t.bitcast(BF16) for t in c.ps]
    c.psi = 0
    c.stop_after = stop_after
    c.dbg = dbg
    c.dscr = dscr

    p.dma("sp", c.ident_f[:, :], W["ident"])
    p.dma("pool", c.ident_b[:, :], W["ident"])
    p.memset("dve", c.ones_b[:, :], 1.0)
    p.memset("dve", c.eps_ap[:, :], EPS)
    for l in range(2):
        p.dma("sp", gm[:, l, :], W["g_mix"][l])
        p.dma("sp", gf[:, l, :], W["g_ffn"][l])
    p.dma("sp", gfin[:, :], W["g_fin"])
    for tb in range(4):
        p.dma("sp", c.xT[:, :, tb * 512:(tb + 1) * 512],
              W["xT_in"][:, :, tb * 512:(tb + 1) * 512].rearrange("k p t -> p k t"), sub={c.xT.name: tb})
    c.g_mix = [gm[:, l, :] for l in range(2)]
    c.g_ffn = [gf[:, l, :] for l in range(2)]
    c.g_fin = gfin

    layer0_mixer(p, c, W)
    stages = ["gmlp", "indexer", "attn", "mix0", "ffn0", "hproj", "hrec", "mix1", "ffn1", None]
    si_ = stages.index(stop_after)
    if si_ >= stages.index("ffn0"):
        conv_ffn(p, c, W, 0)
    if si_ >= stages.index("hproj"):
        hgrn_mixer(p, c, W)
    if si_ >= stages.index("ffn1"):
        conv_ffn(p, c, W, 1)
    if stop_after is None:
        final_norm(p, c, out)
        p.barrier()
        return nc, p, c

    for k in range(8):
        p.dma("sp", out[k], c.xT[:, k, :])
    p.barrier()
    return nc, p, c


def t5_onehot():
    rel = np.arange(-255, 128)
    half, max_exact = 16, 8
    ret = np.where(rel > 0, half, 0)
    n = np.abs(rel)
    nf = np.maximum(n, max_exact).astype(np.float32)
    large = max_exact + (np.log(nf / np.float32(max_exact)) / np.float32(np.log(128 / 8)) * np.float32(half - max_exact)).astype(np.int32)
    large = np.minimum(large, half - 1)
    bucket = ret + np.where(n < max_exact, n, large)
    oh = np.zeros((32, 383), np.float32)
    oh[bucket, np.arange(383)] = 1.0
    return oh


def host_inputs(inp, b):
    f = np.float32
    m = {}
    x = inp["x"][b]
    m["xT_in"] = np.ascontiguousarray(x.T.reshape(8, 128, T))
    m["ident"] = np.eye(128, dtype=f)
    m["g_mix"] = np.ascontiguousarray(inp["mix_norm"].reshape(2, 8, 128).transpose(0, 2, 1))
    m["g_ffn"] = np.ascontiguousarray(inp["ffn_norm"].reshape(2, 8, 128).transpose(0, 2, 1))
    m["g_fin"] = np.ascontiguousarray(inp["final_norm"].reshape(8, 128).T)
    Wi = inp["ab_w_in"][0]
    m["w_q"] = lay(Wi[:, 0:512], 128)
    m["w_k"] = lay(Wi[:, 512:1024], 128)
    m["w_v"] = lay(Wi[:, 1024:1536], 512)
    m["w_iq"] = lay(Wi[:, 1536:2560], 128)
    m["w_ik2"] = lay(np.concatenate([Wi[:, 2560:2624], Wi[:, 2560:2624]], axis=1), 128)
    m["w_iw"] = lay(Wi[:, 2624:2640], 16)
    m["w_u"] = lay(Wi[:, 2640:3152], 512)
    m["w_vg"] = lay(Wi[:, 3152:3664], 512)
    m["gmlp_norm"] = np.ascontiguousarray(inp["ab_gmlp_norm"][0].reshape(1, 512))
    m["w_sT"] = np.ascontiguousarray(inp["ab_w_s"][0].transpose(2, 0, 1))
    m["b_s"] = np.ascontiguousarray(inp["ab_b_s"][0])
    e8 = np.zeros((8, 512), f)
    for g in range(8):
        e8[g, g * 64:(g + 1) * 64] = 1.0
    m["e8"] = e8
    m["gk2"] = np.concatenate([inp["ab_idx_k_norm"][0], inp["ab_idx_k_norm"][0]]).reshape(128, 1)
    m["pow2"] = np.tile((0.5 ** np.arange(1, NBIS + 1)).astype(f)[None, :], (128, 1))
    m["oh1"] = t5_onehot()
    m["rel_bias"] = inp["rel_bias"]
    m["w_out0"] = lay(inp["ab_w_out"][0], 128)
    Wc = inp["c_w_in"][0]
    m["w_cq"] = lay(Wc[:, 0:1024], 128)
    m["w_cf"] = lay(Wc[:, 1024:2048], 128)
    m["w_ci"] = lay(Wc[:, 2048:3072], 512)
    m["w_cg"] = lay(Wc[:, 3072:4096], 128)
    m["hgrn_lb"] = inp["hgrn_lb"].reshape(2, 8, 128).transpose(2, 0, 1)
    m["tri64"] = np.triu(np.ones((64, 64), f))
    m["c_out_norm"] = inp["c_out_norm"][0].reshape(128, 1)
    m["w_out1"] = lay(inp["c_w_out"][0], 128)
    for l in range(2):
        m["w_up%d" % l] = lay(inp["ffn_w_up"][l], 128)
        m["w_dn%d" % l] = lay(inp["ffn_w_down"][l], 128)
        m["ffn_cw%d" % l] = inp["ffn_conv_w"][l].reshape(3, 44, 128).transpose(2, 1, 0)
        m["ffn_cb%d" % l] = inp["ffn_conv_b"][l].reshape(44, 128).T
    return {k: np.ascontiguousarray(v, dtype=f) for k, v in m.items()}


def kernel(**inputs):
    inp = {k: np.asarray(v) for k, v in inputs.items()}
    nc, p, c = build()
    in_maps = [host_inputs(inp, b) for b in range(8)]
    res = run_bass_kernel_spmd(nc, in_maps, core_ids=list(range(8)))
    outs = [np.asarray(r["outT"]).reshape(D, T).T for r in res.results]
    return np.stack(outs, axis=0).astype(np.float32)
```
